# Optimizing a Trainium2 kernel written in Bass

```python
import math
import jax, jax.numpy as jnp
from jax import lax
import numpy as np

D_MODEL = 4096
BATCH = 4
SEQ = 2048
DEPTH = 4

N_EVEN = (DEPTH + 1) // 2
N_ODD = DEPTH // 2
RWKV_WIDTH = D_MODEL // 2
RWKV_HEAD_DIM = 64
RWKV_HEADS = RWKV_WIDTH // RWKV_HEAD_DIM
DECAY_LORA = max(32, int(round(1.8 * D_MODEL ** 0.5 / 32)) * 32)
ICLR_LORA = max(32, int(round(1.8 * D_MODEL ** 0.5 / 32)) * 32)
GATE_LORA = max(32, int(round(0.6 * D_MODEL ** 0.8 / 32)) * 32)
RWKV_COLS = 3 * RWKV_WIDTH + DECAY_LORA + ICLR_LORA + GATE_LORA
RWKV_LN_EPS = 64e-5
MOBA_WIDTH = D_MODEL - RWKV_WIDTH
MOBA_HEAD_DIM = 128
MOBA_HEADS = MOBA_WIDTH // MOBA_HEAD_DIM
MOBA_BLOCK = 256
MOBA_TOPK = 3
MOBA_QCHUNK = 8
ROPE_THETA = 10000.0
N_IN = RWKV_COLS + 3 * MOBA_WIDTH
POOL_WINDOWS = (2, 4, 8, 16)
POOL_GROUPS = len(POOL_WINDOWS)
POOL_GROUP_DIM = D_MODEL // POOL_GROUPS
FFN_DIM = 256 * math.ceil(8 * D_MODEL / 3 / 256)
N_EXPERTS = 8
MOE_TOPK = 2
EXPERT_DIM = 7 * D_MODEL // 16
N_MOD = 6
LN_EPS = 1e-5
DEEPNORM_ALPHA = (2 * DEPTH) ** 0.25
DEEPNORM_BETA = (8 * DEPTH) ** -0.25
NEG_INF = -1e30

kernel_name = "hybrid_rwkv7_moba_pool_moe_trunk"


def layer_norm(x, g, b):
    xf = x.astype(jnp.float32)
    mu = jnp.mean(xf, axis=-1, keepdims=True)
    var = jnp.mean(jnp.square(xf - mu), axis=-1, keepdims=True)
    return ((xf - mu) * lax.rsqrt(var + LN_EPS) * g + b).astype(x.dtype)


def rope(t):
    T_, Dh = t.shape[1], t.shape[-1]
    inv = jnp.power(ROPE_THETA, -jnp.arange(0, Dh, 2, dtype=jnp.float32) / Dh)
    ang = jnp.arange(T_, dtype=jnp.float32)[:, None] * inv[None, :]
    cos = jnp.cos(ang)[None, :, None, :]
    sin = jnp.sin(ang)[None, :, None, :]
    t1 = t[..., : Dh // 2].astype(jnp.float32)
    t2 = t[..., Dh // 2:].astype(jnp.float32)
    return jnp.concatenate([t1 * cos - t2 * sin, t2 * cos + t1 * sin], axis=-1).astype(t.dtype)


def wkv7_scan(r, w, k, v, a, b):
    B_, _, H, N = r.shape

    def step(S, inp):
        r_t, w_t, k_t, v_t, a_t, b_t = inp
        sa = jnp.einsum('bhij,bhj->bhi', S, a_t)
        S = S * w_t[:, :, None, :] + sa[..., None] * b_t[:, :, None, :] + v_t[..., None] * k_t[:, :, None, :]
        return S, jnp.einsum('bhij,bhj->bhi', S, r_t)

    xs = tuple(jnp.swapaxes(t, 0, 1) for t in (r, w, k, v, a, b))
    S0 = jnp.zeros((B_, H, N, N), jnp.float32)
    _, y = lax.scan(step, S0, xs)
    return jnp.swapaxes(y, 0, 1)


def rwkv7_time_mix(zA, mu, w0, w2, a0, a2, g2, k_k, k_a, r_k, lnx_g, lnx_b):
    B_, T_, _ = zA.shape
    z_prev = jnp.pad(zA, ((0, 0), (1, 0), (0, 0)))[:, :-1]
    zA = zA + (z_prev - zA) * mu
    DA = RWKV_WIDTH
    r, k, v, wl, al, gl = jnp.split(
        zA, [DA, 2 * DA, 3 * DA, 3 * DA + DECAY_LORA, 3 * DA + DECAY_LORA + ICLR_LORA], axis=-1)
    w = -jax.nn.softplus(-(w0 + jnp.tanh(wl) @ w2)) - 0.5
    a = jax.nn.sigmoid(a0 + al @ a2)
    g = jax.nn.sigmoid(gl) @ g2
    heads = lambda t: t.reshape(B_, T_, RWKV_HEADS, RWKV_HEAD_DIM).astype(jnp.float32)
    kk = heads(k * k_k)
    kk = kk / jnp.maximum(jnp.sqrt(jnp.sum(kk * kk, axis=-1, keepdims=True)), 1e-12)
    k = k * (1 + (a - 1) * k_a)
    decay = jnp.exp(-jnp.exp(heads(w)))
    rh, kh, vh = heads(r), heads(k), heads(v)
    y = wkv7_scan(rh, decay, kh, vh, -kk, kk * heads(a))
    mean = jnp.mean(y, axis=-1, keepdims=True)
    var = jnp.mean(jnp.square(y - mean), axis=-1, keepdims=True)
    y = (y - mean) * lax.rsqrt(var + RWKV_LN_EPS)
    y = y * lnx_g.reshape(RWKV_HEADS, RWKV_HEAD_DIM) + lnx_b.reshape(RWKV_HEADS, RWKV_HEAD_DIM)
    bonus = jnp.sum(rh * kh * r_k.astype(jnp.float32), axis=-1, keepdims=True) * vh
    y = (y + bonus).reshape(B_, T_, RWKV_WIDTH).astype(zA.dtype)
    return y * g


def moba_attention(q, k, v):
    B_, T_, H, Dh = q.shape
    BS, QC = MOBA_BLOCK, MOBA_QCHUNK
    nb = -(-T_ // BS)
    Tp = nb * BS
    pad = ((0, 0), (0, Tp - T_), (0, 0), (0, 0))
    q, k, v = [jnp.swapaxes(jnp.pad(t, pad), 1, 2) for t in (q, k, v)]
    kb = k.reshape(B_, H, nb, BS, Dh)
    vb = v.reshape(B_, H, nb, BS, Dh)
    kmean = jnp.mean(kb.astype(jnp.float32), axis=3)
    n_sel = min(MOBA_TOPK, nb - 1)
    n_chunks = Tp // QC
    qc = jnp.moveaxis(q.reshape(B_, H, n_chunks, QC, Dh), 2, 0)
    bi = jnp.arange(B_)[:, None, None, None]
    hi = jnp.arange(H)[None, :, None, None]
    scale = Dh ** -0.5

    def chunk(args):
        ci, q_c = args
        q0 = ci * QC
        blk = q0 // BS
        qpos = q0 + jnp.arange(QC)
        kpos = blk * BS + jnp.arange(BS)
        k_own = lax.dynamic_index_in_dim(kb, blk, axis=2, keepdims=False)
        v_own = lax.dynamic_index_in_dim(vb, blk, axis=2, keepdims=False)
        s_own = jnp.einsum('bhqd,bhkd->bhqk', q_c, k_own).astype(jnp.float32) * scale
        s_own = jnp.where(kpos[None, :] <= qpos[:, None], s_own, NEG_INF)
        if n_sel == 0:
            p = jax.nn.softmax(s_own, axis=-1).astype(v_own.dtype)
            return jnp.einsum('bhqk,bhkd->bhqd', p, v_own)
        gate = jnp.einsum('bhqd,bhnd->bhqn', q_c.astype(jnp.float32), kmean)
        gate = jnp.where(jnp.arange(nb) < blk, gate, -jnp.inf)
        _, idx = lax.top_k(gate, n_sel)
        k_sel = kb[bi, hi, idx]
        v_sel = vb[bi, hi, idx]
        s_sel = jnp.einsum('bhqd,bhqskd->bhqsk', q_c, k_sel).astype(jnp.float32) * scale
        s_sel = jnp.where((jnp.arange(n_sel) < blk)[:, None], s_sel, NEG_INF)
        s = jnp.concatenate([s_own, s_sel.reshape(B_, H, QC, n_sel * BS)], axis=-1)
        p = jax.nn.softmax(s, axis=-1).astype(v_own.dtype)
        p_own = p[..., :BS]
        p_sel = p[..., BS:].reshape(B_, H, QC, n_sel, BS)
        return (jnp.einsum('bhqk,bhkd->bhqd', p_own, v_own)
                + jnp.einsum('bhqsk,bhqskd->bhqd', p_sel, v_sel))

    out = lax.map(chunk, (jnp.arange(n_chunks), qc))
    out = jnp.moveaxis(out, 0, 2).reshape(B_, H, Tp, Dh)[:, :, :T_]
    return jnp.swapaxes(out, 1, 2)


def rwkv_moba_mixer(h, w_in, mu, w0, w2, a0, a2, g2, k_k, k_a, r_k, lnx_g, lnx_b, w_out):
    B_, T_, _ = h.shape
    z = jnp.einsum('btd,dn->btn', h, w_in)
    zA = z[..., :RWKV_COLS]
    zq, zk, zv = jnp.split(z[..., RWKV_COLS:], 3, axis=-1)
    yA = rwkv7_time_mix(zA, mu, w0, w2, a0, a2, g2, k_k, k_a, r_k, lnx_g, lnx_b)
    hsplit = lambda t: t.reshape(B_, T_, MOBA_HEADS, MOBA_HEAD_DIM)
    yB = moba_attention(rope(hsplit(zq)), rope(hsplit(zk)), hsplit(zv)).reshape(B_, T_, MOBA_WIDTH)
    return jnp.einsum('btc,cd->btd', jnp.concatenate([yA, yB], axis=-1), w_out)


def multiscale_pool_mixer(h, pool_w, pool_scale):
    B_, T_, _ = h.shape
    hg = h.reshape(B_, T_, POOL_GROUPS, POOL_GROUP_DIM)
    cs = jnp.cumsum(hg.astype(jnp.float32), axis=1)
    t_cnt = jnp.arange(1, T_ + 1, dtype=jnp.float32)
    pooled = []
    for g, win in enumerate(POOL_WINDOWS):
        c_g = cs[:, :, g]
        lag = jnp.pad(c_g, ((0, 0), (win, 0), (0, 0)))[:, :T_]
        cnt = jnp.minimum(t_cnt, float(win))[None, :, None]
        pooled.append((c_g - lag) / cnt - hg[:, :, g].astype(jnp.float32))
    p = jnp.stack(pooled, axis=2).astype(h.dtype)
    y = jnp.einsum('btgc,gce->btge', p, pool_w).reshape(B_, T_, D_MODEL)
    return y * pool_scale


def swiglu(h, w_gate, w_up, w_down):
    return (jax.nn.silu(h @ w_gate) * (h @ w_up)) @ w_down


def moe_swiglu(h, router_w, router_b, w_gate, w_up, w_down):
    logits = (h @ router_w).astype(jnp.float32) + router_b.astype(jnp.float32)
    top_val, top_idx = lax.top_k(logits, MOE_TOPK)
    gates = jax.nn.softmax(top_val, axis=-1)
    combine = jnp.sum(jax.nn.one_hot(top_idx, N_EXPERTS, dtype=jnp.float32) * gates[..., None], axis=-2)
    combine = combine.astype(h.dtype)
    out = jnp.zeros_like(h)
    for e in range(N_EXPERTS):
        out = out + combine[..., e:e + 1] * swiglu(h, w_gate[e], w_up[e], w_down[e])
    return out


def setup_inputs(seed: int = 0) -> dict:
    key = jax.random.key(seed)
    ks = iter(jax.random.split(key, 48))
    f32 = jnp.float32

    def nrm(shape, scale):
        return jax.random.normal(next(ks), shape, f32) * scale

    D = D_MODEL
    gate_rows = jnp.array([0.0, 0.0, 1.0, 0.0, 0.0, 1.0], f32)[None, :, None]
    return {
        "x": nrm((BATCH, SEQ, D), 1.0),
        "c": nrm((BATCH, D), 1.0),
        "ada_w": nrm((D, N_MOD * D), 0.1 * D ** -0.5),
        "ada_b": nrm((N_MOD * D,), 0.02),
        "ada_table": nrm((DEPTH, N_MOD, D), 0.02) + gate_rows,
        "ln_g": 1.0 + nrm((DEPTH, 2, D), 0.02),
        "ln_b": nrm((DEPTH, 2, D), 0.02),
        "mix_w_in": nrm((N_EVEN, D, N_IN), D ** -0.5),
        "mix_mu": jax.random.uniform(next(ks), (N_EVEN, RWKV_COLS), f32),
        "rwkv_w0": jax.random.uniform(next(ks), (N_EVEN, RWKV_WIDTH), f32, -6.0, 1.0),
        "rwkv_w2": nrm((N_EVEN, DECAY_LORA, RWKV_WIDTH), 0.5 * DECAY_LORA ** -0.5),
        "rwkv_a0": nrm((N_EVEN, RWKV_WIDTH), 0.1),
        "rwkv_a2": nrm((N_EVEN, ICLR_LORA, RWKV_WIDTH), 0.5 * ICLR_LORA ** -0.5),
        "rwkv_g2": nrm((N_EVEN, GATE_LORA, RWKV_WIDTH), 2.0 * GATE_LORA ** -0.5),
        "rwkv_kk": 0.85 + nrm((N_EVEN, RWKV_WIDTH), 0.02),
        "rwkv_ka": 1.0 + nrm((N_EVEN, RWKV_WIDTH), 0.02),
        "rwkv_rk": nrm((N_EVEN, RWKV_HEADS, RWKV_HEAD_DIM), 0.1),
        "rwkv_lnx_g": 1.0 + nrm((N_EVEN, RWKV_WIDTH), 0.02),
        "rwkv_lnx_b": nrm((N_EVEN, RWKV_WIDTH), 0.02),
        "mix_w_out": nrm((N_EVEN, D, D), DEEPNORM_BETA * D ** -0.5),
        "ffn_w_gate": nrm((N_EVEN, D, FFN_DIM), D ** -0.5),
        "ffn_w_up": nrm((N_EVEN, D, FFN_DIM), D ** -0.5),
        "ffn_w_down": nrm((N_EVEN, FFN_DIM, D), DEEPNORM_BETA * FFN_DIM ** -0.5),
        "pool_w": nrm((N_ODD, POOL_GROUPS, POOL_GROUP_DIM, POOL_GROUP_DIM), DEEPNORM_BETA * POOL_GROUP_DIM ** -0.5),
        "pool_scale": 1.0 + nrm((N_ODD, D), 0.02),
        "moe_router_w": nrm((N_ODD, D, N_EXPERTS), D ** -0.5),
        "moe_router_b": nrm((N_ODD, N_EXPERTS), 0.01),
        "moe_w_gate": nrm((N_ODD, N_EXPERTS, D, EXPERT_DIM), D ** -0.5),
        "moe_w_up": nrm((N_ODD, N_EXPERTS, D, EXPERT_DIM), D ** -0.5),
        "moe_w_down": nrm((N_ODD, N_EXPERTS, EXPERT_DIM, D), DEEPNORM_BETA * EXPERT_DIM ** -0.5),
    }


def reference(x, c, ada_w, ada_b, ada_table, ln_g, ln_b,
              mix_w_in, mix_mu, rwkv_w0, rwkv_w2, rwkv_a0, rwkv_a2, rwkv_g2,
              rwkv_kk, rwkv_ka, rwkv_rk, rwkv_lnx_g, rwkv_lnx_b, mix_w_out,
              ffn_w_gate, ffn_w_up, ffn_w_down,
              pool_w, pool_scale,
              moe_router_w, moe_router_b, moe_w_gate, moe_w_up, moe_w_down):
    B_ = x.shape[0]
    mod = (jax.nn.silu(c) @ ada_w + ada_b).reshape(B_, N_MOD, D_MODEL)
    for l in range(DEPTH):
        m = mod + ada_table[l][None]
        sh1, sc1, g1, sh2, sc2, g2 = [m[:, i, None, :] for i in range(N_MOD)]
        i = l // 2
        h = x * (1 + sc1) + sh1
        if l % 2 == 0:
            y = rwkv_moba_mixer(h, mix_w_in[i], mix_mu[i], rwkv_w0[i], rwkv_w2[i], rwkv_a0[i],
                                rwkv_a2[i], rwkv_g2[i], rwkv_kk[i], rwkv_ka[i], rwkv_rk[i],
                                rwkv_lnx_g[i], rwkv_lnx_b[i], mix_w_out[i])
        else:
            y = multiscale_pool_mixer(h, pool_w[i], pool_scale[i])
        x = layer_norm(DEEPNORM_ALPHA * x + g1 * y, ln_g[l, 0], ln_b[l, 0])
        h = x * (1 + sc2) + sh2
        if l % 2 == 0:
            y = swiglu(h, ffn_w_gate[i], ffn_w_up[i], ffn_w_down[i])
        else:
            y = moe_swiglu(h, moe_router_w[i], moe_router_b[i], moe_w_gate[i], moe_w_up[i], moe_w_down[i])
        x = layer_norm(DEEPNORM_ALPHA * x + g2 * y, ln_g[l, 1], ln_b[l, 1])
    return x
```

```python
import numpy as np
from contextlib import ExitStack
import concourse.bass as bass
import concourse.mybir as mybir
from concourse.bass_utils import run_bass_kernel_spmd

F32 = mybir.dt.float32
BF16 = mybir.dt.bfloat16
AF = mybir.ActivationFunctionType
ALU = mybir.AluOpType
AX = mybir.AxisListType


class _Op:
    __slots__ = ("eng", "fn", "deps", "inc", "sem", "val", "dma", "idx", "waits", "call")


class _Rec:
    def __init__(self):
        self.call = None

    def __getattr__(self, name):
        def f(*a, **k):
            self.call = (name, a, k)
        return f


class MK:
    BLK = {"pe": "tensor", "act": "scalar", "dve": "vector", "pool": "gpsimd", "sp": "sync"}
    EPOCH = 4000
    NSLOT = 12

    _uid = 0

    def __init__(self, inorder=("pe",), nc=None):
        MK._uid += 1
        self.pfx = "" if nc is None else f"k{MK._uid}_"
        self.nc = nc if nc is not None else bass.Bass("TRN2", target_bir_lowering=False)
        self.st = ExitStack()
        self.ops = []
        self.lastw = {}
        self.readers = {}
        self.nname = 0
        self.inorder = set(inorder)

    def dram(self, name, shape, dt, kind, addr_space=None):
        if addr_space is not None:
            return self.nc.dram_tensor(name, list(shape), dt, kind=kind, addr_space=addr_space).ap()
        return self.nc.dram_tensor(name, list(shape), dt, kind=kind).ap()

    def sb(self, shape, dt, name=None):
        self.nname += 1
        return self.st.enter_context(self.nc.sbuf_tensor(self.pfx + (name or f"sb{self.nname}"), list(shape), dt))

    def ps(self, shape, dt=F32, name=None):
        self.nname += 1
        return self.st.enter_context(self.nc.psum_tensor(self.pfx + (name or f"ps{self.nname}"), list(shape), dt))

    def add(self, eng, fn, r=(), w=(), dma=False):
        op = _Op()
        op.eng, op.fn, op.dma, op.inc = eng, fn, dma, dma
        op.idx = len(self.ops)
        rec = _Rec(); fn(rec); op.call = rec.call
        assert op.call is not None
        op.sem = op.val = None
        isps = lambda k: (isinstance(k, tuple) and str(k[0]).startswith("ps")) or (isinstance(k, str) and k.startswith("ps"))
        w = list(w) + [k for k in r if isps(k) and k not in w]
        deps = set()
        for k in r:
            if k in self.lastw:
                deps.add(self.lastw[k])
        for k in w:
            if k in self.lastw:
                deps.add(self.lastw[k])
            for rd in self.readers.get(k, {}).values():
                deps.add(rd)
        deps.discard(op.idx)
        op.deps = deps
        for k in w:
            self.lastw[k] = op.idx
            self.readers[k] = {}
        for k in r:
            d = self.readers.setdefault(k, {})
            d[(eng, op.idx) if dma else eng] = op.idx
        self.ops.append(op)
        return op

    def pe(self, fn, r=(), w=()): return self.add("pe", fn, r, w)
    def act(self, fn, r=(), w=()): return self.add("act", fn, r, w)
    def dve(self, fn, r=(), w=()): return self.add("dve", fn, r, w)
    def pool(self, fn, r=(), w=()): return self.add("pool", fn, r, w)
    def dma(self, out, in_, r=(), w=(), eng="sp", **kw):
        return self.add(eng, lambda e: e.dma_start(out=out, in_=in_, **kw), r, w, dma=True)

    def coll(self, kind, op, groups, in_ap, out_ap, r=(), w=()):
        return self.add("pool", lambda e: e.collective_compute(kind, op, replica_groups=groups, ins=[in_ap], outs=[out_ap]), r, w, dma=True)

    def build(self):
        nc = self.nc
        ops = self.ops
        for op in ops:
            for d in op.deps:
                dop = ops[d]
                if dop.dma or dop.eng != op.eng or op.eng not in self.inorder or op.dma:
                    dop.inc = True
        cnt = {}
        semobj = {}

        def getsem(key):
            if key not in semobj:
                semobj[key] = nc.alloc_semaphore(name=self.pfx + "s_%s_%s" % key)
            return semobj[key]

        dcnt = {}
        slotval = {}
        for op in ops:
            op.waits = []
            if op.dma:
                n = dcnt.get(op.eng, 0)
                dcnt[op.eng] = n + 1
                key = ("d" + op.eng, n % self.NSLOT)
                prev = slotval.get(key, 0)
                if prev:
                    op.waits.append((getsem(key), prev))
                op.sem, op.val = getsem(key), prev + 16
                slotval[key] = prev + 16
            elif op.inc:
                n = cnt.get(op.eng, 0)
                cnt[op.eng] = n + 1
                op.sem, op.val = getsem((op.eng, n // self.EPOCH)), n % self.EPOCH + 1
        for op in ops:
            for d in sorted(op.deps):
                dop = ops[d]
                if (not dop.dma) and (not op.dma) and dop.eng == op.eng and op.eng in self.inorder:
                    continue
                op.waits.append((dop.sem, dop.val))
        finals = [(getsem(k), v) for k, v in slotval.items()]
        with nc.Block() as block:
            for eng, bname in self.BLK.items():
                eops = [op for op in ops if op.eng == eng]
                if not eops and eng != "sp":
                    continue

                def body(e, eops=eops, eng=eng):
                    waited = {}
                    for op in eops:
                        for sem, val in op.waits:
                            if waited.get(id(sem), 0) >= val:
                                continue
                            e.wait_ge(sem, val)
                            waited[id(sem)] = val
                        ins = getattr(e, op.call[0])(*op.call[1], **op.call[2])
                        if op.inc:
                            ins.then_inc(op.sem, 16 if op.dma else 1)
                    if eng == "sp":
                        for sem, val in finals:
                            if waited.get(id(sem), 0) < val:
                                e.wait_ge(sem, val)

                getattr(block, bname)(body)
        nc.clear_and_free_semaphores(list(semobj.values()))
        nc.all_engine_barrier()
        self.st.close()
        return nc


def run(mk_or_nc, in_maps, trace=False):
    nc = mk_or_nc.nc if isinstance(mk_or_nc, MK) else mk_or_nc
    return run_bass_kernel_spmd(nc, in_maps, core_ids=list(range(len(in_maps))), trace=trace)


T = 2048
L = 64
NCH = T // L
C0 = float(np.exp(-0.5))
NEG = -1e30


class Ring:
    def __init__(self, tiles, name):
        self.t = tiles; self.n = 0; self.name = name

    def get(self):
        i = self.n % len(self.t); self.n += 1
        return self.t[i], (self.name, i)


def build_pb(do_rwkv=True, do_moba=True, npairs=8, nheads=8, nt=None, stage=3, nc=None, io=None, tag=''):
    m = MK(nc=nc)
    PADDED = io is None
    if io is None:
        zrkv = m.dram("zrkv", [3, 1024, T + 1], F32, "ExternalInput")
        zl = m.dram("zl", [768, T + 1], F32, "ExternalInput")
        mu_rkv = m.dram("mu_rkv", [128, 24], F32, "ExternalInput")
        mu_l = m.dram("mu_l", [128, 6], F32, "ExternalInput")
        w2 = m.dram("w2", [128, 1024], F32, "ExternalInput")
        a2 = m.dram("a2", [128, 1024], F32, "ExternalInput")
        g2 = m.dram("g2", [128, 4, 1024], F32, "ExternalInput")
        pp = m.dram("pp", [128, 8, 8], F32, "ExternalInput")
        consts = m.dram("consts", [128, 6, 128], F32, "ExternalInput")
        zq = m.dram("zq", [1024, T], F32, "ExternalInput")
        zk = m.dram("zkm", [1024, T], F32, "ExternalInput")
        zv = m.dram("zvm", [128, 16, 1024], F32, "ExternalInput")
        rope = m.dram("rope", [128, 2, T], F32, "ExternalInput")
        cmask = m.dram("cmask", [128, 2, 256], F32, "ExternalInput")
        identb = m.dram("identb", [128, 128], F32, "ExternalInput")
        yT = m.dram("yT", [2048, T], F32, "ExternalOutput")


    else:
        zrkv = io["zrkv"]; zl = io["zl"]; mu_rkv = io["mu_rkv"]; mu_l = io["mu_l"]; w2 = io["w2"]; a2 = io["a2"]; g2 = io["g2"]; pp = io["pp"]
        consts = io["consts"]; zq = io["zq"]; zk = io["zkm"]; zvT = io["zvT"]; rope = io["rope"]; cmask = io["cmask"]; identb = io["identb"]; yT = None
    yT_r = yT[0:1024, :] if io is None else io["yT_r"]
    yT_m = yT[1024:2048, :] if io is None else io["yT_m"]
    cst = m.sb([128, 6, 128], F32, "cst")
    m.dma(cst[:], consts, w=["cst"])
    ident = cst[:, 0, :]; bones = cst[:, 1, :]; bones64 = cst[:, 2, :]; rsw = cst[:, 3, :]
    ppt = m.sb([128, 8, 8], F32, "ppt"); m.dma(ppt[:], pp, w=["pp"])
    m.dve(lambda e: e.tensor_scalar(out=ppt[:, 7, :], in0=ppt[:, 3, :], scalar1=-1.0, scalar2=1.0, op0=ALU.mult, op1=ALU.add), r=["pp"], w=["pp"])
    murkv = m.sb([128, 24], F32, "murkv"); m.dma(murkv[:], mu_rkv, w=["mu"])
    mul = m.sb([128, 6], F32, "mul"); m.dma(mul[:], mu_l, w=["mu"])
    psr = Ring([m.ps([128, 512], F32, f"psb{i}") for i in range(7)], "ps")
    psb16 = m.ps([128, 1024], BF16, "psb16")

    if do_rwkv:
        w2t = m.sb([128, 1024], F32, "w2t"); m.dma(w2t[:], w2, w=["w2"])
        a2t = m.sb([128, 1024], F32, "a2t"); m.dma(a2t[:], a2, w=["a2"])
        g2t = m.sb([128, 4, 1024], F32, "g2t"); m.dma(g2t[:], g2, w=["g2"])
        TT = 512
        NT = nt or (T // TT)
        CPT = TT // L
        gmask = m.sb([64, 512], F32, "gmask")
        for q in range(4):
            m.dve(lambda e, q=q: e.tensor_copy(out=gmask[:, q * 128:(q + 1) * 128], in_=cst[0:64, 4, :]), r=["cst"], w=["gmask"])
        lmask = m.sb([64, 128], F32, "lmask")
        for q in range(2):
            m.dve(lambda e, q=q: e.tensor_copy(out=lmask[:, q * 64:(q + 1) * 64], in_=cst[0:64, 5, 0:64]), r=["cst"], w=["lmask"])
        rmask = m.sb([128, TT], F32, "rmask")
        m.dve(lambda e: e.memset(rmask[:], 1.0), w=["rmask"])
        m.dve(lambda e: e.memset(rmask[:].rearrange("p (c l) -> p c l", l=L)[:, :, 0:1], 0.0), r=["rmask"], w=["rmask"])
        P = [m.sb([64, 2, 64], F32, f"P{p}") for p in range(npairs)]
        for p in range(npairs):
            m.dve(lambda e, p=p: e.memset(P[p][:], 0.0), w=[("P", p)])
        lor = [m.sb([128, TT], F32, f"lor{i}") for i in range(6)]
        zring = Ring([m.sb([128, TT + 1], F32, f"zraw{i}") for i in range(3)], "zraw")
        NSLOT = 1
        names = ["r", "k", "v", "a", "sg", "cl", "kkn", "kmod", "e1", "t1", "kt", "bt", "g", "y", "t2", "t3"]
        slots = [{n: m.sb([128, TT], F32, f"{n}_{s}") for n in names} for s in range(NSLOT)]
        ARs = [m.sb([128, CPT, 2, L], F32, f"AR{s}") for s in range(NSLOT)]
        los = [dict(AR=m.sb([64, CPT, 2, L], F32, f"ARlo{s}"), bt=m.sb([64, TT], F32, f"btlo{s}"), kt=m.sb([64, TT], F32, f"ktlo{s}"),
                    e1=m.sb([64, TT], F32, f"e1lo{s}"), y=m.sb([64, 2, TT], F32, f"ylo{s}")) for s in range(NSLOT)]
        small = Ring([m.sb([64, 256], F32, f"sm{i}") for i in range(10)], "sm")
        gmr = Ring([m.sb([64, 512], F32, f"gm{i}") for i in range(2)], "gm")
        tokr = Ring([m.sb([64, 384], F32, f"tok{i}") for i in range(2)], "tok")
        xur = Ring([m.sb([64, 128], F32, f"xu{i}") for i in range(4)], "xu")

        def shift(dst, dkey, src_dram_rows, t0, mucol, extra_act=None):
            zt, zkey = zring.get()
            if PADDED:
                m.dma(zt[:], src_dram_rows[:, t0:t0 + TT + 1], w=[zkey])
            elif t0 == 0:
                m.dve(lambda e: e.memset(zt[:, 0:1], 0.0), w=[zkey])
                m.dma(zt[:, 1:TT + 1], src_dram_rows[:, 0:TT], r=[zkey], w=[zkey])
            else:
                m.dma(zt[:], src_dram_rows[:, t0 - 1:t0 + TT], w=[zkey])
            m.dve(lambda e: e.tensor_tensor(out=dst[:], in0=zt[:, 0:TT], in1=zt[:, 1:TT + 1], op=ALU.subtract),
                  r=[zkey], w=[dkey])
            m.dve(lambda e: e.scalar_tensor_tensor(out=dst[:], in0=dst[:], scalar=mucol, in1=zt[:, 1:TT + 1],
                                                   op0=ALU.mult, op1=ALU.add), r=[zkey, dkey, "mu"], w=[dkey])

        for ti in range(NT):
            t0 = ti * TT
            for i in range(6):
                shift(lor[i], ("lor", i), zl[i * 128:(i + 1) * 128, :], t0, mul[:, i:i + 1])
                if i == 0:
                    m.act(lambda e: e.activation(out=lor[0][:], in_=lor[0][:], func=AF.Tanh), r=[("lor", 0)], w=[("lor", 0)])
                elif i >= 2:
                    m.act(lambda e, i=i: e.activation(out=lor[i][:], in_=lor[i][:], func=AF.Sigmoid), r=[("lor", i)], w=[("lor", i)])
            for p in range(npairs):
                s = p % NSLOT
                B = slots[s]
                K = lambda n: (n, s)
                pc = slice(p * 128, (p + 1) * 128)
                par = lambda j: ppt[:, j, p:p + 1]
                AR = ARs[s]
                shift(B["r"], K("r"), zrkv[0, pc, :], t0, murkv[:, p:p + 1])
                shift(B["k"], K("k"), zrkv[1, pc, :], t0, murkv[:, 8 + p:9 + p])
                shift(B["v"], K("v"), zrkv[2, pc, :], t0, murkv[:, 16 + p:17 + p])
                ps, pk = psr.get()
                m.pe(lambda e, ps=ps: e.matmul(ps[:, :], lhsT=w2t[:, pc], rhs=lor[0][:], start=True, stop=True),
                     r=["w2", ("lor", 0)], w=[pk])
                m.act(lambda e, ps=ps: e.activation(out=B["sg"][:], in_=ps[:, :], func=AF.Sigmoid, bias=par(0)),
                      r=[pk, "pp"], w=[K("sg")])
                ps, pk = psr.get()
                m.pe(lambda e, ps=ps: e.matmul(ps[:, :], lhsT=a2t[:, pc], rhs=lor[1][:], start=True, stop=True),
                     r=["a2", ("lor", 1)], w=[pk])
                m.act(lambda e, ps=ps: e.activation(out=B["a"][:], in_=ps[:, :], func=AF.Sigmoid, bias=par(1)),
                      r=[pk, "pp"], w=[K("a")])
                ps, pk = psr.get()
                for j in range(4):
                    m.pe(lambda e, ps=ps, j=j: e.matmul(ps[:, :], lhsT=g2t[:, j, pc], rhs=lor[2 + j][:], start=(j == 0), stop=(j == 3)),
                         r=["g2", ("lor", 2 + j)], w=[pk])
                m.act(lambda e, ps=ps: e.copy(out=B["g"][:], in_=ps[:, :]), r=[pk], w=[K("g")])
                m.dve(lambda e: e.tensor_scalar(out=B["kkn"][:], in0=B["k"][:], scalar1=par(2), scalar2=None, op0=ALU.mult),
                      r=[K("k"), "pp"], w=[K("kkn")])
                m.act(lambda e: e.activation(out=B["t1"][:], in_=B["kkn"][:], func=AF.Square), r=[K("kkn")], w=[K("t1")])
                ps, pk = psr.get()
                m.pe(lambda e, ps=ps: e.matmul(ps[:, :], lhsT=bones, rhs=B["t1"][:], start=True, stop=True), r=["cst", K("t1")], w=[pk])
                m.dve(lambda e, ps=ps: e.tensor_scalar(out=B["t1"][:], in0=ps[:, :], scalar1=1e-24, scalar2=None, op0=ALU.max),
                      r=[pk], w=[K("t1")])
                m.act(lambda e: e.activation(out=B["t1"][:], in_=B["t1"][:], func=AF.Ln), r=[K("t1")], w=[K("t1")])
                m.act(lambda e: e.activation(out=B["t1"][:], in_=B["t1"][:], func=AF.Exp, scale=-0.5), r=[K("t1")], w=[K("t1")])
                m.dve(lambda e: e.tensor_tensor(out=B["kkn"][:], in0=B["kkn"][:], in1=B["t1"][:], op=ALU.mult),
                      r=[K("kkn"), K("t1")], w=[K("kkn")])
                m.dve(lambda e: e.tensor_scalar(out=B["kmod"][:], in0=B["a"][:], scalar1=par(3), scalar2=par(7), op0=ALU.mult, op1=ALU.add),
                      r=[K("a"), "pp"], w=[K("kmod")])
                m.dve(lambda e: e.tensor_tensor(out=B["kmod"][:], in0=B["kmod"][:], in1=B["k"][:], op=ALU.mult),
                      r=[K("kmod"), K("k")], w=[K("kmod")])
                m.dve(lambda e: e.tensor_tensor_scan(out=B["cl"][:], data0=rmask[:], data1=B["sg"][:], initial=0.0, op0=ALU.mult, op1=ALU.add),
                      r=["rmask", K("sg")], w=[K("cl")])
                m.act(lambda e: e.activation(out=B["e1"][:], in_=B["cl"][:], func=AF.Exp, scale=-C0), r=[K("cl")], w=[K("e1")])
                m.dve(lambda e: e.tensor_tensor(out=AR[:, :, 1, :], in0=B["r"][:].rearrange("p (c l) -> p c l", l=L),
                                                in1=B["e1"][:].rearrange("p (c l) -> p c l", l=L), op=ALU.mult),
                      r=[K("r"), K("e1")], w=[("AR", s)])
                m.dve(lambda e: e.tensor_tensor(out=B["t1"][:], in0=B["cl"][:], in1=B["sg"][:], op=ALU.subtract),
                      r=[K("cl"), K("sg")], w=[K("t1")])
                m.act(lambda e: e.activation(out=B["t1"][:], in_=B["t1"][:], func=AF.Exp, scale=-C0), r=[K("t1")], w=[K("t1")])
                m.dve(lambda e: e.scalar_tensor_tensor(out=AR[:, :, 0, :], in0=B["kkn"][:].rearrange("p (c l) -> p c l", l=L), scalar=-1.0,
                                                       in1=B["t1"][:].rearrange("p (c l) -> p c l", l=L), op0=ALU.mult, op1=ALU.mult),
                      r=[K("kkn"), K("t1"), ("AR", s)], w=[("AR", s)])
                m.act(lambda e: e.activation(out=B["t2"][:], in_=B["cl"][:], func=AF.Exp, scale=C0), r=[K("cl")], w=[K("t2")])
                m.dve(lambda e: e.tensor_tensor(out=B["kt"][:], in0=B["kmod"][:], in1=B["t2"][:], op=ALU.mult),
                      r=[K("kmod"), K("t2")], w=[K("kt")])
                m.dve(lambda e: e.tensor_tensor(out=B["bt"][:], in0=B["kkn"][:], in1=B["a"][:], op=ALU.mult),
                      r=[K("kkn"), K("a")], w=[K("bt")])
                m.dve(lambda e: e.tensor_tensor(out=B["bt"][:], in0=B["bt"][:], in1=B["t2"][:], op=ALU.mult),
                      r=[K("bt"), K("t2")], w=[K("bt")])
                if stage == 0:
                    m.dma(yT[p * 128:(p + 1) * 128, t0:t0 + TT], B["kt"][:], r=[K("kt")])
                    continue
                LO = los[s]
                m.dma(LO["AR"][:], AR[64:128, :, :, :], r=[("AR", s)], w=[("ARlo", s)])
                m.dma(LO["bt"][:], B["bt"][64:128, :], r=[K("bt")], w=[("btlo", s)])
                m.dma(LO["kt"][:], B["kt"][64:128, :], r=[K("kt")], w=[("ktlo", s)])
                m.dma(LO["e1"][:], B["e1"][64:128, :], r=[K("e1")], w=[("e1lo", s)])
                ARk = [("AR", s), ("ARlo", s)]; btk = [K("bt"), ("btlo", s)]; ktk = [K("kt"), ("ktlo", s)]
                P2 = P[p]; Pk = ("P", p)
                ylo = LO["y"]
                for c in range(CPT):
                    cs = slice(c * L, (c + 1) * L)
                    ARh = [AR[0:64, c, :, :], LO["AR"][:, c, :, :]]
                    ARa = [AR[0:64, c, 0, :], LO["AR"][:, c, 0, :]]
                    ARr = [AR[0:64, c, 1, :], LO["AR"][:, c, 1, :]]
                    bth = [B["bt"][0:64, cs], LO["bt"][:, cs]]
                    kth = [B["kt"][0:64, cs], LO["kt"][:, cs]]
                    ps, pk = psr.get()
                    for h in range(2):
                        m.pe(lambda e, ps=ps, h=h: e.matmul(ps[0:64, h * 128:(h + 1) * 128], lhsT=bth[h], rhs=ARh[h], start=True, stop=True),
                             r=[btk[h], ARk[h]], w=[pk])
                        m.pe(lambda e, ps=ps, h=h: e.matmul(ps[0:64, 256 + h * 128:256 + (h + 1) * 128], lhsT=kth[h], rhs=ARh[h], start=True, stop=True),
                             r=[ktk[h], ARk[h]], w=[pk])
                    GM, gk = gmr.get()
                    m.dve(lambda e, ps=ps, GM=GM: e.tensor_tensor(out=GM[:, :], in0=ps[0:64, :], in1=gmask[:, :], op=ALU.mult),
                          r=[pk, "gmask"], w=[gk])
                    ps, pk = psr.get()
                    for h in range(2):
                        m.pe(lambda e, ps=ps, h=h: e.matmul(ps[0:64, h * 64:(h + 1) * 64], lhsT=ARa[h], rhs=bth[h], start=True, stop=True),
                             r=[btk[h], ARk[h]], w=[pk])
                    FE, fk = small.get()
                    m.dve(lambda e, ps=ps, FE=FE: e.tensor_tensor(out=FE[:, 0:128], in0=ps[0:64, 0:128], in1=lmask[:, :], op=ALU.mult),
                          r=[pk, "lmask"], w=[fk])
                    for h in range(2):
                        m.act(lambda e, FE=FE, GM=GM, h=h: e.copy(out=FE[:, 128 + h * 64:128 + (h + 1) * 64], in_=GM[:, h * 128:h * 128 + 64]),
                              r=[gk, fk], w=[fk])
                    ps, pk = psr.get()
                    for j, nm in enumerate(["v", "bt", "kt"]):
                        m.pe(lambda e, ps=ps, j=j, nm=nm: e.transpose(ps[0:64, j * 128:(j + 1) * 128], B[nm][:, cs], ident),
                             r=[K(nm), "cst"], w=[pk])
                    FE0, fk0 = FE, fk
                    TOK, tk = tokr.get()
                    m.act(lambda e, ps=ps, TOK=TOK: e.copy(out=TOK[:, 0:384], in_=ps[0:64, 0:384]), r=[pk], w=[tk])
                    Tt, ttk = small.get()
                    for h in range(2):
                        m.act(lambda e, Tt=Tt, h=h: e.copy(out=Tt[:, h * 64:(h + 1) * 64], in_=cst[0:64, 0, 0:64]), r=["cst"], w=[ttk])
                    for lev in range(6):
                        ps, pk = psr.get()
                        for h in range(2):
                            f = slice(h * 64, (h + 1) * 64)
                            m.pe(lambda e, ps=ps, Tt=Tt, f=f: e.matmul(ps[0:64, f], lhsT=cst[0:64, 0, 0:64], rhs=Tt[:, f], start=True, stop=False),
                                 r=["cst", ttk], w=[pk])
                            m.pe(lambda e, ps=ps, Tt=Tt, FE=FE, f=f: e.matmul(ps[0:64, f], lhsT=FE[:, f], rhs=Tt[:, f], start=False, stop=True),
                                 r=[fk, ttk], w=[pk])
                        Tn, tnk = small.get()
                        m.dve(lambda e, ps=ps, Tn=Tn: e.tensor_copy(out=Tn[:, 0:128], in_=ps[0:64, 0:128]), r=[pk], w=[tnk])
                        Tt, ttk = Tn, tnk
                        if lev < 5:
                            ps, pk = psr.get()
                            for h in range(2):
                                f = slice(h * 64, (h + 1) * 64)
                                ef = slice(128 + h * 64, 128 + (h + 1) * 64)
                                m.pe(lambda e, ps=ps, FE=FE, f=f, ef=ef: e.matmul(ps[0:64, f], lhsT=FE[:, ef], rhs=FE[:, f], start=True, stop=True),
                                     r=[fk], w=[pk])
                                if lev < 4:
                                    m.pe(lambda e, ps=ps, FE=FE, f=f, ef=ef: e.matmul(ps[0:64, ef], lhsT=FE[:, f], rhs=FE[:, ef], start=True, stop=True),
                                         r=[fk], w=[pk])
                            FEn, fnk = small.get()
                            wdt = 256 if lev < 4 else 128
                            m.act(lambda e, ps=ps, FEn=FEn, wdt=wdt: e.copy(out=FEn[:, 0:wdt], in_=ps[0:64, 0:wdt]), r=[pk], w=[fnk])
                            FE, fk = FEn, fnk
                    if stage == 1:
                        if c == 0 and p == 0 and ti == 0:
                            m.dma(yT[1024:1088, 0:512], GM[:, :], r=[gk])
                            m.dma(yT[1088:1152, 0:384], TOK[:, 0:384], r=[tk])
                            m.dma(yT[1152:1216, 0:128], Tt[:, 0:128], r=[ttk])
                            m.dma(yT[1216:1280, 0:256], FE0[:, 0:256], r=[fk0])
                        continue
                    ps, pk = psr.get()
                    for h in range(2):
                        f = slice(h * 64, (h + 1) * 64)
                        m.pe(lambda e, ps=ps, GM=GM, TOK=TOK, h=h, f=f: e.matmul(ps[0:64, f], lhsT=GM[:, 256 + h * 128:256 + h * 128 + 64], rhs=TOK[:, f],
                                                                                 start=True, stop=False), r=[gk, tk], w=[pk])
                        m.pe(lambda e, ps=ps, f=f, h=h: e.matmul(ps[0:64, f], lhsT=ARa[h], rhs=P2[:, h, :], start=False, stop=True),
                             r=[ARk[h], Pk], w=[pk])
                    X0, xk = xur.get()
                    m.dve(lambda e, ps=ps, X0=X0: e.tensor_copy(out=X0[:, 0:128], in_=ps[0:64, 0:128]), r=[pk], w=[xk])
                    ps, pk = psr.get()
                    for h in range(2):
                        f = slice(h * 64, (h + 1) * 64)
                        m.pe(lambda e, ps=ps, Tt=Tt, X0=X0, f=f: e.matmul(ps[0:64, f], lhsT=Tt[:, f], rhs=X0[:, f], start=True, stop=True),
                             r=[ttk, xk], w=[pk])
                    U, uk = xur.get()
                    m.act(lambda e, ps=ps, U=U: e.copy(out=U[:, 0:128], in_=ps[0:64, 0:128]), r=[pk], w=[uk])
                    ps, pk = psr.get()
                    for h in range(2):
                        f = slice(h * 64, (h + 1) * 64)
                        m.pe(lambda e, ps=ps, TOK=TOK, GM=GM, f=f, h=h: e.matmul(ps[0:64, f], lhsT=TOK[:, f], rhs=GM[:, 256 + h * 128 + 64:256 + (h + 1) * 128],
                                                                                 start=True, stop=False), r=[tk, gk], w=[pk])
                        m.pe(lambda e, ps=ps, U=U, GM=GM, f=f, h=h: e.matmul(ps[0:64, f], lhsT=U[:, f], rhs=GM[:, h * 128 + 64:(h + 1) * 128],
                                                                             start=False, stop=False), r=[uk, gk], w=[pk])
                        m.pe(lambda e, ps=ps, f=f, h=h: e.matmul(ps[0:64, f], lhsT=P2[:, h, :], rhs=ARr[h], start=False, stop=True),
                             r=[Pk, ARk[h]], w=[pk])
                    for h in range(2):
                        f = slice(h * 64, (h + 1) * 64)
                        o = slice(128 + h * 64, 128 + (h + 1) * 64)
                        m.pe(lambda e, ps=ps, TOK=TOK, f=f, o=o, h=h: e.matmul(ps[0:64, o], lhsT=TOK[:, 256 + h * 64:256 + (h + 1) * 64], rhs=TOK[:, f],
                                                                               start=True, stop=False), r=[tk], w=[pk])
                        m.pe(lambda e, ps=ps, TOK=TOK, U=U, f=f, o=o, h=h: e.matmul(ps[0:64, o], lhsT=TOK[:, 128 + h * 64:128 + (h + 1) * 64], rhs=U[:, f],
                                                                                    start=False, stop=True), r=[tk, uk], w=[pk])
                    m.act(lambda e, ps=ps: e.copy(out=ylo[:, :, cs], in_=ps[0:64, 0:128].rearrange("p (h t) -> p h t", h=2)), r=[pk], w=[("ylo", s)])
                    m.dve(lambda e, ps=ps: e.tensor_tensor(out=P2[:, :, :], in0=P2[:, :, :], in1=ps[0:64, 128:256].rearrange("p (h t) -> p h t", h=2), op=ALU.add),
                          r=[pk, Pk], w=[Pk])
                    gcol = c * L + L - 1
                    m.dve(lambda e, gcol=gcol: e.tensor_scalar(out=P2[:, 0, :], in0=P2[:, 0, :], scalar1=B["e1"][0:64, gcol:gcol + 1], scalar2=None, op0=ALU.mult),
                          r=[Pk, K("e1")], w=[Pk])
                    m.dve(lambda e, gcol=gcol: e.tensor_scalar(out=P2[:, 1, :], in0=P2[:, 1, :], scalar1=LO["e1"][:, gcol:gcol + 1], scalar2=None, op0=ALU.mult),
                          r=[Pk, ("e1lo", s)], w=[Pk])
                if stage == 1:
                    m.dma(yT[p * 128:(p + 1) * 128, t0:t0 + TT], B["bt"][:], r=[K("bt")])
                    continue
                m.dma(B["y"][0:64, :], ylo[:, 0, :], r=[("ylo", s)], w=[K("y")])
                m.dma(B["y"][64:128, :], ylo[:, 1, :], r=[("ylo", s)], w=[K("y")])
                if stage == 2:
                    m.dma(yT[p * 128:(p + 1) * 128, t0:t0 + TT], B["y"][:], r=[K("y")])
                    continue
                ps, pk = psr.get()
                m.pe(lambda e, ps=ps: e.matmul(ps[:, :], lhsT=bones64, rhs=B["y"][:], start=True, stop=True), r=["cst", K("y")], w=[pk])
                m.dve(lambda e, ps=ps: e.tensor_tensor(out=B["y"][:], in0=B["y"][:], in1=ps[:, :], op=ALU.subtract), r=[pk, K("y")], w=[K("y")])
                m.act(lambda e: e.activation(out=B["t1"][:], in_=B["y"][:], func=AF.Square), r=[K("y")], w=[K("t1")])
                ps, pk = psr.get()
                m.pe(lambda e, ps=ps: e.matmul(ps[:, :], lhsT=bones64, rhs=B["t1"][:], start=True, stop=True), r=["cst", K("t1")], w=[pk])
                m.dve(lambda e, ps=ps: e.tensor_scalar(out=B["t1"][:], in0=ps[:, :], scalar1=64e-5, scalar2=None, op0=ALU.add), r=[pk], w=[K("t1")])
                m.act(lambda e: e.activation(out=B["t1"][:], in_=B["t1"][:], func=AF.Ln), r=[K("t1")], w=[K("t1")])
                m.act(lambda e: e.activation(out=B["t1"][:], in_=B["t1"][:], func=AF.Exp, scale=-0.5), r=[K("t1")], w=[K("t1")])
                m.dve(lambda e: e.tensor_tensor(out=B["y"][:], in0=B["y"][:], in1=B["t1"][:], op=ALU.mult), r=[K("y"), K("t1")], w=[K("y")])
                m.act(lambda e: e.activation(out=B["y"][:], in_=B["y"][:], func=AF.Identity, scale=par(5), bias=par(6)), r=[K("y"), "pp"], w=[K("y")])
                m.dve(lambda e: e.scalar_tensor_tensor(out=B["t2"][:], in0=B["r"][:], scalar=par(4), in1=B["kmod"][:], op0=ALU.mult, op1=ALU.mult),
                      r=[K("r"), K("kmod"), "pp"], w=[K("t2")])
                ps, pk = psr.get()
                m.pe(lambda e, ps=ps: e.matmul(ps[:, :], lhsT=bones, rhs=B["t2"][:], start=True, stop=True), r=["cst", K("t2")], w=[pk])
                m.dve(lambda e, ps=ps: e.tensor_tensor(out=B["t2"][:], in0=ps[:, :], in1=B["v"][:], op=ALU.mult), r=[pk, K("v")], w=[K("t2")])
                m.dve(lambda e: e.tensor_tensor(out=B["y"][:], in0=B["y"][:], in1=B["t2"][:], op=ALU.add), r=[K("y"), K("t2")], w=[K("y")])
                m.dve(lambda e: e.tensor_tensor(out=B["t3"][:], in0=B["y"][:], in1=B["g"][:], op=ALU.mult), r=[K("y"), K("g")], w=[K("t3")])
                m.dma(yT_r[p * 128:(p + 1) * 128, t0:t0 + TT], B["t3"][:], r=[K("t3")])

    if do_moba:
        ropet = m.sb([128, 2, T], F32, "ropet"); m.dma(ropet[:], rope, w=["rope"])
        cm = m.sb([128, 2, 256], F32, "cm"); m.dma(cm[:], cmask, w=["cm"])
        idb = m.sb([128, 128], BF16, "idb"); m.dma(idb[:], identb, w=["idb"], eng="pool")
        qf = m.sb([128, T], F32, "qf"); kf = m.sb([128, T], F32, "kf")
        qb = m.sb([128, T], BF16, "qb"); kb = m.sb([128, T], BF16, "kb")
        vb = m.sb([128, 16, 128], BF16, "vb")
        raw = m.sb([128, T], F32, "mraw")
        kmean = m.sb([128, 8], F32, "kmean")
        oT = m.sb([128, T], F32, "oT")
        sS = [m.sb([128, T], F32, f"sS{i}") for i in range(1)]
        pB = [m.sb([128, T], BF16, f"pB{i}") for i in range(1)]
        pT = [m.sb([128, 16, 128], BF16, f"pT{i}") for i in range(1)]
        gt = [m.sb([128, 8], F32, f"gt{i}") for i in range(2)]
        v8 = [m.sb([128, 8], F32, f"v8{i}") for i in range(2)]
        bias = [m.sb([128, 8], F32, f"bias{i}") for i in range(2)]
        st = [m.sb([128, 4], F32, f"st{i}") for i in range(2)]
        SC = 128 ** -0.5
        for hd in range(nheads):
            hr = slice(hd * 128, (hd + 1) * 128)
            for nm, src, dstf, dstb in (("q", zq, qf, qb), ("k", zk, kf, kb)):
                m.dma(raw[:], src[hr, :], w=["mraw"])
                for tt in range(4):
                    ts_ = slice(tt * 512, (tt + 1) * 512)
                    ps, pk = psr.get()
                    m.pe(lambda e, ps=ps, ts_=ts_: e.matmul(ps[:, :], lhsT=rsw, rhs=raw[:, ts_], start=True, stop=True), r=["cst", "mraw"], w=[pk])
                    m.dve(lambda e, ps=ps, ts_=ts_, dstf=dstf: e.tensor_tensor(out=dstf[:, ts_], in0=ps[:, :], in1=ropet[:, 1, ts_], op=ALU.mult),
                          r=[pk, "rope"], w=[nm + "f"])
                m.dve(lambda e: e.tensor_tensor(out=raw[:], in0=raw[:], in1=ropet[:, 0, :], op=ALU.mult), r=["mraw", "rope"], w=["mraw"])
                m.dve(lambda e, dstf=dstf: e.tensor_tensor(out=dstf[:], in0=dstf[:], in1=raw[:], op=ALU.add), r=["mraw", nm + "f"], w=[nm + "f"])
                m.act(lambda e, dstf=dstf, dstb=dstb: e.copy(out=dstb[:], in_=dstf[:]), r=[nm + "f"], w=[nm + "b"])
            m.dve(lambda e: e.tensor_reduce(out=kmean[:], in_=kf[:].rearrange("p (n k) -> p n k", k=256), axis=AX.X, op=ALU.add),
                  r=["kf"], w=["kmean"])
            m.dve(lambda e: e.tensor_scalar(out=kmean[:], in0=kmean[:], scalar1=1.0 / 256, scalar2=None, op0=ALU.mult), r=["kmean"], w=["kmean"])
            if PADDED:
                m.dma(vb[:], zv[:, :, hr], w=["vb"], eng="pool")
            else:
                m.dma(raw[:], zvT[hr, :], w=["mraw"])
                for g4 in range(4):
                    ps, pk = psr.get()
                    for j in range(4):
                        kc = g4 * 4 + j
                        m.pe(lambda e, ps=ps, j=j, kc=kc: e.transpose(ps[:, j * 128:(j + 1) * 128], raw[:, kc * 128:(kc + 1) * 128], ident), r=["mraw", "cst"], w=[pk])
                    m.act(lambda e, ps=ps, g4=g4: e.copy(out=vb[:, g4 * 4:(g4 + 1) * 4, :], in_=ps[:, :].rearrange("p (a b) -> p a b", b=128)), r=[pk], w=["vb"])
            for qi in range(16):
                s = 0
                blk = qi // 2
                nk = (blk + 1) * 256
                qs = slice(qi * 128, (qi + 1) * 128)
                if blk > 0:
                    ps, pk = psr.get()
                    m.pe(lambda e, ps=ps, qs=qs: e.matmul(ps[:, 0:8], lhsT=qf[:, qs], rhs=kmean[:, :], start=True, stop=True), r=["qf", "kmean"], w=[pk])
                    m.dve(lambda e, s=s: e.memset(gt[s][:], NEG), w=[("gt", s)])
                    m.dve(lambda e, ps=ps, s=s, blk=blk: e.tensor_copy(out=gt[s][:, 0:blk], in_=ps[:, 0:blk]), r=[pk, ("gt", s)], w=[("gt", s)])
                    m.dve(lambda e, s=s: e.max(out=v8[s][:], in_=gt[s][:]), r=[("gt", s)], w=[("v8", s)])
                    m.dve(lambda e, s=s: e.tensor_scalar(out=bias[s][:], in0=gt[s][:], scalar1=v8[s][:, 2:3], scalar2=NEG, op0=ALU.is_lt, op1=ALU.mult),
                          r=[("gt", s), ("v8", s)], w=[("bias", s)])
                for kg in range((nk + 511) // 512):
                    w_ = min(512, nk - kg * 512)
                    ps, pk = psr.get()
                    m.pe(lambda e, ps=ps, qs=qs, kg=kg, w_=w_: e.matmul(ps[:, 0:w_], lhsT=qb[:, qs], rhs=kb[:, kg * 512:kg * 512 + w_], start=True, stop=True),
                         r=["qb", "kb"], w=[pk])
                    for j in range(w_ // 256):
                        n = kg * 2 + j
                        cols = slice(n * 256, (n + 1) * 256)
                        if n < blk:
                            m.act(lambda e, ps=ps, s=s, j=j, n=n, cols=cols: e.activation(out=sS[s][:, cols], in_=ps[:, j * 256:(j + 1) * 256], func=AF.Identity,
                                                                                         scale=SC, bias=bias[s][:, n:n + 1]),
                                  r=[pk, ("bias", s)], w=[("sS", s)])
                        else:
                            m.dve(lambda e, ps=ps, s=s, j=j, cols=cols, qi=qi: e.scalar_tensor_tensor(out=sS[s][:, cols], in0=ps[:, j * 256:(j + 1) * 256], scalar=SC,
                                                                                                      in1=cm[:, qi % 2, :], op0=ALU.mult, op1=ALU.add),
                                  r=[pk, "cm"], w=[("sS", s)])
                m.dve(lambda e, s=s, nk=nk: e.tensor_reduce(out=st[s][:, 0:1], in_=sS[s][:, 0:nk], axis=AX.X, op=ALU.max), r=[("sS", s)], w=[("st", s)])
                m.dve(lambda e, s=s: e.tensor_scalar(out=st[s][:, 1:2], in0=st[s][:, 0:1], scalar1=-1.0, scalar2=None, op0=ALU.mult), r=[("st", s)], w=[("st", s)])
                m.act(lambda e, s=s, nk=nk: e.activation(out=sS[s][:, 0:nk], in_=sS[s][:, 0:nk], func=AF.Exp, bias=st[s][:, 1:2], accum_out=st[s][:, 2:3]),
                      r=[("sS", s), ("st", s)], w=[("sS", s), ("st", s)])
                m.dve(lambda e, s=s: e.reciprocal(out=st[s][:, 3:4], in_=st[s][:, 2:3]), r=[("st", s)], w=[("st", s)])
                m.dve(lambda e, s=s, nk=nk: e.tensor_scalar(out=pB[s][:, 0:nk], in0=sS[s][:, 0:nk], scalar1=st[s][:, 3:4], scalar2=None, op0=ALU.mult),
                      r=[("sS", s), ("st", s)], w=[("pB", s)])
                nkc = nk // 128
                for g0 in range(0, nkc, 8):
                    gn = min(8, nkc - g0)
                    for j in range(gn):
                        m.pe(lambda e, s=s, g0=g0, j=j: e.transpose(psb16[:, j * 128:(j + 1) * 128], pB[s][:, (g0 + j) * 128:(g0 + j + 1) * 128], idb[:]),
                             r=[("pB", s), "idb"], w=["psb16"])
                    eng = m.act if (g0 // 8) % 2 == 0 else m.dve
                    if eng is m.act:
                        m.act(lambda e, s=s, g0=g0, gn=gn: e.copy(out=pT[s][:, g0:g0 + gn, :], in_=psb16[:, 0:gn * 128].rearrange("p (a b) -> p a b", b=128)),
                              r=["psb16"], w=[("pT", s)])
                    else:
                        m.dve(lambda e, s=s, g0=g0, gn=gn: e.tensor_copy(out=pT[s][:, g0:g0 + gn, :], in_=psb16[:, 0:gn * 128].rearrange("p (a b) -> p a b", b=128)),
                              r=["psb16"], w=[("pT", s)])
                ps, pk = psr.get()
                for kc in range(nkc):
                    m.pe(lambda e, ps=ps, s=s, kc=kc, nkc=nkc: e.matmul(ps[:, 0:128], lhsT=vb[:, kc, :], rhs=pT[s][:, kc, :], start=(kc == 0), stop=(kc == nkc - 1)),
                         r=["vb", ("pT", s)], w=[pk])
                m.act(lambda e, ps=ps, qs=qs: e.copy(out=oT[:, qs], in_=ps[:, 0:128]), r=[pk], w=["oT"])
            m.dma(yT_m[hd * 128:(hd + 1) * 128, :], oT[:], r=["oT"])
    m.build()
    return m


def pb_consts():
    c = np.zeros((128, 6, 128), np.float32)
    c[:, 0, :] = np.eye(128)
    bo = np.zeros((128, 128), np.float32); bo[:64, :64] = 1; bo[64:, 64:] = 1
    c[:, 1, :] = bo; c[:, 2, :] = bo / 64
    R = np.zeros((128, 128), np.float32)
    for mm in range(64):
        R[mm + 64, mm] = 1; R[mm, mm + 64] = 1
    c[:, 3, :] = R
    s_ = np.arange(64)[:, None]; t_ = np.arange(64)[None, :]
    c[:64, 4, 0:64] = (s_ < t_); c[:64, 4, 64:128] = (s_ <= t_)
    c[:64, 5, 0:64] = (s_ > t_)
    return c


def rope_tables():
    inv = np.power(10000.0, -np.arange(0, 128, 2, dtype=np.float32) / 128).astype(np.float32)
    ang = np.arange(T, dtype=np.float32)[:, None] * inv[None, :]
    cos = np.cos(ang).T.astype(np.float32); sin = np.sin(ang).T.astype(np.float32)
    r = np.zeros((128, 2, T), np.float32)
    r[:64, 0] = cos; r[64:, 0] = cos
    r[:64, 1] = -sin; r[64:, 1] = sin
    return r


def causal_masks():
    cmk = np.zeros((128, 2, 256), np.float32)
    q = np.arange(128)[:, None]; k = np.arange(256)[None, :]
    cmk[:, 0, :] = np.where(k <= q, 0, NEG)
    cmk[:, 1, :] = np.where(k <= q + 128, 0, NEG)
    return cmk


D = 4096; KC = 32; TT = 512
ALPHA = float(8 ** 0.25)
LN_EPS = 1e-5


def load_mods(m, modb, tab, names=("mod",)):
    mb = m.sb([128, 6, KC], F32, "modb_sb"); tb = m.sb([128, 6, KC], F32, "modt_sb")
    m.dma(mb[:], modb, w=["modb"]); m.dma(tb[:], tab, w=["modt"])
    m.dve(lambda e: e.tensor_tensor(out=mb[:], in0=mb[:], in1=tb[:], op=ALU.add), r=["modb", "modt"], w=["modb"])
    for i in (1, 4):
        m.dve(lambda e, i=i: e.tensor_scalar_add(out=mb[:, i, :], in0=mb[:, i, :], scalar1=1.0), r=["modb"], w=["modb"])
    return mb


def build_p0():
    m = MK()
    cT = m.dram("cT", [128, KC, 4], F32, "ExternalInput")
    W = m.dram("W", [D, 3072], F32, "ExternalInput")
    bvec = m.dram("b", [1, 3072], F32, "ExternalInput")
    out = m.dram("out", [4, 3072], F32, "ExternalOutput")
    Wv = W.rearrange("(kc p) n -> p kc n", p=128)
    ct = m.sb([128, KC, 4], F32, "ct"); m.dma(ct[:], cT, w=["ct"])
    m.act(lambda e: e.activation(out=ct[:], in_=ct[:], func=AF.Silu), r=["ct"], w=["ct"])
    bt = m.sb([1, 3072], F32, "bt"); m.dma(bt[:], bvec, w=["bt"])
    ones = m.sb([1, 4], F32, "ones"); m.dve(lambda e: e.memset(ones[:], 1.0), w=["ones"])
    wr = Ring([m.sb([128, 8, 512], F32, f"w{i}") for i in range(4)], "w")
    pss = Ring([m.ps([128, 512], F32, f"ps{i}") for i in range(2)], "ps")
    ob = m.sb([4, 3072], F32, "ob")
    for g in range(6):
        n0 = g * 512
        ps, pk = pss.get()
        for q in range(4):
            wt, wk = wr.get()
            m.dma(wt[:], Wv[:, q * 8:(q + 1) * 8, n0:n0 + 512], w=[wk])
            for j in range(8):
                kc = q * 8 + j
                m.pe(lambda e, ps=ps, wt=wt, kc=kc, j=j: e.matmul(ps[0:4, :], lhsT=ct[:, kc, :], rhs=wt[:, j, :], start=(kc == 0), stop=False),
                     r=["ct", wk], w=[pk])
        m.pe(lambda e, ps=ps: e.matmul(ps[0:4, :], lhsT=ones[:, :], rhs=bt[:, n0:n0 + 512], start=False, stop=True), r=["ones", "bt"], w=[pk])
        m.dve(lambda e, ps=ps: e.tensor_copy(out=ob[:, n0:n0 + 512], in_=ps[0:4, :]), r=[pk], w=["ob"])
    m.dma(out, ob[:], r=["ob"])
    m.build()
    return m


N_IN = 13024


def build_pa(nc=None, io=None):
    m = MK(nc=nc)
    NT = 1024
    NCH = (N_IN + 127) // 128
    if io is None:
        xT = m.dram("xT", [D, NT], F32, "ExternalInput")
        modb = m.dram("modb", [128, 6, KC], F32, "ExternalInput")
        tab = m.dram("tab", [128, 6, KC], F32, "ExternalInput")
        Wt = m.dram("Wt", [NCH, 128, KC, 128], F32, "ExternalInput")
        zT = m.dram("zT", [NCH * 128, NT], F32, "ExternalOutput")
    else:
        xT = io["xT"]; modb = io["modb"]; tab = io["tab"]; Wt = io["Wt"]; zT = io["zT"]
    mod = load_mods(m, modb, tab)
    hT = m.sb([128, KC, NT], BF16, "hT")
    xr = Ring([m.sb([128, NT], F32, f"xs{i}") for i in range(3)], "xs")
    for kc in range(KC):
        xs, xk = xr.get()
        m.dma(xs[:], xT[kc * 128:(kc + 1) * 128, :], w=[xk])
        m.act(lambda e, xs=xs, kc=kc: e.activation(out=hT[:, kc, :], in_=xs[:], func=AF.Identity, scale=mod[:, 1, kc:kc + 1], bias=mod[:, 0, kc:kc + 1]),
              r=[xk, "modb"], w=[("hT", kc)])
    wr = Ring([m.sb([128, KC, 128], BF16, f"w{i}") for i in range(4)], "w")
    pss = Ring([m.ps([128, 512], F32, f"ps{i}") for i in range(6)], "ps")
    obr = Ring([m.sb([128, NT], F32, f"ob{i}") for i in range(3)], "ob")
    for nch in range(NCH):
        wt, wk = wr.get()
        m.dma(wt[:], Wt[nch], w=[wk], eng="pool")
        ob, ok = obr.get()
        for tt in range(2):
            ps, pk = pss.get()
            for kc in range(KC):
                m.pe(lambda e, ps=ps, wt=wt, kc=kc, tt=tt: e.matmul(ps[:, :], lhsT=wt[:, kc, :], rhs=hT[:, kc, tt * 512:(tt + 1) * 512], start=(kc == 0), stop=(kc == KC - 1)),
                     r=[wk, ("hT", kc)], w=[pk])
            if tt == 0:
                m.dve(lambda e, ps=ps, ob=ob: e.tensor_copy(out=ob[:, 0:512], in_=ps[:, :]), r=[pk], w=[ok])
            else:
                m.act(lambda e, ps=ps, ob=ob: e.copy(out=ob[:, 512:1024], in_=ps[:, :]), r=[pk, ok], w=[ok])
        m.dma(zT[nch * 128:(nch + 1) * 128, :], ob[:], r=[ok])
    m.build()
    return m


def build_pt(kind, nc=None, io=None):
    m = MK(nc=nc)
    even = kind == "even"
    NT = 1024
    HALO = 16
    tok0 = 0 if io is None else io.get("tok0", 0)
    if even:
        NFC = 86
        parts = [(0, 22), (22, 22), (44, 21), (65, 21)]
        plist = [(0, f0, n) for (f0, n) in parts]
    else:
        NFC = 14
        plist = [(e, 0, NFC) for e in range(8)]
    if io is None:
        if even:
            xT = m.dram("xT", [D, NT], F32, "ExternalInput")
            yT = m.dram("yT", [D, NT], F32, "ExternalInput")
            Wo = m.dram("Wo", [32, 128, KC, 128], F32, "ExternalInput")
            Wg = [m.dram("Wg", [NFC, 128, KC, 128], F32, "ExternalInput")]
            Wu = [m.dram("Wu", [NFC, 128, KC, 128], F32, "ExternalInput")]
            Wd = [m.dram("Wd", [32, 128, NFC, 128], F32, "ExternalInput")]
        else:
            xT = m.dram("xT", [D, NT + HALO], F32, "ExternalInput")
            hv = m.dram("hv", [128, 1], F32, "ExternalInput")
            icnt = m.dram("icnt", [128, 4, NT], F32, "ExternalInput")
            Wp = m.dram("Wp", [4, 8, 128, 8, 128], F32, "ExternalInput")
            pscale = m.dram("pscale", [128, KC], F32, "ExternalInput")
            Wr = m.dram("Wr", [128, KC, 8], F32, "ExternalInput")
            rb = m.dram("rb", [1, 8], F32, "ExternalInput")
            Wg = [m.dram(f"Wg{e}", [NFC, 128, KC, 128], F32, "ExternalInput") for e in range(8)]
            Wu = [m.dram(f"Wu{e}", [NFC, 128, KC, 128], F32, "ExternalInput") for e in range(8)]
            Wd = [m.dram(f"Wd{e}", [32, 128, NFC, 128], F32, "ExternalInput") for e in range(8)]
        modb = m.dram("modb", [128, 6, KC], F32, "ExternalInput")
        tab = m.dram("tab", [128, 6, KC], F32, "ExternalInput")
        lng = m.dram("lng", [128, 2, KC], F32, "ExternalInput")
        lnb = m.dram("lnb", [128, 2, KC], F32, "ExternalInput")
        cdram = m.dram("cst", [128, 2, 128], F32, "ExternalInput")
        outT = m.dram("outT", [D, NT], F32, "ExternalOutput")
    else:
        xT = io["xT"]; modb = io["modb"]; tab = io["tab"]; lng = io["lng"]; lnb = io["lnb"]; cdram = io["cst"]; outT = io["outT"]
        Wg = io["Wg"]; Wu = io["Wu"]; Wd = io["Wd"]
        if even:
            yT = io["yT"]; Wo = io["Wo"]
        else:
            icnt = io["icnt"]; Wp = io["Wp"]; pscale = io["pscale"]; Wr = io["Wr"]; rb = io["rb"]; xfull = io["xfull"]; hv = None

    mod = load_mods(m, modb, tab)
    lg = m.sb([128, 2, KC], F32, "lg"); m.dma(lg[:], lng, w=["lg"])
    lb = m.sb([128, 2, KC], F32, "lb"); m.dma(lb[:], lnb, w=["lb"])
    cst = m.sb([128, 2, 128], F32, "cst_sb"); m.dma(cst[:], cdram, w=["cst"])
    onesD = cst[:, 0, :]; ident = cst[:, 1, :]
    ones = m.sb([128, 128], F32, "ones"); m.dve(lambda e: e.memset(ones[:], 1.0), w=["ones"])

    X = m.sb([128, KC, TT], F32, "X")
    hT = m.sb([128, KC, TT], BF16, "hT")
    wr = Ring([m.sb([128, KC, 128], BF16, f"w{i}") for i in range(4 if even else 3)], "w")
    wdr = Ring([m.sb([128, 22 if even else 14, 128], BF16, f"wd{i}") for i in range(3)], "wd")
    pss = Ring([m.ps([128, 512], F32, f"ps{i}") for i in range(8)], "ps")
    tr = Ring([m.sb([128, TT], F32, f"tmp{i}") for i in range(4)], "tmp")
    act = m.sb([128, 22 if even else 14, TT], BF16, "act")
    stat = m.sb([128, 4, TT], F32, "stat")

    if not even:
        hvt = m.sb([128, 1], F32, "hvt")
        if hv is not None:
            m.dma(hvt[:], hv, w=["hvt"])
        ps_t = m.sb([128, KC], F32, "pst"); m.dma(ps_t[:], pscale, w=["pst"])
        m.dve(lambda e: e.tensor_tensor(out=ps_t[:], in0=ps_t[:], in1=mod[:, 2, :], op=ALU.mult), r=["pst", "modb"], w=["pst"])
        wrt = m.sb([128, KC, 8], F32, "wrt"); m.dma(wrt[:], Wr, w=["wrt"])
        wr1 = m.sb([128, KC, 8], F32, "wr1"); wr2 = m.sb([128, KC, 8], F32, "wr2")
        for kc in range(KC):
            m.dve(lambda e, kc=kc: e.tensor_scalar(out=wr1[:, kc, :], in0=wrt[:, kc, :], scalar1=mod[:, 4, kc:kc + 1], scalar2=None, op0=ALU.mult),
                  r=["wrt", "modb"], w=["wr1"])
            m.dve(lambda e, kc=kc: e.tensor_scalar(out=wr2[:, kc, :], in0=wrt[:, kc, :], scalar1=mod[:, 3, kc:kc + 1], scalar2=None, op0=ALU.mult),
                  r=["wrt", "modb"], w=["wr2"])
        rbt = m.sb([1, 8], F32, "rbt"); m.dma(rbt[:], rb, w=["rbt"])
        bc = m.sb([128, 8, TT], F32, "bc")
        sm = {n: m.sb([128, 8], F32, "sm_" + n) for n in ("lg", "v8", "c1", "c2", "g")}
        dg = Ring([m.sb([128, 128], F32, f"dg{i}") for i in range(2)], "dg")
        hp = Ring([m.sb([128, TT + HALO], F32, f"hp{i}") for i in range(2)], "hp")
        hq = Ring([m.sb([128, TT + HALO], F32, f"hq{i}") for i in range(2)], "hq")
        hs_ = Ring([m.sb([128, TT + HALO], F32, f"hs{i}") for i in range(3)], "hs")
        ic = m.sb([128, 4, TT], F32, "ic")

    def layer_norm(li, gate_next):
        ps1, k1 = pss.get(); ps2, k2 = pss.get()
        for kc in range(KC):
            m.pe(lambda e, kc=kc: e.matmul(ps1[:, :], lhsT=onesD, rhs=X[:, kc, :], start=(kc == 0), stop=(kc == KC - 1)), r=["cst", ("X", kc)], w=[k1])
        for kc in range(KC):
            t, tk = tr.get()
            m.act(lambda e, t=t, kc=kc: e.activation(out=t[:], in_=X[:, kc, :], func=AF.Square), r=[("X", kc)], w=[tk])
            m.pe(lambda e, t=t, kc=kc: e.matmul(ps2[:, :], lhsT=onesD, rhs=t[:], start=(kc == 0), stop=(kc == KC - 1)), r=["cst", tk], w=[k2])
        m.dve(lambda e: e.tensor_copy(out=stat[:, 0, :], in_=ps1[:, :]), r=[k1], w=["stat"])
        m.dve(lambda e: e.tensor_tensor(out=stat[:, 1, :], in0=stat[:, 0, :], in1=stat[:, 0, :], op=ALU.mult), r=["stat"], w=["stat"])
        m.dve(lambda e: e.tensor_tensor(out=stat[:, 1, :], in0=ps2[:, :], in1=stat[:, 1, :], op=ALU.subtract), r=[k2, "stat"], w=["stat"])
        m.dve(lambda e: e.tensor_scalar(out=stat[:, 1, :], in0=stat[:, 1, :], scalar1=LN_EPS, scalar2=None, op0=ALU.add), r=["stat"], w=["stat"])
        m.act(lambda e: e.activation(out=stat[:, 1, :], in_=stat[:, 1, :], func=AF.Ln), r=["stat"], w=["stat"])
        m.act(lambda e: e.activation(out=stat[:, 2, :], in_=stat[:, 1, :], func=AF.Exp, scale=-0.5), r=["stat"], w=["stat"])
        m.dve(lambda e: e.scalar_tensor_tensor(out=stat[:, 3, :], in0=stat[:, 0, :], scalar=-1.0, in1=stat[:, 2, :], op0=ALU.mult, op1=ALU.mult),
              r=["stat"], w=["stat"])
        for kc in range(KC):
            m.dve(lambda e, kc=kc: e.tensor_tensor(out=X[:, kc, :], in0=X[:, kc, :], in1=stat[:, 2, :], op=ALU.mult), r=[("X", kc), "stat"], w=[("X", kc)])
            m.dve(lambda e, kc=kc: e.tensor_tensor(out=X[:, kc, :], in0=X[:, kc, :], in1=stat[:, 3, :], op=ALU.add), r=[("X", kc), "stat"], w=[("X", kc)])
            m.act(lambda e, kc=kc: e.activation(out=X[:, kc, :], in_=X[:, kc, :], func=AF.Identity, scale=lg[:, li, kc:kc + 1], bias=lb[:, li, kc:kc + 1]),
                  r=[("X", kc), "lg", "lb"], w=[("X", kc)])
            if gate_next:
                m.act(lambda e, kc=kc: e.activation(out=hT[:, kc, :], in_=X[:, kc, :], func=AF.Identity, scale=mod[:, 4, kc:kc + 1], bias=mod[:, 3, kc:kc + 1]),
                      r=[("X", kc), "modb"], w=[("hT", kc)])

    for ti in range(2):
        c0 = ti * TT
        if even:
            for kc in range(KC):
                m.dma(hT[:, kc, :], yT[kc * 128:(kc + 1) * 128, c0:c0 + TT], w=[("hT", kc)], eng="pool")
                m.dma(X[:, kc, :], xT[kc * 128:(kc + 1) * 128, c0:c0 + TT], w=[("X", kc)])
            for dc in range(KC):
                wt, wk = wr.get()
                m.dma(wt[:], Wo[dc], w=[wk], eng="pool")
                ps, pk = pss.get()
                for kc in range(KC):
                    m.pe(lambda e, ps=ps, wt=wt, kc=kc: e.matmul(ps[:, :], lhsT=wt[:, kc, :], rhs=hT[:, kc, :], start=(kc == 0), stop=(kc == KC - 1)),
                         r=[wk, ("hT", kc)], w=[pk])
                m.act(lambda e, dc=dc: e.mul(out=X[:, dc, :], in_=X[:, dc, :], mul=ALPHA), r=[("X", dc)], w=[("X", dc)])
                m.dve(lambda e, ps=ps, dc=dc: e.scalar_tensor_tensor(out=X[:, dc, :], in0=ps[:, :], scalar=mod[:, 2, dc:dc + 1], in1=X[:, dc, :], op0=ALU.mult, op1=ALU.add),
                      r=[pk, ("X", dc), "modb"], w=[("X", dc)])
        else:
            m.dma(ic[:], icnt[:, :, c0:c0 + TT], w=["ic"])
            for kc in range(KC):
                g = kc // 8
                xh, xk = hp.get()
                g0 = tok0 + c0
                if io is None:
                    m.dma(xh[:], xT[kc * 128:(kc + 1) * 128, c0:c0 + TT + HALO], w=[xk])
                elif g0 == 0:
                    m.dve(lambda e, xh=xh: e.memset(xh[:, 0:HALO], 0.0), w=[xk])
                    m.dma(xh[:, HALO:], xfull[kc * 128:(kc + 1) * 128, 0:TT], r=[xk], w=[xk])
                else:
                    m.dma(xh[:], xfull[kc * 128:(kc + 1) * 128, g0 - HALO:g0 + TT], w=[xk])
                m.dve(lambda e, xh=xh, kc=kc: e.tensor_copy(out=X[:, kc, :], in_=xh[:, HALO:]), r=[xk], w=[("X", kc)])
                h, hk = hq.get()
                m.act(lambda e, xh=xh, h=h, kc=kc: e.activation(out=h[:], in_=xh[:], func=AF.Identity, scale=mod[:, 1, kc:kc + 1], bias=mod[:, 0, kc:kc + 1]),
                      r=[xk, "modb"], w=[hk])
                if io is None and ti == 0:
                    m.dve(lambda e, h=h: e.tensor_scalar(out=h[:, 0:HALO], in0=h[:, 0:HALO], scalar1=hvt[:, 0:1], scalar2=None, op0=ALU.mult),
                          r=[hk, "hvt"], w=[hk])
                elif io is not None and g0 == 0:
                    m.dve(lambda e, h=h: e.memset(h[:, 0:HALO], 0.0), r=[hk], w=[hk])
                s, sk = h, hk
                for st in range(g + 1):
                    step = 1 << st
                    lo = 2 * step - 1
                    s2, s2k = hs_.get()
                    m.dve(lambda e, s=s, s2=s2, step=step, lo=lo: e.tensor_tensor(out=s2[:, lo:], in0=s[:, lo:], in1=s[:, lo - step:TT + HALO - step], op=ALU.add),
                          r=[sk], w=[s2k])
                    s, sk = s2, s2k
                t, tk = tr.get()
                m.dve(lambda e, s=s, t=t, g=g: e.tensor_tensor(out=t[:], in0=s[:, HALO:], in1=ic[:, g, :], op=ALU.mult), r=[sk, "ic"], w=[tk])
                m.dve(lambda e, t=t, h=h, kc=kc: e.tensor_tensor(out=hT[:, kc, :], in0=t[:], in1=h[:, HALO:], op=ALU.subtract), r=[tk, hk], w=[("hT", kc)])
            for dc in range(KC):
                g, ec = dc // 8, dc % 8
                wt, wk = wr.get()
                m.dma(wt[:, 0:8, :], Wp[g, ec], w=[wk], eng="pool")
                ps, pk = pss.get()
                for cc in range(8):
                    m.pe(lambda e, ps=ps, wt=wt, cc=cc, g=g: e.matmul(ps[:, :], lhsT=wt[:, cc, :], rhs=hT[:, g * 8 + cc, :], start=(cc == 0), stop=(cc == 7)),
                         r=[wk, ("hT", g * 8 + cc)], w=[pk])
                m.act(lambda e, dc=dc: e.mul(out=X[:, dc, :], in_=X[:, dc, :], mul=ALPHA), r=[("X", dc)], w=[("X", dc)])
                m.dve(lambda e, ps=ps, dc=dc: e.scalar_tensor_tensor(out=X[:, dc, :], in0=ps[:, :], scalar=ps_t[:, dc:dc + 1], in1=X[:, dc, :], op0=ALU.mult, op1=ALU.add),
                      r=[pk, ("X", dc), "pst"], w=[("X", dc)])
        layer_norm(0, True)
        if not even:
            for tc_ in range(4):
                tsl = slice(tc_ * 128, (tc_ + 1) * 128)
                ps, pk = pss.get()
                for kc in range(KC):
                    m.pe(lambda e, ps=ps, kc=kc, tsl=tsl: e.matmul(ps[:, 0:8], lhsT=X[:, kc, tsl], rhs=wr1[:, kc, :], start=(kc == 0), stop=False),
                         r=[("X", kc), "wr1"], w=[pk])
                for kc in range(KC):
                    m.pe(lambda e, ps=ps, kc=kc: e.matmul(ps[:, 0:8], lhsT=ones[:, :], rhs=wr2[:, kc, :], start=False, stop=False), r=["ones", "wr2"], w=[pk])
                m.pe(lambda e, ps=ps: e.matmul(ps[:, 0:8], lhsT=ones[0:1, :], rhs=rbt[:, :], start=False, stop=True), r=["ones", "rbt"], w=[pk])
                L_ = sm["lg"]; V8 = sm["v8"]; C1 = sm["c1"]; C2 = sm["c2"]; G = sm["g"]
                m.dve(lambda e, ps=ps: e.tensor_copy(out=L_[:], in_=ps[:, 0:8]), r=[pk], w=["sm_lg"])
                m.dve(lambda e: e.max(out=V8[:], in_=L_[:]), r=["sm_lg"], w=["sm_v8"])
                m.dve(lambda e: e.tensor_tensor(out=G[:, 0:1], in0=V8[:, 0:1], in1=V8[:, 1:2], op=ALU.subtract), r=["sm_v8"], w=["sm_g"])
                m.act(lambda e: e.activation(out=G[:, 1:2], in_=G[:, 0:1], func=AF.Sigmoid), r=["sm_g"], w=["sm_g"])
                m.act(lambda e: e.activation(out=G[:, 2:3], in_=G[:, 0:1], func=AF.Sigmoid, scale=-1.0), r=["sm_g"], w=["sm_g"])
                m.dve(lambda e: e.tensor_scalar(out=C1[:], in0=L_[:], scalar1=V8[:, 0:1], scalar2=G[:, 1:2], op0=ALU.is_equal, op1=ALU.mult),
                      r=["sm_lg", "sm_v8", "sm_g"], w=["sm_c1"])
                m.dve(lambda e: e.tensor_scalar(out=C2[:], in0=L_[:], scalar1=V8[:, 1:2], scalar2=G[:, 2:3], op0=ALU.is_equal, op1=ALU.mult),
                      r=["sm_lg", "sm_v8", "sm_g"], w=["sm_c2"])
                m.dve(lambda e: e.tensor_tensor(out=C1[:], in0=C1[:], in1=C2[:], op=ALU.add), r=["sm_c1", "sm_c2"], w=["sm_c1"])
                for ex in range(8):
                    dt_, dk = dg.get()
                    m.dve(lambda e, dt_=dt_, ex=ex: e.tensor_scalar(out=dt_[:], in0=ident, scalar1=C1[:, ex:ex + 1], scalar2=None, op0=ALU.mult),
                          r=["cst", "sm_c1"], w=[dk])
                    ps2, pk2 = pss.get()
                    m.pe(lambda e, ps2=ps2, dt_=dt_: e.matmul(ps2[:, 0:128], lhsT=ones[:, :], rhs=dt_[:], start=True, stop=True), r=["ones", dk], w=[pk2])
                    m.act(lambda e, ps2=ps2, ex=ex, tsl=tsl: e.copy(out=bc[:, ex, tsl], in_=ps2[:, 0:128]), r=[pk2], w=[("bc", ex)])
        for kc in range(KC):
            m.act(lambda e, kc=kc: e.mul(out=X[:, kc, :], in_=X[:, kc, :], mul=ALPHA), r=[("X", kc)], w=[("X", kc)])
        for (ex, f0, nf) in plist:
            for fi in range(nf):
                fc = f0 + fi
                wg, wgk = wr.get()
                m.dma(wg[:], Wg[ex][fc], w=[wgk], eng="pool")
                wu, wuk = wr.get()
                m.dma(wu[:], Wu[ex][fc], w=[wuk], eng="pool")
                psg, pgk = pss.get(); psu, puk = pss.get()
                for kc in range(KC):
                    m.pe(lambda e, psg=psg, wg=wg, kc=kc: e.matmul(psg[:, :], lhsT=wg[:, kc, :], rhs=hT[:, kc, :], start=(kc == 0), stop=(kc == KC - 1)),
                         r=[wgk, ("hT", kc)], w=[pgk])
                for kc in range(KC):
                    m.pe(lambda e, psu=psu, wu=wu, kc=kc: e.matmul(psu[:, :], lhsT=wu[:, kc, :], rhs=hT[:, kc, :], start=(kc == 0), stop=(kc == KC - 1)),
                         r=[wuk, ("hT", kc)], w=[puk])
                t, tk = tr.get()
                m.act(lambda e, t=t, psg=psg: e.activation(out=t[:], in_=psg[:, :], func=AF.Silu), r=[pgk], w=[tk])
                if even:
                    m.dve(lambda e, t=t, psu=psu, fi=fi: e.tensor_tensor(out=act[:, fi, :], in0=t[:], in1=psu[:, :], op=ALU.mult), r=[tk, puk], w=[("act", fi)])
                else:
                    m.dve(lambda e, t=t, psu=psu: e.tensor_tensor(out=t[:], in0=t[:], in1=psu[:, :], op=ALU.mult), r=[tk, puk], w=[tk])
                    m.dve(lambda e, t=t, fi=fi, ex=ex: e.tensor_tensor(out=act[:, fi, :], in0=t[:], in1=bc[:, ex, :], op=ALU.mult), r=[tk, ("bc", ex)], w=[("act", fi)])
            for dc in range(KC):
                wd, wdk = wdr.get()
                m.dma(wd[:, 0:nf, :], Wd[ex][dc, :, f0:f0 + nf, :], w=[wdk], eng="pool")
                ps, pk = pss.get()
                for fi in range(nf):
                    m.pe(lambda e, ps=ps, wd=wd, fi=fi, nf=nf: e.matmul(ps[:, :], lhsT=wd[:, fi, :], rhs=act[:, fi, :], start=(fi == 0), stop=(fi == nf - 1)),
                         r=[wdk, ("act", fi)], w=[pk])
                m.dve(lambda e, ps=ps, dc=dc: e.scalar_tensor_tensor(out=X[:, dc, :], in0=ps[:, :], scalar=mod[:, 5, dc:dc + 1], in1=X[:, dc, :], op0=ALU.mult, op1=ALU.add),
                      r=[pk, ("X", dc), "modb"], w=[("X", dc)])
        layer_norm(1, False)
        for kc in range(KC):
            m.dma(outT[kc * 128:(kc + 1) * 128, c0:c0 + TT], X[:, kc, :], r=[("X", kc)])
    m.build()
    return m


def build_p0f(nc, io):
    m = MK(nc=nc)
    cT = io["cT"]; W = io["ada_w"]; bT = io["bT"]; oh = io["oh"]; out = io["modbase"]
    Wv = W.rearrange("(kc p) n -> p kc n", p=128)
    ct = m.sb([128, KC, 4], F32, "ct"); m.dma(ct[:], cT, w=["ct"])
    m.act(lambda e: e.activation(out=ct[:], in_=ct[:], func=AF.Silu), r=["ct"], w=["ct"])
    bt = m.sb([128, 192], F32, "bt"); m.dma(bt[:], bT, w=["bt"])
    oht = m.sb([128, 4], F32, "oht"); m.dma(oht[:], oh, w=["oht"])
    wr = Ring([m.sb([128, 8, 512], F32, f"w{i}") for i in range(8)], "w")
    pss = Ring([m.ps([128, 512], F32, f"ps{i}") for i in range(4)], "ps")
    baseT = m.sb([128, 192, 4], F32, "baseT")
    for g in range(48):
        n0 = g * 512
        tiles = []
        for q in range(4):
            wt, wk = wr.get()
            m.dma(wt[:], Wv[:, q * 8:(q + 1) * 8, n0:n0 + 512], w=[wk])
            tiles.append((wt, wk))
        ps, pk = pss.get()
        for j in range(4):
            for kc in range(KC):
                wt, wk = tiles[kc // 8]
                m.pe(lambda e, ps=ps, wt=wt, kc=kc, j=j: e.matmul(ps[:, j * 4:(j + 1) * 4], lhsT=wt[:, kc % 8, j * 128:(j + 1) * 128], rhs=ct[:, kc, :],
                                                                 start=(kc == 0), stop=(kc == KC - 1)), r=["ct", wk], w=[pk])
        m.dve(lambda e, ps=ps, g=g: e.tensor_copy(out=baseT[:, g * 4:(g + 1) * 4, :], in_=ps[:, 0:16].rearrange("p (a b) -> p a b", b=4)), r=[pk], w=["baseT"])
    mb = m.sb([128, 192], F32, "mb")
    m.dve(lambda e: e.tensor_scalar(out=mb[:], in0=baseT[:, :, 0], scalar1=oht[:, 0:1], scalar2=None, op0=ALU.mult), r=["baseT", "oht"], w=["mb"])
    for b in range(1, 4):
        m.dve(lambda e, b=b: e.scalar_tensor_tensor(out=mb[:], in0=baseT[:, :, b], scalar=oht[:, b:b + 1], in1=mb[:], op0=ALU.mult, op1=ALU.add),
              r=["baseT", "oht", "mb"], w=["mb"])
    m.dve(lambda e: e.tensor_tensor(out=mb[:], in0=mb[:], in1=bt[:], op=ALU.add), r=["mb", "bt"], w=["mb"])
    m.dma(out, mb[:], r=["mb"])
    m.build()
    return m


SEQ = 2048


def build_fused():
    nc = bass.Bass("TRN2", target_bir_lowering=False)

    def dr(name, shape, kind="ExternalInput"):
        return nc.dram_tensor(name, list(shape), F32, kind=kind).ap()

    xin = dr("xT", [D, SEQ]); out = dr("outT", [D, SEQ], "ExternalOutput")
    cT = dr("cT", [128, KC, 4]); ada_w = dr("ada_w", [D, 6 * D]); bT = dr("bT", [128, 192]); oh = dr("oh", [128, 4])
    tabs = dr("tabs", [4, 128, 6, KC]); lngs = dr("lngs", [4, 128, 2, KC]); lnbs = dr("lnbs", [4, 128, 2, KC])
    cstT = dr("cstT", [128, 2, 128]); pbc = dr("pbc", [128, 6, 128]); rope = dr("rope", [128, 2, SEQ]); cmask = dr("cmask", [128, 2, 256])
    identb = dr("identb", [128, 128]); icnt = dr("icnt", [128, 4, SEQ])
    ev = []
    for i in range(2):
        ev.append(dict(Wt=dr(f"Wt{i}", [102, 128, KC, 128]), mu_rkv=dr(f"mu_rkv{i}", [2, 128, 24]), mu_l=dr(f"mu_l{i}", [128, 6]),
                       w2=dr(f"w2_{i}", [2, 128, 1024]), a2=dr(f"a2_{i}", [2, 128, 1024]), g2=dr(f"g2_{i}", [2, 128, 4, 1024]), pp=dr(f"pp{i}", [2, 128, 8, 8]),
                       Wo=dr(f"Wo{i}", [32, 128, KC, 128]), Wg=dr(f"Wg{i}", [86, 128, KC, 128]), Wu=dr(f"Wu{i}", [86, 128, KC, 128]), Wd=dr(f"Wd{i}", [32, 128, 86, 128])))
    od = []
    for i in range(2):
        od.append(dict(Wp=dr(f"Wp{i}", [4, 8, 128, 8, 128]), pscale=dr(f"pscale{i}", [128, KC]), Wr=dr(f"Wr{i}", [128, KC, 8]), rb=dr(f"rb{i}", [1, 8]),
                       Wg=dr(f"mWg{i}", [8, 14, 128, KC, 128]), Wu=dr(f"mWu{i}", [8, 14, 128, KC, 128]), Wd=dr(f"mWd{i}", [8, 32, 128, 14, 128])))
    modbase = dr("modbase", [128, 192], "Internal")
    xbuf = [dr("xbufA", [D, SEQ], "Internal"), dr("xbufB", [D, SEQ], "Internal")]
    zT = dr("zT_i", [102 * 128, SEQ], "Internal")
    yfull = dr("yfull", [D, SEQ], "Internal")
    modb = modbase.rearrange("p (i k) -> p i k", k=KC)

    build_p0f(nc, dict(cT=cT, ada_w=ada_w, bT=bT, oh=oh, modbase=modbase))
    src = xin
    for l in range(4):
        i = l // 2
        dst = out if l == 3 else xbuf[l % 2]
        if l % 2 == 0:
            E = ev[i]
            for th in range(2):
                ts_ = slice(th * 1024, (th + 1) * 1024)
                build_pa(nc=nc, io=dict(xT=src[:, ts_], modb=modb, tab=tabs[l], Wt=E["Wt"], zT=zT[:, ts_]))
            z3 = zT[0:6144, :].rearrange("(s c) t -> s c t", s=3)
            for hh in range(2):
                hs = slice(hh * 1024, (hh + 1) * 1024)
                q0 = 6880 + hh * 1024
                build_pb(nc=nc, io=dict(zrkv=z3[:, hs, :], zl=zT[6144:6912, :], mu_rkv=E["mu_rkv"][hh], mu_l=E["mu_l"], w2=E["w2"][hh], a2=E["a2"][hh],
                                        g2=E["g2"][hh], pp=E["pp"][hh], consts=pbc, zq=zT[q0:q0 + 1024, :], zkm=zT[q0 + 2048:q0 + 3072, :],
                                        zvT=zT[q0 + 4096:q0 + 5120, :], rope=rope, cmask=cmask, identb=identb,
                                        yT_r=yfull[hh * 1024:(hh + 1) * 1024, :], yT_m=yfull[2048 + hh * 1024:2048 + (hh + 1) * 1024, :]))
            for th in range(2):
                ts_ = slice(th * 1024, (th + 1) * 1024)
                build_pt("even", nc=nc, io=dict(xT=src[:, ts_], yT=yfull[:, ts_], Wo=E["Wo"], Wg=[E["Wg"]], Wu=[E["Wu"]], Wd=[E["Wd"]], modb=modb, tab=tabs[l],
                                                lng=lngs[l], lnb=lnbs[l], cst=cstT, outT=dst[:, ts_]))
        else:
            O = od[i]
            for th in range(2):
                ts_ = slice(th * 1024, (th + 1) * 1024)
                build_pt("odd", nc=nc, io=dict(xT=None, xfull=src, tok0=th * 1024, icnt=icnt[:, :, ts_], Wp=O["Wp"], pscale=O["pscale"], Wr=O["Wr"], rb=O["rb"],
                                               Wg=[O["Wg"][e] for e in range(8)], Wu=[O["Wu"][e] for e in range(8)], Wd=[O["Wd"][e] for e in range(8)],
                                               modb=modb, tab=tabs[l], lng=lngs[l], lnb=lnbs[l], cst=cstT, outT=dst[:, ts_]))
        src = dst
    return nc


_NC = None


def _c(a):
    return np.ascontiguousarray(a, dtype=np.float32)


def _tile_in(W, nch):
    N = W.shape[1]
    if N < nch * 128:
        W = np.concatenate([W, np.zeros((W.shape[0], nch * 128 - N), np.float32)], 1)
    return _c(W.reshape(32, 128, nch, 128).transpose(2, 1, 0, 3))


def _tile_down(W, nfc):
    return _c(W.reshape(nfc, 128, 32, 128).transpose(2, 1, 0, 3))


def kernel(**inp):
    global _NC
    inp = {k: np.asarray(v) for k, v in inp.items()}
    x = inp["x"].astype(np.float32)
    Bn, Tn, Dn = x.shape
    sh = {}
    sh["cT"] = _c(inp["c"].T.reshape(32, 128, 4).transpose(1, 0, 2))
    sh["ada_w"] = _c(inp["ada_w"])
    sh["bT"] = _c(inp["ada_b"].reshape(192, 128).T)
    sh["tabs"] = _c(inp["ada_table"].reshape(4, 6, 32, 128).transpose(0, 3, 1, 2))
    sh["lngs"] = _c(inp["ln_g"].reshape(4, 2, 32, 128).transpose(0, 3, 1, 2))
    sh["lnbs"] = _c(inp["ln_b"].reshape(4, 2, 32, 128).transpose(0, 3, 1, 2))
    cstT = np.zeros((128, 2, 128), np.float32); cstT[:, 0, :] = 1.0 / Dn; cstT[:, 1, :] = np.eye(128)
    sh["cstT"] = cstT; sh["pbc"] = pb_consts(); sh["rope"] = rope_tables(); sh["cmask"] = causal_masks(); sh["identb"] = np.eye(128, dtype=np.float32)
    tpos = np.arange(Tn, dtype=np.float32) + 1.0
    ic = np.stack([1.0 / np.minimum(tpos, float(w)) for w in (2, 4, 8, 16)]).astype(np.float32)
    sh["icnt"] = _c(np.broadcast_to(ic[None], (128, 4, Tn)))
    DA = 2048
    for i in range(2):
        sh[f"Wt{i}"] = _tile_in(inp["mix_w_in"][i], 102)
        mu = inp["mix_mu"][i]
        sh[f"mu_rkv{i}"] = _c(np.stack([np.concatenate([mu[s * DA + hh * 1024: s * DA + hh * 1024 + 1024].reshape(8, 128).T for s in range(3)], 1) for hh in range(2)]))
        mul = np.zeros(768, np.float32); mul[:736] = mu[3 * DA:3 * DA + 736]
        sh[f"mu_l{i}"] = _c(mul.reshape(6, 128).T)
        sh[f"w2_{i}"] = _c(np.stack([inp["rwkv_w2"][i][:, hh * 1024:(hh + 1) * 1024] for hh in range(2)]))
        sh[f"a2_{i}"] = _c(np.stack([inp["rwkv_a2"][i][:, hh * 1024:(hh + 1) * 1024] for hh in range(2)]))
        g2p = np.zeros((512, DA), np.float32); g2p[:480] = inp["rwkv_g2"][i]
        sh[f"g2_{i}"] = _c(np.stack([g2p[:, hh * 1024:(hh + 1) * 1024].reshape(4, 128, 1024).transpose(1, 0, 2) for hh in range(2)]))
        pv = lambda v, hh: np.asarray(v).reshape(-1)[hh * 1024:(hh + 1) * 1024].reshape(8, 128).T
        sh[f"pp{i}"] = _c(np.stack([np.stack([pv(inp["rwkv_w0"][i], hh), pv(inp["rwkv_a0"][i], hh), pv(inp["rwkv_kk"][i], hh), pv(inp["rwkv_ka"][i], hh),
                                             pv(inp["rwkv_rk"][i], hh), pv(inp["rwkv_lnx_g"][i], hh), pv(inp["rwkv_lnx_b"][i], hh),
                                             np.ones((128, 8), np.float32)], 1) for hh in range(2)]))
        sh[f"Wo{i}"] = _tile_in(inp["mix_w_out"][i], 32)
        sh[f"Wg{i}"] = _tile_in(inp["ffn_w_gate"][i], 86); sh[f"Wu{i}"] = _tile_in(inp["ffn_w_up"][i], 86); sh[f"Wd{i}"] = _tile_down(inp["ffn_w_down"][i], 86)
        sh[f"Wp{i}"] = _c(inp["pool_w"][i].reshape(4, 8, 128, 8, 128).transpose(0, 3, 2, 1, 4))
        sh[f"pscale{i}"] = _c(inp["pool_scale"][i].reshape(32, 128).T)
        sh[f"Wr{i}"] = _c(inp["moe_router_w"][i].reshape(32, 128, 8).transpose(1, 0, 2)); sh[f"rb{i}"] = _c(inp["moe_router_b"][i][None, :])
        sh[f"mWg{i}"] = np.stack([_tile_in(inp["moe_w_gate"][i, e], 14) for e in range(8)])
        sh[f"mWu{i}"] = np.stack([_tile_in(inp["moe_w_up"][i, e], 14) for e in range(8)])
        sh[f"mWd{i}"] = np.stack([_tile_down(inp["moe_w_down"][i, e], 14) for e in range(8)])
    if _NC is None:
        _NC = build_fused()
    maps = []
    for b in range(Bn):
        d = dict(sh)
        d["xT"] = _c(x[b].T)
        ohb = np.zeros((128, 4), np.float32); ohb[:, b] = 1.0
        d["oh"] = ohb
        maps.append(d)
    res = run_bass_kernel_spmd(_NC, maps, core_ids=list(range(Bn))).results
    return np.stack([res[b]["outT"].T for b in range(Bn)]).astype(np.float32)
```

```python
import numpy as np
from contextlib import ExitStack
import concourse.bass as bass
import concourse.mybir as mybir
from concourse.bass_utils import run_bass_kernel_spmd

F32 = mybir.dt.float32
BF16 = mybir.dt.bfloat16
AF = mybir.ActivationFunctionType
ALU = mybir.AluOpType
AX = mybir.AxisListType


class _Op:
    __slots__ = ("eng", "fn", "deps", "inc", "sem", "val", "dma", "idx", "waits", "call")


class _Rec:
    def __init__(self):
        self.call = None

    def __getattr__(self, name):
        def f(*a, **k):
            self.call = (name, a, k)
        return f


class MK:
    BLK = {"pe": "tensor", "act": "scalar", "dve": "vector", "pool": "gpsimd", "sp": "sync"}
    EPOCH = 4000
    NSLOT = 12

    _uid = 0

    def __init__(self, inorder=("pe",), nc=None):
        MK._uid += 1
        self.pfx = "" if nc is None else f"k{MK._uid}_"
        self.nc = nc if nc is not None else bass.Bass("TRN2", target_bir_lowering=False)
        self.st = ExitStack()
        self.ops = []
        self.lastw = {}
        self.readers = {}
        self.nname = 0
        self.inorder = set(inorder)

    def dram(self, name, shape, dt, kind, addr_space=None):
        if addr_space is not None:
            return self.nc.dram_tensor(name, list(shape), dt, kind=kind, addr_space=addr_space).ap()
        return self.nc.dram_tensor(name, list(shape), dt, kind=kind).ap()

    def sb(self, shape, dt, name=None):
        self.nname += 1
        return self.st.enter_context(self.nc.sbuf_tensor(self.pfx + (name or f"sb{self.nname}"), list(shape), dt))

    def ps(self, shape, dt=F32, name=None):
        self.nname += 1
        return self.st.enter_context(self.nc.psum_tensor(self.pfx + (name or f"ps{self.nname}"), list(shape), dt))

    def add(self, eng, fn, r=(), w=(), dma=False):
        op = _Op()
        op.eng, op.fn, op.dma, op.inc = eng, fn, dma, dma
        op.idx = len(self.ops)
        rec = _Rec(); fn(rec); op.call = rec.call
        assert op.call is not None
        op.sem = op.val = None
        isps = lambda k: (isinstance(k, tuple) and str(k[0]).startswith("ps")) or (isinstance(k, str) and k.startswith("ps"))
        w = list(w) + [k for k in r if isps(k) and k not in w]
        deps = set()
        for k in r:
            if k in self.lastw:
                deps.add(self.lastw[k])
        for k in w:
            if k in self.lastw:
                deps.add(self.lastw[k])
            for rd in self.readers.get(k, {}).values():
                deps.add(rd)
        deps.discard(op.idx)
        op.deps = deps
        for k in w:
            self.lastw[k] = op.idx
            self.readers[k] = {}
        for k in r:
            d = self.readers.setdefault(k, {})
            d[(eng, op.idx) if dma else eng] = op.idx
        self.ops.append(op)
        return op

    def pe(self, fn, r=(), w=()): return self.add("pe", fn, r, w)
    def act(self, fn, r=(), w=()): return self.add("act", fn, r, w)
    def dve(self, fn, r=(), w=()): return self.add("dve", fn, r, w)
    def pool(self, fn, r=(), w=()): return self.add("pool", fn, r, w)
    def dma(self, out, in_, r=(), w=(), eng="sp", **kw):
        return self.add(eng, lambda e: e.dma_start(out=out, in_=in_, **kw), r, w, dma=True)

    def coll(self, kind, op, groups, in_ap, out_ap, r=(), w=()):
        return self.add("pool", lambda e: e.collective_compute(kind, op, replica_groups=groups, ins=[in_ap], outs=[out_ap]), r, w, dma=True)

    def build(self):
        nc = self.nc
        ops = self.ops
        for op in ops:
            for d in op.deps:
                dop = ops[d]
                if dop.dma or dop.eng != op.eng or op.eng not in self.inorder or op.dma:
                    dop.inc = True
        cnt = {}
        semobj = {}

        def getsem(key):
            if key not in semobj:
                semobj[key] = nc.alloc_semaphore(name=self.pfx + "s_%s_%s" % key)
            return semobj[key]

        dcnt = {}
        slotval = {}
        for op in ops:
            op.waits = []
            if op.dma:
                n = dcnt.get(op.eng, 0)
                dcnt[op.eng] = n + 1
                key = ("d" + op.eng, n % self.NSLOT)
                prev = slotval.get(key, 0)
                if prev:
                    op.waits.append((getsem(key), prev))
                op.sem, op.val = getsem(key), prev + 16
                slotval[key] = prev + 16
            elif op.inc:
                n = cnt.get(op.eng, 0)
                cnt[op.eng] = n + 1
                op.sem, op.val = getsem((op.eng, n // self.EPOCH)), n % self.EPOCH + 1
        for op in ops:
            for d in sorted(op.deps):
                dop = ops[d]
                if (not dop.dma) and (not op.dma) and dop.eng == op.eng and op.eng in self.inorder:
                    continue
                op.waits.append((dop.sem, dop.val))
        finals = [(getsem(k), v) for k, v in slotval.items()]
        with nc.Block() as block:
            for eng, bname in self.BLK.items():
                eops = [op for op in ops if op.eng == eng]
                if not eops and eng != "sp":
                    continue

                def body(e, eops=eops, eng=eng):
                    waited = {}
                    for op in eops:
                        for sem, val in op.waits:
                            if waited.get(id(sem), 0) >= val:
                                continue
                            e.wait_ge(sem, val)
                            waited[id(sem)] = val
                        ins = getattr(e, op.call[0])(*op.call[1], **op.call[2])
                        if op.inc:
                            ins.then_inc(op.sem, 16 if op.dma else 1)
                    if eng == "sp":
                        for sem, val in finals:
                            if waited.get(id(sem), 0) < val:
                                e.wait_ge(sem, val)

                getattr(block, bname)(body)
        nc.clear_and_free_semaphores(list(semobj.values()))
        nc.all_engine_barrier()
        self.st.close()
        return nc


def run(mk_or_nc, in_maps, trace=False):
    nc = mk_or_nc.nc if isinstance(mk_or_nc, MK) else mk_or_nc
    return run_bass_kernel_spmd(nc, in_maps, core_ids=list(range(len(in_maps))), trace=trace)


T = 2048
L = 64
NCH = T // L
C0 = float(np.exp(-0.5))
NEG = -1e30


class Ring:
    def __init__(self, tiles, name):
        self.t = tiles; self.n = 0; self.name = name

    def get(self):
        i = self.n % len(self.t); self.n += 1
        return self.t[i], (self.name, i)


def build_pb(do_rwkv=True, do_moba=True, npairs=8, nheads=8, nt=None, stage=3, nc=None, io=None, tag=''):
    m = MK(nc=nc)
    PADDED = io is None
    if io is None:
        zrkv = m.dram("zrkv", [3, 1024, T + 1], F32, "ExternalInput")
        zl = m.dram("zl", [768, T + 1], F32, "ExternalInput")
        mu_rkv = m.dram("mu_rkv", [128, 24], F32, "ExternalInput")
        mu_l = m.dram("mu_l", [128, 6], F32, "ExternalInput")
        w2 = m.dram("w2", [128, 1024], F32, "ExternalInput")
        a2 = m.dram("a2", [128, 1024], F32, "ExternalInput")
        g2 = m.dram("g2", [128, 4, 1024], F32, "ExternalInput")
        pp = m.dram("pp", [128, 8, 8], F32, "ExternalInput")
        consts = m.dram("consts", [128, 6, 128], F32, "ExternalInput")
        zq = m.dram("zq", [1024, T], F32, "ExternalInput")
        zk = m.dram("zkm", [1024, T], F32, "ExternalInput")
        zv = m.dram("zvm", [128, 16, 1024], F32, "ExternalInput")
        rope = m.dram("rope", [128, 2, T], F32, "ExternalInput")
        cmask = m.dram("cmask", [128, 2, 256], F32, "ExternalInput")
        identb = m.dram("identb", [128, 128], F32, "ExternalInput")
        yT = m.dram("yT", [2048, T], F32, "ExternalOutput")


    else:
        zrkv = io["zrkv"]; zl = io["zl"]; mu_rkv = io["mu_rkv"]; mu_l = io["mu_l"]; w2 = io["w2"]; a2 = io["a2"]; g2 = io["g2"]; pp = io["pp"]
        consts = io["consts"]; zq = io["zq"]; zk = io["zkm"]; zvT = io["zvT"]; rope = io["rope"]; cmask = io["cmask"]; identb = io["identb"]; yT = None
    yT_r = yT[0:1024, :] if io is None else io["yT_r"]
    yT_m = yT[1024:2048, :] if io is None else io["yT_m"]
    cst = m.sb([128, 6, 128], F32, "cst")
    m.dma(cst[:], consts, w=["cst"])
    ident = cst[:, 0, :]; bones = cst[:, 1, :]; bones64 = cst[:, 2, :]; rsw = cst[:, 3, :]
    ppt = m.sb([128, 8, 8], F32, "ppt"); m.dma(ppt[:], pp, w=["pp"])
    m.dve(lambda e: e.tensor_scalar(out=ppt[:, 7, :], in0=ppt[:, 3, :], scalar1=-1.0, scalar2=1.0, op0=ALU.mult, op1=ALU.add), r=["pp"], w=["pp"])
    murkv = m.sb([128, 24], F32, "murkv"); m.dma(murkv[:], mu_rkv, w=["mu"])
    mul = m.sb([128, 6], F32, "mul"); m.dma(mul[:], mu_l, w=["mu"])
    psr = Ring([m.ps([128, 512], F32, f"psb{i}") for i in range(7)], "ps")
    psb16 = m.ps([128, 1024], BF16, "psb16")

    if do_rwkv:
        w2t = m.sb([128, 1024], F32, "w2t"); m.dma(w2t[:], w2, w=["w2"])
        a2t = m.sb([128, 1024], F32, "a2t"); m.dma(a2t[:], a2, w=["a2"])
        g2t = m.sb([128, 4, 1024], F32, "g2t"); m.dma(g2t[:], g2, w=["g2"])
        TT = 512 if do_moba else 256
        NT = nt or (T // TT)
        CPT = TT // L
        gmask = m.sb([64, 512], F32, "gmask")
        for q in range(4):
            m.dve(lambda e, q=q: e.tensor_copy(out=gmask[:, q * 128:(q + 1) * 128], in_=cst[0:64, 4, :]), r=["cst"], w=["gmask"])
        lmask = m.sb([64, 128], F32, "lmask")
        for q in range(2):
            m.dve(lambda e, q=q: e.tensor_copy(out=lmask[:, q * 64:(q + 1) * 64], in_=cst[0:64, 5, 0:64]), r=["cst"], w=["lmask"])
        id2 = m.sb([64, 128], F32, "id2")
        for q in range(2):
            m.dve(lambda e, q=q: e.tensor_copy(out=id2[:, q * 64:(q + 1) * 64], in_=cst[0:64, 0, 0:64]), r=["cst"], w=["id2"])
        rmask = m.sb([128, TT], F32, "rmask")
        m.dve(lambda e: e.memset(rmask[:], 1.0), w=["rmask"])
        m.dve(lambda e: e.memset(rmask[:].rearrange("p (c l) -> p c l", l=L)[:, :, 0:1], 0.0), r=["rmask"], w=["rmask"])
        P = [m.sb([64, 2, 64], F32, f"P{p}") for p in range(npairs)]
        for p in range(npairs):
            m.dve(lambda e, p=p: e.memset(P[p][:], 0.0), w=[("P", p)])
        lor = [m.sb([128, TT], F32, f"lor{i}") for i in range(6)]
        zring = Ring([m.sb([128, TT + 1], F32, f"zraw{i}") for i in range(3 if do_moba else 6)], "zraw")
        NSLOT = 1 if do_moba else 4
        names = ["r", "k", "v", "a", "sg", "cl", "kkn", "kmod", "e1", "t1", "kt", "bt", "g", "y", "t2"]
        slots = [{n: m.sb([128, TT], F32, f"{n}_{s}") for n in names} for s in range(NSLOT)]
        ARs = [m.sb([128, CPT, 2, L], F32, f"AR{s}") for s in range(NSLOT)]
        los = [dict(AR=m.sb([64, CPT, 2, L], F32, f"ARlo{s}"), bt=m.sb([64, TT], F32, f"btlo{s}"), kt=m.sb([64, TT], F32, f"ktlo{s}"),
                    e1=m.sb([64, TT], F32, f"e1lo{s}"), y=m.sb([64, 2, TT], F32, f"ylo{s}")) for s in range(NSLOT)]
        smalls = [Ring([m.sb([64, 256], F32, f"sm{s}_{i}") for i in range(6)], f"sm{s}") for s in range(NSLOT)]
        gmrs = [Ring([m.sb([64, 512], F32, f"gm{s}_{i}") for i in range(2)], f"gm{s}") for s in range(NSLOT)]
        tokrs = [Ring([m.sb([64, 384], F32, f"tok{s}_{i}") for i in range(2)], f"tok{s}") for s in range(NSLOT)]
        xurs = [Ring([m.sb([64, 128], F32, f"xu{s}_{i}") for i in range(4)], f"xu{s}") for s in range(NSLOT)]

        def shift(dst, dkey, src_dram_rows, t0, mucol, extra_act=None):
            zt, zkey = zring.get()
            if PADDED:
                m.dma(zt[:], src_dram_rows[:, t0:t0 + TT + 1], w=[zkey])
            elif t0 == 0:
                m.dve(lambda e: e.memset(zt[:, 0:1], 0.0), w=[zkey])
                m.dma(zt[:, 1:TT + 1], src_dram_rows[:, 0:TT], r=[zkey], w=[zkey])
            else:
                m.dma(zt[:], src_dram_rows[:, t0 - 1:t0 + TT], w=[zkey])
            m.dve(lambda e: e.tensor_tensor(out=dst[:], in0=zt[:, 0:TT], in1=zt[:, 1:TT + 1], op=ALU.subtract),
                  r=[zkey], w=[dkey])
            m.dve(lambda e: e.scalar_tensor_tensor(out=dst[:], in0=dst[:], scalar=mucol, in1=zt[:, 1:TT + 1],
                                                   op0=ALU.mult, op1=ALU.add), r=[zkey, dkey, "mu"], w=[dkey])

        for ti in range(NT):
            t0 = ti * TT
            for i in range(6):
                shift(lor[i], ("lor", i), zl[i * 128:(i + 1) * 128, :], t0, mul[:, i:i + 1])
                if i == 0:
                    m.act(lambda e: e.activation(out=lor[0][:], in_=lor[0][:], func=AF.Tanh), r=[("lor", 0)], w=[("lor", 0)])
                elif i >= 2:
                    m.act(lambda e, i=i: e.activation(out=lor[i][:], in_=lor[i][:], func=AF.Sigmoid), r=[("lor", i)], w=[("lor", i)])
            def pair_body(p, s):
                small = smalls[s]; gmr = gmrs[s]; tokr = tokrs[s]; xur = xurs[s]
                B = slots[s]
                K = lambda n: (n, s)
                pc = slice(p * 128, (p + 1) * 128)
                par = lambda j: ppt[:, j, p:p + 1]
                AR = ARs[s]
                shift(B["r"], K("r"), zrkv[0, pc, :], t0, murkv[:, p:p + 1])
                shift(B["k"], K("k"), zrkv[1, pc, :], t0, murkv[:, 8 + p:9 + p])
                shift(B["v"], K("v"), zrkv[2, pc, :], t0, murkv[:, 16 + p:17 + p])
                yield
                ps, pk = psr.get()
                m.pe(lambda e, ps=ps: e.matmul(ps[:, 0:TT], lhsT=w2t[:, pc], rhs=lor[0][:], start=True, stop=True),
                     r=["w2", ("lor", 0)], w=[pk])
                m.act(lambda e, ps=ps: e.activation(out=B["sg"][:], in_=ps[:, 0:TT], func=AF.Sigmoid, bias=par(0)),
                      r=[pk, "pp"], w=[K("sg")])
                yield
                ps, pk = psr.get()
                m.pe(lambda e, ps=ps: e.matmul(ps[:, 0:TT], lhsT=a2t[:, pc], rhs=lor[1][:], start=True, stop=True),
                     r=["a2", ("lor", 1)], w=[pk])
                m.act(lambda e, ps=ps: e.activation(out=B["a"][:], in_=ps[:, 0:TT], func=AF.Sigmoid, bias=par(1)),
                      r=[pk, "pp"], w=[K("a")])
                yield
                ps, pk = psr.get()
                for j in range(4):
                    m.pe(lambda e, ps=ps, j=j: e.matmul(ps[:, 0:TT], lhsT=g2t[:, j, pc], rhs=lor[2 + j][:], start=(j == 0), stop=(j == 3)),
                         r=["g2", ("lor", 2 + j)], w=[pk])
                m.act(lambda e, ps=ps: e.copy(out=B["g"][:], in_=ps[:, 0:TT]), r=[pk], w=[K("g")])
                m.dve(lambda e: e.tensor_scalar(out=B["kkn"][:], in0=B["k"][:], scalar1=par(2), scalar2=None, op0=ALU.mult),
                      r=[K("k"), "pp"], w=[K("kkn")])
                m.act(lambda e: e.activation(out=B["t1"][:], in_=B["kkn"][:], func=AF.Square), r=[K("kkn")], w=[K("t1")])
                yield
                ps, pk = psr.get()
                m.pe(lambda e, ps=ps: e.matmul(ps[:, 0:TT], lhsT=bones, rhs=B["t1"][:], start=True, stop=True), r=["cst", K("t1")], w=[pk])
                m.dve(lambda e, ps=ps: e.tensor_scalar(out=B["t1"][:], in0=ps[:, 0:TT], scalar1=1e-24, scalar2=None, op0=ALU.max),
                      r=[pk], w=[K("t1")])
                m.act(lambda e: e.activation(out=B["t1"][:], in_=B["t1"][:], func=AF.Ln), r=[K("t1")], w=[K("t1")])
                m.act(lambda e: e.activation(out=B["t1"][:], in_=B["t1"][:], func=AF.Exp, scale=-0.5), r=[K("t1")], w=[K("t1")])
                m.dve(lambda e: e.tensor_tensor(out=B["kkn"][:], in0=B["kkn"][:], in1=B["t1"][:], op=ALU.mult),
                      r=[K("kkn"), K("t1")], w=[K("kkn")])
                m.dve(lambda e: e.tensor_scalar(out=B["kmod"][:], in0=B["a"][:], scalar1=par(3), scalar2=par(7), op0=ALU.mult, op1=ALU.add),
                      r=[K("a"), "pp"], w=[K("kmod")])
                m.dve(lambda e: e.tensor_tensor(out=B["kmod"][:], in0=B["kmod"][:], in1=B["k"][:], op=ALU.mult),
                      r=[K("kmod"), K("k")], w=[K("kmod")])
                m.dve(lambda e: e.tensor_tensor_scan(out=B["cl"][:], data0=rmask[:], data1=B["sg"][:], initial=0.0, op0=ALU.mult, op1=ALU.add),
                      r=["rmask", K("sg")], w=[K("cl")])
                m.act(lambda e: e.activation(out=B["e1"][:], in_=B["cl"][:], func=AF.Exp, scale=-C0), r=[K("cl")], w=[K("e1")])
                m.dve(lambda e: e.tensor_tensor(out=AR[:, :, 1, :], in0=B["r"][:].rearrange("p (c l) -> p c l", l=L),
                                                in1=B["e1"][:].rearrange("p (c l) -> p c l", l=L), op=ALU.mult),
                      r=[K("r"), K("e1")], w=[("AR", s)])
                m.dve(lambda e: e.tensor_tensor(out=B["t1"][:], in0=B["cl"][:], in1=B["sg"][:], op=ALU.subtract),
                      r=[K("cl"), K("sg")], w=[K("t1")])
                m.act(lambda e: e.activation(out=B["t1"][:], in_=B["t1"][:], func=AF.Exp, scale=-C0), r=[K("t1")], w=[K("t1")])
                m.dve(lambda e: e.scalar_tensor_tensor(out=AR[:, :, 0, :], in0=B["kkn"][:].rearrange("p (c l) -> p c l", l=L), scalar=-1.0,
                                                       in1=B["t1"][:].rearrange("p (c l) -> p c l", l=L), op0=ALU.mult, op1=ALU.mult),
                      r=[K("kkn"), K("t1"), ("AR", s)], w=[("AR", s)])
                m.act(lambda e: e.activation(out=B["t2"][:], in_=B["cl"][:], func=AF.Exp, scale=C0), r=[K("cl")], w=[K("t2")])
                m.dve(lambda e: e.tensor_tensor(out=B["kt"][:], in0=B["kmod"][:], in1=B["t2"][:], op=ALU.mult),
                      r=[K("kmod"), K("t2")], w=[K("kt")])
                m.dve(lambda e: e.tensor_tensor(out=B["bt"][:], in0=B["kkn"][:], in1=B["a"][:], op=ALU.mult),
                      r=[K("kkn"), K("a")], w=[K("bt")])
                m.dve(lambda e: e.tensor_tensor(out=B["bt"][:], in0=B["bt"][:], in1=B["t2"][:], op=ALU.mult),
                      r=[K("bt"), K("t2")], w=[K("bt")])
                if stage == 0:
                    m.dma(yT[p * 128:(p + 1) * 128, t0:t0 + TT], B["kt"][:], r=[K("kt")])
                    return
                LO = los[s]
                m.dma(LO["AR"][:], AR[64:128, :, :, :], r=[("AR", s)], w=[("ARlo", s)])
                m.dma(LO["bt"][:], B["bt"][64:128, :], r=[K("bt")], w=[("btlo", s)])
                m.dma(LO["kt"][:], B["kt"][64:128, :], r=[K("kt")], w=[("ktlo", s)])
                m.dma(LO["e1"][:], B["e1"][64:128, :], r=[K("e1")], w=[("e1lo", s)])
                ARk = [("AR", s), ("ARlo", s)]; btk = [K("bt"), ("btlo", s)]; ktk = [K("kt"), ("ktlo", s)]
                P2 = P[p]; Pk = ("P", p)
                ylo = LO["y"]
                for c in range(CPT):
                    cs = slice(c * L, (c + 1) * L)
                    ARh = [AR[0:64, c, :, :], LO["AR"][:, c, :, :]]
                    ARa = [AR[0:64, c, 0, :], LO["AR"][:, c, 0, :]]
                    ARr = [AR[0:64, c, 1, :], LO["AR"][:, c, 1, :]]
                    bth = [B["bt"][0:64, cs], LO["bt"][:, cs]]
                    kth = [B["kt"][0:64, cs], LO["kt"][:, cs]]
                    yield
                    ps, pk = psr.get()
                    for h in range(2):
                        m.pe(lambda e, ps=ps, h=h: e.matmul(ps[0:64, h * 128:(h + 1) * 128], lhsT=bth[h], rhs=ARh[h], start=True, stop=True),
                             r=[btk[h], ARk[h]], w=[pk])
                        m.pe(lambda e, ps=ps, h=h: e.matmul(ps[0:64, 256 + h * 128:256 + (h + 1) * 128], lhsT=kth[h], rhs=ARh[h], start=True, stop=True),
                             r=[ktk[h], ARk[h]], w=[pk])
                    GM, gk = gmr.get()
                    m.dve(lambda e, ps=ps, GM=GM: e.tensor_tensor(out=GM[:, :], in0=ps[0:64, :], in1=gmask[:, :], op=ALU.mult),
                          r=[pk, "gmask"], w=[gk])
                    yield
                    ps, pk = psr.get()
                    for h in range(2):
                        m.pe(lambda e, ps=ps, h=h: e.matmul(ps[0:64, h * 64:(h + 1) * 64], lhsT=ARa[h], rhs=bth[h], start=True, stop=True),
                             r=[btk[h], ARk[h]], w=[pk])
                    FE, fk = small.get()
                    m.dve(lambda e, ps=ps, FE=FE: e.tensor_tensor(out=FE[:, 0:128], in0=ps[0:64, 0:128], in1=lmask[:, :], op=ALU.mult),
                          r=[pk, "lmask"], w=[fk])
                    for h in range(2):
                        m.act(lambda e, FE=FE, GM=GM, h=h: e.copy(out=FE[:, 128 + h * 64:128 + (h + 1) * 64], in_=GM[:, h * 128:h * 128 + 64]),
                              r=[gk, fk], w=[fk])
                    yield
                    ps, pk = psr.get()
                    for j, nm in enumerate(["v", "bt", "kt"]):
                        m.pe(lambda e, ps=ps, j=j, nm=nm: e.transpose(ps[0:64, j * 128:(j + 1) * 128], B[nm][:, cs], ident),
                             r=[K(nm), "cst"], w=[pk])
                    FE0, fk0 = FE, fk
                    TOK, tk = tokr.get()
                    m.act(lambda e, ps=ps, TOK=TOK: e.copy(out=TOK[:, 0:384], in_=ps[0:64, 0:384]), r=[pk], w=[tk])
                    Tt, ttk = small.get()
                    m.dve(lambda e, Tt=Tt, FE=FE: e.tensor_tensor(out=Tt[:, 0:128], in0=FE[:, 128:256], in1=id2[:, :], op=ALU.add), r=[fk, "id2"], w=[ttk])
                    for lev in range(6):
                        if lev == 0 or lev < 5 or True:
                            yield
                            ps, pk = psr.get()
                        if lev >= 1:
                            for h in range(2):
                                f = slice(h * 64, (h + 1) * 64)
                                m.pe(lambda e, ps=ps, Tt=Tt, FE=FE, f=f: e.matmul(ps[0:64, f], lhsT=FE[:, f], rhs=Tt[:, f], start=True, stop=True),
                                     r=[fk, ttk], w=[pk])
                        if lev < 5:
                            for h in range(2):
                                f = slice(h * 64, (h + 1) * 64)
                                ef = slice(128 + h * 64, 128 + (h + 1) * 64)
                                m.pe(lambda e, ps=ps, FE=FE, f=f, ef=ef, h=h: e.matmul(ps[0:64, 128 + h * 64:128 + (h + 1) * 64], lhsT=FE[:, ef], rhs=FE[:, f], start=True, stop=True),
                                     r=[fk], w=[pk])
                                if lev < 4:
                                    m.pe(lambda e, ps=ps, FE=FE, f=f, ef=ef, h=h: e.matmul(ps[0:64, 256 + h * 64:256 + (h + 1) * 64], lhsT=FE[:, f], rhs=FE[:, ef], start=True, stop=True),
                                         r=[fk], w=[pk])
                        if lev >= 1:
                            Tn, tnk = small.get()
                            m.dve(lambda e, ps=ps, Tn=Tn, Tt=Tt: e.tensor_tensor(out=Tn[:, 0:128], in0=ps[0:64, 0:128], in1=Tt[:, 0:128], op=ALU.add), r=[pk, ttk], w=[tnk])
                            Tt, ttk = Tn, tnk
                        if lev < 5:
                            FEn, fnk = small.get()
                            wdt = 256 if lev < 4 else 128
                            m.act(lambda e, ps=ps, FEn=FEn, wdt=wdt: e.copy(out=FEn[:, 0:wdt], in_=ps[0:64, 128:128 + wdt]), r=[pk], w=[fnk])
                            FE, fk = FEn, fnk
                    if stage == 1:
                        if c == 0 and p == 0 and ti == 0:
                            m.dma(yT[1024:1088, 0:512], GM[:, :], r=[gk])
                            m.dma(yT[1088:1152, 0:384], TOK[:, 0:384], r=[tk])
                            m.dma(yT[1152:1216, 0:128], Tt[:, 0:128], r=[ttk])
                            m.dma(yT[1216:1280, 0:256], FE0[:, 0:256], r=[fk0])
                        continue
                    yield
                    ps, pk = psr.get()
                    for h in range(2):
                        f = slice(h * 64, (h + 1) * 64)
                        m.pe(lambda e, ps=ps, GM=GM, TOK=TOK, h=h, f=f: e.matmul(ps[0:64, f], lhsT=GM[:, 256 + h * 128:256 + h * 128 + 64], rhs=TOK[:, f],
                                                                                 start=True, stop=False), r=[gk, tk], w=[pk])
                        m.pe(lambda e, ps=ps, f=f, h=h: e.matmul(ps[0:64, f], lhsT=ARa[h], rhs=P2[:, h, :], start=False, stop=True),
                             r=[ARk[h], Pk], w=[pk])
                    X0, xk = xur.get()
                    m.dve(lambda e, ps=ps, X0=X0: e.tensor_copy(out=X0[:, 0:128], in_=ps[0:64, 0:128]), r=[pk], w=[xk])
                    yield
                    ps, pk = psr.get()
                    for h in range(2):
                        f = slice(h * 64, (h + 1) * 64)
                        m.pe(lambda e, ps=ps, Tt=Tt, X0=X0, f=f: e.matmul(ps[0:64, f], lhsT=Tt[:, f], rhs=X0[:, f], start=True, stop=True),
                             r=[ttk, xk], w=[pk])
                    U, uk = xur.get()
                    m.act(lambda e, ps=ps, U=U: e.copy(out=U[:, 0:128], in_=ps[0:64, 0:128]), r=[pk], w=[uk])
                    yield
                    ps, pk = psr.get()
                    for h in range(2):
                        f = slice(h * 64, (h + 1) * 64)
                        m.pe(lambda e, ps=ps, TOK=TOK, GM=GM, f=f, h=h: e.matmul(ps[0:64, f], lhsT=TOK[:, f], rhs=GM[:, 256 + h * 128 + 64:256 + (h + 1) * 128],
                                                                                 start=True, stop=False), r=[tk, gk], w=[pk])
                        m.pe(lambda e, ps=ps, U=U, GM=GM, f=f, h=h: e.matmul(ps[0:64, f], lhsT=U[:, f], rhs=GM[:, h * 128 + 64:(h + 1) * 128],
                                                                             start=False, stop=False), r=[uk, gk], w=[pk])
                        m.pe(lambda e, ps=ps, f=f, h=h: e.matmul(ps[0:64, f], lhsT=P2[:, h, :], rhs=ARr[h], start=False, stop=True),
                             r=[Pk, ARk[h]], w=[pk])
                    for h in range(2):
                        f = slice(h * 64, (h + 1) * 64)
                        o = slice(128 + h * 64, 128 + (h + 1) * 64)
                        m.pe(lambda e, ps=ps, TOK=TOK, f=f, o=o, h=h: e.matmul(ps[0:64, o], lhsT=TOK[:, 256 + h * 64:256 + (h + 1) * 64], rhs=TOK[:, f],
                                                                               start=True, stop=False), r=[tk], w=[pk])
                        m.pe(lambda e, ps=ps, TOK=TOK, U=U, f=f, o=o, h=h: e.matmul(ps[0:64, o], lhsT=TOK[:, 128 + h * 64:128 + (h + 1) * 64], rhs=U[:, f],
                                                                                    start=False, stop=True), r=[tk, uk], w=[pk])
                    m.act(lambda e, ps=ps: e.copy(out=ylo[:, :, cs], in_=ps[0:64, 0:128].rearrange("p (h t) -> p h t", h=2)), r=[pk], w=[("ylo", s)])
                    m.dve(lambda e, ps=ps: e.tensor_tensor(out=P2[:, :, :], in0=P2[:, :, :], in1=ps[0:64, 128:256].rearrange("p (h t) -> p h t", h=2), op=ALU.add),
                          r=[pk, Pk], w=[Pk])
                    gcol = c * L + L - 1
                    m.dve(lambda e, gcol=gcol: e.tensor_scalar(out=P2[:, 0, :], in0=P2[:, 0, :], scalar1=B["e1"][0:64, gcol:gcol + 1], scalar2=None, op0=ALU.mult),
                          r=[Pk, K("e1")], w=[Pk])
                    m.dve(lambda e, gcol=gcol: e.tensor_scalar(out=P2[:, 1, :], in0=P2[:, 1, :], scalar1=LO["e1"][:, gcol:gcol + 1], scalar2=None, op0=ALU.mult),
                          r=[Pk, ("e1lo", s)], w=[Pk])
                if stage == 1:
                    m.dma(yT[p * 128:(p + 1) * 128, t0:t0 + TT], B["bt"][:], r=[K("bt")])
                    return
                m.dma(B["y"][0:64, :], ylo[:, 0, :], r=[("ylo", s)], w=[K("y")])
                m.dma(B["y"][64:128, :], ylo[:, 1, :], r=[("ylo", s)], w=[K("y")])
                if stage == 2:
                    m.dma(yT[p * 128:(p + 1) * 128, t0:t0 + TT], B["y"][:], r=[K("y")])
                    return
                yield
                ps, pk = psr.get()
                m.pe(lambda e, ps=ps: e.matmul(ps[:, 0:TT], lhsT=bones64, rhs=B["y"][:], start=True, stop=True), r=["cst", K("y")], w=[pk])
                m.dve(lambda e, ps=ps: e.tensor_tensor(out=B["y"][:], in0=B["y"][:], in1=ps[:, 0:TT], op=ALU.subtract), r=[pk, K("y")], w=[K("y")])
                m.act(lambda e: e.activation(out=B["t1"][:], in_=B["y"][:], func=AF.Square), r=[K("y")], w=[K("t1")])
                yield
                ps, pk = psr.get()
                m.pe(lambda e, ps=ps: e.matmul(ps[:, 0:TT], lhsT=bones64, rhs=B["t1"][:], start=True, stop=True), r=["cst", K("t1")], w=[pk])
                m.dve(lambda e, ps=ps: e.tensor_scalar(out=B["t1"][:], in0=ps[:, 0:TT], scalar1=64e-5, scalar2=None, op0=ALU.add), r=[pk], w=[K("t1")])
                m.act(lambda e: e.activation(out=B["t1"][:], in_=B["t1"][:], func=AF.Ln), r=[K("t1")], w=[K("t1")])
                m.act(lambda e: e.activation(out=B["t1"][:], in_=B["t1"][:], func=AF.Exp, scale=-0.5), r=[K("t1")], w=[K("t1")])
                m.dve(lambda e: e.tensor_tensor(out=B["y"][:], in0=B["y"][:], in1=B["t1"][:], op=ALU.mult), r=[K("y"), K("t1")], w=[K("y")])
                m.act(lambda e: e.activation(out=B["y"][:], in_=B["y"][:], func=AF.Identity, scale=par(5), bias=par(6)), r=[K("y"), "pp"], w=[K("y")])
                m.dve(lambda e: e.scalar_tensor_tensor(out=B["t2"][:], in0=B["r"][:], scalar=par(4), in1=B["kmod"][:], op0=ALU.mult, op1=ALU.mult),
                      r=[K("r"), K("kmod"), "pp"], w=[K("t2")])
                yield
                ps, pk = psr.get()
                m.pe(lambda e, ps=ps: e.matmul(ps[:, 0:TT], lhsT=bones, rhs=B["t2"][:], start=True, stop=True), r=["cst", K("t2")], w=[pk])
                m.dve(lambda e, ps=ps: e.tensor_tensor(out=B["t2"][:], in0=ps[:, 0:TT], in1=B["v"][:], op=ALU.mult), r=[pk, K("v")], w=[K("t2")])
                m.dve(lambda e: e.tensor_tensor(out=B["y"][:], in0=B["y"][:], in1=B["t2"][:], op=ALU.add), r=[K("y"), K("t2")], w=[K("y")])
                m.dve(lambda e: e.tensor_tensor(out=B["t2"][:], in0=B["y"][:], in1=B["g"][:], op=ALU.mult), r=[K("y"), K("g"), K("t2")], w=[K("t2")])
                m.dma(yT_r[p * 128:(p + 1) * 128, t0:t0 + TT], B["t2"][:], r=[K("t2")])

            for p0 in range(0, npairs, NSLOT):
                gens = [pair_body(p0 + j, j) for j in range(min(NSLOT, npairs - p0))]
                while gens:
                    for g_ in list(gens):
                        try:
                            next(g_)
                        except StopIteration:
                            gens.remove(g_)
    if do_moba:
        ropet = m.sb([128, 2, T], F32, "ropet"); m.dma(ropet[:], rope, w=["rope"])
        cm = m.sb([128, 2, 256], F32, "cm"); m.dma(cm[:], cmask, w=["cm"])
        idb = m.sb([128, 128], BF16, "idb"); m.dma(idb[:], identb, w=["idb"], eng="pool")
        qf = m.sb([128, T], F32, "qf"); kf = m.sb([128, T], F32, "kf")
        qb = m.sb([128, T], BF16, "qb"); kb = m.sb([128, T], BF16, "kb")
        vb = m.sb([128, 16, 128], BF16, "vb")
        raw = m.sb([128, T], F32, "mraw")
        kmean = m.sb([128, 8], F32, "kmean")
        oT = m.sb([128, T], F32, "oT")
        NB = 1 if do_rwkv else 2
        sS = [m.sb([128, T], F32, f"sS{i}") for i in range(NB)]
        pB = [m.sb([128, T], BF16, f"pB{i}") for i in range(NB)]
        pT = [m.sb([128, 16, 128], BF16, f"pT{i}") for i in range(NB)]
        gt = [m.sb([128, 8], F32, f"gt{i}") for i in range(2)]
        v8 = [m.sb([128, 8], F32, f"v8{i}") for i in range(2)]
        bias = [m.sb([128, 8], F32, f"bias{i}") for i in range(2)]
        st = [m.sb([128, 4], F32, f"st{i}") for i in range(2)]
        SC = 128 ** -0.5
        for hd in range(nheads):
            hr = slice(hd * 128, (hd + 1) * 128)
            for nm, src, dstf, dstb in (("q", zq, qf, qb), ("k", zk, kf, kb)):
                m.dma(raw[:], src[hr, :], w=["mraw"])
                for tt in range(4):
                    ts_ = slice(tt * 512, (tt + 1) * 512)
                    ps, pk = psr.get()
                    m.pe(lambda e, ps=ps, ts_=ts_: e.matmul(ps[:, :], lhsT=rsw, rhs=raw[:, ts_], start=True, stop=True), r=["cst", "mraw"], w=[pk])
                    m.dve(lambda e, ps=ps, ts_=ts_, dstf=dstf: e.tensor_tensor(out=dstf[:, ts_], in0=ps[:, :], in1=ropet[:, 1, ts_], op=ALU.mult),
                          r=[pk, "rope"], w=[nm + "f"])
                m.dve(lambda e: e.tensor_tensor(out=raw[:], in0=raw[:], in1=ropet[:, 0, :], op=ALU.mult), r=["mraw", "rope"], w=["mraw"])
                m.dve(lambda e, dstf=dstf: e.tensor_tensor(out=dstf[:], in0=dstf[:], in1=raw[:], op=ALU.add), r=["mraw", nm + "f"], w=[nm + "f"])
                m.act(lambda e, dstf=dstf, dstb=dstb: e.copy(out=dstb[:], in_=dstf[:]), r=[nm + "f"], w=[nm + "b"])
            m.dve(lambda e: e.tensor_reduce(out=kmean[:], in_=kf[:].rearrange("p (n k) -> p n k", k=256), axis=AX.X, op=ALU.add),
                  r=["kf"], w=["kmean"])
            m.dve(lambda e: e.tensor_scalar(out=kmean[:], in0=kmean[:], scalar1=1.0 / 256, scalar2=None, op0=ALU.mult), r=["kmean"], w=["kmean"])
            if PADDED:
                m.dma(vb[:], zv[:, :, hr], w=["vb"], eng="pool")
            else:
                m.dma(raw[:], zvT[hr, :], w=["mraw"])
                for g4 in range(4):
                    ps, pk = psr.get()
                    for j in range(4):
                        kc = g4 * 4 + j
                        m.pe(lambda e, ps=ps, j=j, kc=kc: e.transpose(ps[:, j * 128:(j + 1) * 128], raw[:, kc * 128:(kc + 1) * 128], ident), r=["mraw", "cst"], w=[pk])
                    m.act(lambda e, ps=ps, g4=g4: e.copy(out=vb[:, g4 * 4:(g4 + 1) * 4, :], in_=ps[:, :].rearrange("p (a b) -> p a b", b=128)), r=[pk], w=["vb"])
            for qi in range(16):
                s = qi % NB
                blk = qi // 2
                nk = (blk + 1) * 256
                qs = slice(qi * 128, (qi + 1) * 128)
                if blk > 0:
                    ps, pk = psr.get()
                    m.pe(lambda e, ps=ps, qs=qs: e.matmul(ps[:, 0:8], lhsT=qf[:, qs], rhs=kmean[:, :], start=True, stop=True), r=["qf", "kmean"], w=[pk])
                    m.dve(lambda e, s=s: e.memset(gt[s][:], NEG), w=[("gt", s)])
                    m.dve(lambda e, ps=ps, s=s, blk=blk: e.tensor_copy(out=gt[s][:, 0:blk], in_=ps[:, 0:blk]), r=[pk, ("gt", s)], w=[("gt", s)])
                    m.dve(lambda e, s=s: e.max(out=v8[s][:], in_=gt[s][:]), r=[("gt", s)], w=[("v8", s)])
                    m.dve(lambda e, s=s: e.tensor_scalar(out=bias[s][:], in0=gt[s][:], scalar1=v8[s][:, 2:3], scalar2=NEG, op0=ALU.is_lt, op1=ALU.mult),
                          r=[("gt", s), ("v8", s)], w=[("bias", s)])
                for kg in range((nk + 511) // 512):
                    w_ = min(512, nk - kg * 512)
                    ps, pk = psr.get()
                    m.pe(lambda e, ps=ps, qs=qs, kg=kg, w_=w_: e.matmul(ps[:, 0:w_], lhsT=qb[:, qs], rhs=kb[:, kg * 512:kg * 512 + w_], start=True, stop=True),
                         r=["qb", "kb"], w=[pk])
                    for j in range(w_ // 256):
                        n = kg * 2 + j
                        cols = slice(n * 256, (n + 1) * 256)
                        if n < blk:
                            m.act(lambda e, ps=ps, s=s, j=j, n=n, cols=cols: e.activation(out=sS[s][:, cols], in_=ps[:, j * 256:(j + 1) * 256], func=AF.Identity,
                                                                                         scale=SC, bias=bias[s][:, n:n + 1]),
                                  r=[pk, ("bias", s)], w=[("sS", s)])
                        else:
                            m.dve(lambda e, ps=ps, s=s, j=j, cols=cols, qi=qi: e.scalar_tensor_tensor(out=sS[s][:, cols], in0=ps[:, j * 256:(j + 1) * 256], scalar=SC,
                                                                                                      in1=cm[:, qi % 2, :], op0=ALU.mult, op1=ALU.add),
                                  r=[pk, "cm"], w=[("sS", s)])
                m.dve(lambda e, s=s, nk=nk: e.tensor_reduce(out=st[s][:, 0:1], in_=sS[s][:, 0:nk], axis=AX.X, op=ALU.max), r=[("sS", s)], w=[("st", s)])
                m.dve(lambda e, s=s: e.tensor_scalar(out=st[s][:, 1:2], in0=st[s][:, 0:1], scalar1=-1.0, scalar2=None, op0=ALU.mult), r=[("st", s)], w=[("st", s)])
                m.act(lambda e, s=s, nk=nk: e.activation(out=sS[s][:, 0:nk], in_=sS[s][:, 0:nk], func=AF.Exp, bias=st[s][:, 1:2], accum_out=st[s][:, 2:3]),
                      r=[("sS", s), ("st", s)], w=[("sS", s), ("st", s)])
                m.dve(lambda e, s=s: e.reciprocal(out=st[s][:, 3:4], in_=st[s][:, 2:3]), r=[("st", s)], w=[("st", s)])
                m.dve(lambda e, s=s, nk=nk: e.tensor_scalar(out=pB[s][:, 0:nk], in0=sS[s][:, 0:nk], scalar1=st[s][:, 3:4], scalar2=None, op0=ALU.mult),
                      r=[("sS", s), ("st", s)], w=[("pB", s)])
                nkc = nk // 128
                for g0 in range(0, nkc, 8):
                    gn = min(8, nkc - g0)
                    for j in range(gn):
                        m.pe(lambda e, s=s, g0=g0, j=j: e.transpose(psb16[:, j * 128:(j + 1) * 128], pB[s][:, (g0 + j) * 128:(g0 + j + 1) * 128], idb[:]),
                             r=[("pB", s), "idb"], w=["psb16"])
                    eng = m.act if (g0 // 8) % 2 == 0 else m.dve
                    if eng is m.act:
                        m.act(lambda e, s=s, g0=g0, gn=gn: e.copy(out=pT[s][:, g0:g0 + gn, :], in_=psb16[:, 0:gn * 128].rearrange("p (a b) -> p a b", b=128)),
                              r=["psb16"], w=[("pT", s)])
                    else:
                        m.dve(lambda e, s=s, g0=g0, gn=gn: e.tensor_copy(out=pT[s][:, g0:g0 + gn, :], in_=psb16[:, 0:gn * 128].rearrange("p (a b) -> p a b", b=128)),
                              r=["psb16"], w=[("pT", s)])
                ps, pk = psr.get()
                for kc in range(nkc):
                    m.pe(lambda e, ps=ps, s=s, kc=kc, nkc=nkc: e.matmul(ps[:, 0:128], lhsT=vb[:, kc, :], rhs=pT[s][:, kc, :], start=(kc == 0), stop=(kc == nkc - 1)),
                         r=["vb", ("pT", s)], w=[pk])
                m.act(lambda e, ps=ps, qs=qs: e.copy(out=oT[:, qs], in_=ps[:, 0:128]), r=[pk], w=["oT"])
            m.dma(yT_m[hd * 128:(hd + 1) * 128, :], oT[:], r=["oT"])
    m.build()
    return m


def pb_consts():
    c = np.zeros((128, 6, 128), np.float32)
    c[:, 0, :] = np.eye(128)
    bo = np.zeros((128, 128), np.float32); bo[:64, :64] = 1; bo[64:, 64:] = 1
    c[:, 1, :] = bo; c[:, 2, :] = bo / 64
    R = np.zeros((128, 128), np.float32)
    for mm in range(64):
        R[mm + 64, mm] = 1; R[mm, mm + 64] = 1
    c[:, 3, :] = R
    s_ = np.arange(64)[:, None]; t_ = np.arange(64)[None, :]
    c[:64, 4, 0:64] = (s_ < t_); c[:64, 4, 64:128] = (s_ <= t_)
    c[:64, 5, 0:64] = (s_ > t_)
    return c


def rope_tables():
    inv = np.power(10000.0, -np.arange(0, 128, 2, dtype=np.float32) / 128).astype(np.float32)
    ang = np.arange(T, dtype=np.float32)[:, None] * inv[None, :]
    cos = np.cos(ang).T.astype(np.float32); sin = np.sin(ang).T.astype(np.float32)
    r = np.zeros((128, 2, T), np.float32)
    r[:64, 0] = cos; r[64:, 0] = cos
    r[:64, 1] = -sin; r[64:, 1] = sin
    return r


def causal_masks():
    cmk = np.zeros((128, 2, 256), np.float32)
    q = np.arange(128)[:, None]; k = np.arange(256)[None, :]
    cmk[:, 0, :] = np.where(k <= q, 0, NEG)
    cmk[:, 1, :] = np.where(k <= q + 128, 0, NEG)
    return cmk


D = 4096; KC = 32; TT = 512
ALPHA = float(8 ** 0.25)
LN_EPS = 1e-5


def load_mods(m, modb, tab, names=("mod",)):
    mb = m.sb([128, 6, KC], F32, "modb_sb"); tb = m.sb([128, 6, KC], F32, "modt_sb")
    m.dma(mb[:], modb, w=["modb"]); m.dma(tb[:], tab, w=["modt"])
    m.dve(lambda e: e.tensor_tensor(out=mb[:], in0=mb[:], in1=tb[:], op=ALU.add), r=["modb", "modt"], w=["modb"])
    for i in (1, 4):
        m.dve(lambda e, i=i: e.tensor_scalar_add(out=mb[:, i, :], in0=mb[:, i, :], scalar1=1.0), r=["modb"], w=["modb"])
    return mb


def build_p0():
    m = MK()
    cT = m.dram("cT", [128, KC, 4], F32, "ExternalInput")
    W = m.dram("W", [D, 3072], F32, "ExternalInput")
    bvec = m.dram("b", [1, 3072], F32, "ExternalInput")
    out = m.dram("out", [4, 3072], F32, "ExternalOutput")
    Wv = W.rearrange("(kc p) n -> p kc n", p=128)
    ct = m.sb([128, KC, 4], F32, "ct"); m.dma(ct[:], cT, w=["ct"])
    m.act(lambda e: e.activation(out=ct[:], in_=ct[:], func=AF.Silu), r=["ct"], w=["ct"])
    bt = m.sb([1, 3072], F32, "bt"); m.dma(bt[:], bvec, w=["bt"])
    ones = m.sb([1, 4], F32, "ones"); m.dve(lambda e: e.memset(ones[:], 1.0), w=["ones"])
    wr = Ring([m.sb([128, 8, 512], F32, f"w{i}") for i in range(4)], "w")
    pss = Ring([m.ps([128, 512], F32, f"ps{i}") for i in range(2)], "ps")
    ob = m.sb([4, 3072], F32, "ob")
    for g in range(6):
        n0 = g * 512
        ps, pk = pss.get()
        for q in range(4):
            wt, wk = wr.get()
            m.dma(wt[:], Wv[:, q * 8:(q + 1) * 8, n0:n0 + 512], w=[wk])
            for j in range(8):
                kc = q * 8 + j
                m.pe(lambda e, ps=ps, wt=wt, kc=kc, j=j: e.matmul(ps[0:4, :], lhsT=ct[:, kc, :], rhs=wt[:, j, :], start=(kc == 0), stop=False),
                     r=["ct", wk], w=[pk])
        m.pe(lambda e, ps=ps: e.matmul(ps[0:4, :], lhsT=ones[:, :], rhs=bt[:, n0:n0 + 512], start=False, stop=True), r=["ones", "bt"], w=[pk])
        m.dve(lambda e, ps=ps: e.tensor_copy(out=ob[:, n0:n0 + 512], in_=ps[0:4, :]), r=[pk], w=["ob"])
    m.dma(out, ob[:], r=["ob"])
    m.build()
    return m


N_IN = 13024


def build_pa(nc=None, io=None):
    m = MK(nc=nc)
    NT = 1024
    NCH = (N_IN + 127) // 128
    if io is None:
        xT = m.dram("xT", [D, NT], F32, "ExternalInput")
        modb = m.dram("modb", [128, 6, KC], F32, "ExternalInput")
        tab = m.dram("tab", [128, 6, KC], F32, "ExternalInput")
        Wt = m.dram("Wt", [NCH, 128, KC, 128], F32, "ExternalInput")
        zT = m.dram("zT", [NCH * 128, NT], F32, "ExternalOutput")
    else:
        xT = io["xT"]; modb = io["modb"]; tab = io["tab"]; Wt = io["Wt"]; zT = io["zT"]
    mod = load_mods(m, modb, tab)
    hT = m.sb([128, KC, NT], BF16, "hT")
    xr = Ring([m.sb([128, NT], F32, f"xs{i}") for i in range(3)], "xs")
    for kc in range(KC):
        xs, xk = xr.get()
        m.dma(xs[:], xT[kc * 128:(kc + 1) * 128, :], w=[xk])
        m.act(lambda e, xs=xs, kc=kc: e.activation(out=hT[:, kc, :], in_=xs[:], func=AF.Identity, scale=mod[:, 1, kc:kc + 1], bias=mod[:, 0, kc:kc + 1]),
              r=[xk, "modb"], w=[("hT", kc)])
    wr = Ring([m.sb([128, KC, 128], BF16, f"w{i}") for i in range(4)], "w")
    pss = Ring([m.ps([128, 512], F32, f"ps{i}") for i in range(6)], "ps")
    obr = Ring([m.sb([128, NT], F32, f"ob{i}") for i in range(3)], "ob")
    for nch in range(NCH):
        wt, wk = wr.get()
        m.dma(wt[:], Wt[nch], w=[wk], eng="pool")
        ob, ok = obr.get()
        for tt in range(2):
            ps, pk = pss.get()
            for kc in range(KC):
                m.pe(lambda e, ps=ps, wt=wt, kc=kc, tt=tt: e.matmul(ps[:, :], lhsT=wt[:, kc, :], rhs=hT[:, kc, tt * 512:(tt + 1) * 512], start=(kc == 0), stop=(kc == KC - 1)),
                     r=[wk, ("hT", kc)], w=[pk])
            if tt == 0:
                m.dve(lambda e, ps=ps, ob=ob: e.tensor_copy(out=ob[:, 0:512], in_=ps[:, :]), r=[pk], w=[ok])
            else:
                m.act(lambda e, ps=ps, ob=ob: e.copy(out=ob[:, 512:1024], in_=ps[:, :]), r=[pk, ok], w=[ok])
        m.dma(zT[nch * 128:(nch + 1) * 128, :], ob[:], r=[ok])
    m.build()
    return m


def build_pt(kind, nc=None, io=None):
    m = MK(nc=nc)
    even = kind == "even"
    NT = 1024
    HALO = 16
    tok0 = 0 if io is None else io.get("tok0", 0)
    if even:
        NFC = 86
        parts = [(0, 22), (22, 22), (44, 21), (65, 21)]
        plist = [(0, f0, n) for (f0, n) in parts]
    else:
        NFC = 14
        plist = [(e, 0, NFC) for e in range(8)]
    if io is None:
        if even:
            xT = m.dram("xT", [D, NT], F32, "ExternalInput")
            yT = m.dram("yT", [D, NT], F32, "ExternalInput")
            Wo = m.dram("Wo", [32, 128, KC, 128], F32, "ExternalInput")
            Wg = [m.dram("Wg", [NFC, 128, KC, 128], F32, "ExternalInput")]
            Wu = [m.dram("Wu", [NFC, 128, KC, 128], F32, "ExternalInput")]
            Wd = [m.dram("Wd", [32, 128, NFC, 128], F32, "ExternalInput")]
        else:
            xT = m.dram("xT", [D, NT + HALO], F32, "ExternalInput")
            hv = m.dram("hv", [128, 1], F32, "ExternalInput")
            icnt = m.dram("icnt", [128, 4, NT], F32, "ExternalInput")
            Wp = m.dram("Wp", [4, 8, 128, 8, 128], F32, "ExternalInput")
            pscale = m.dram("pscale", [128, KC], F32, "ExternalInput")
            Wr = m.dram("Wr", [128, KC, 8], F32, "ExternalInput")
            rb = m.dram("rb", [1, 8], F32, "ExternalInput")
            Wg = [m.dram(f"Wg{e}", [NFC, 128, KC, 128], F32, "ExternalInput") for e in range(8)]
            Wu = [m.dram(f"Wu{e}", [NFC, 128, KC, 128], F32, "ExternalInput") for e in range(8)]
            Wd = [m.dram(f"Wd{e}", [32, 128, NFC, 128], F32, "ExternalInput") for e in range(8)]
        modb = m.dram("modb", [128, 6, KC], F32, "ExternalInput")
        tab = m.dram("tab", [128, 6, KC], F32, "ExternalInput")
        lng = m.dram("lng", [128, 2, KC], F32, "ExternalInput")
        lnb = m.dram("lnb", [128, 2, KC], F32, "ExternalInput")
        cdram = m.dram("cst", [128, 2, 128], F32, "ExternalInput")
        outT = m.dram("outT", [D, NT], F32, "ExternalOutput")
    else:
        xT = io["xT"]; modb = io["modb"]; tab = io["tab"]; lng = io["lng"]; lnb = io["lnb"]; cdram = io["cst"]; outT = io["outT"]
        Wg = io["Wg"]; Wu = io["Wu"]; Wd = io["Wd"]
        if even:
            yT = io["yT"]; Wo = io["Wo"]
        else:
            icnt = io["icnt"]; Wp = io["Wp"]; pscale = io["pscale"]; Wr = io["Wr"]; rb = io["rb"]; xfull = io["xfull"]; hv = None

    mod = load_mods(m, modb, tab)
    lg = m.sb([128, 2, KC], F32, "lg"); m.dma(lg[:], lng, w=["lg"])
    lb = m.sb([128, 2, KC], F32, "lb"); m.dma(lb[:], lnb, w=["lb"])
    cst = m.sb([128, 2, 128], F32, "cst_sb"); m.dma(cst[:], cdram, w=["cst"])
    onesD = cst[:, 0, :]; ident = cst[:, 1, :]
    ones = m.sb([128, 128], F32, "ones"); m.dve(lambda e: e.memset(ones[:], 1.0), w=["ones"])

    X = m.sb([128, KC, TT], F32, "X")
    hT = m.sb([128, KC, TT], BF16, "hT")
    wr = Ring([m.sb([128, KC, 128], BF16, f"w{i}") for i in range(4 if even else 3)], "w")
    wdr = Ring([m.sb([128, 22 if even else 14, 128], BF16, f"wd{i}") for i in range(3)], "wd")
    pss = Ring([m.ps([128, 512], F32, f"ps{i}") for i in range(8)], "ps")
    tr = Ring([m.sb([128, TT], F32, f"tmp{i}") for i in range(4)], "tmp")
    act = m.sb([128, 22 if even else 14, TT], BF16, "act")
    stat = m.sb([128, 4, TT], F32, "stat")

    if not even:
        hvt = m.sb([128, 1], F32, "hvt")
        if hv is not None:
            m.dma(hvt[:], hv, w=["hvt"])
        ps_t = m.sb([128, KC], F32, "pst"); m.dma(ps_t[:], pscale, w=["pst"])
        m.dve(lambda e: e.tensor_tensor(out=ps_t[:], in0=ps_t[:], in1=mod[:, 2, :], op=ALU.mult), r=["pst", "modb"], w=["pst"])
        wrt = m.sb([128, KC, 8], F32, "wrt"); m.dma(wrt[:], Wr, w=["wrt"])
        wr1 = m.sb([128, KC, 8], F32, "wr1"); wr2 = m.sb([128, KC, 8], F32, "wr2")
        for kc in range(KC):
            m.dve(lambda e, kc=kc: e.tensor_scalar(out=wr1[:, kc, :], in0=wrt[:, kc, :], scalar1=mod[:, 4, kc:kc + 1], scalar2=None, op0=ALU.mult),
                  r=["wrt", "modb"], w=["wr1"])
            m.dve(lambda e, kc=kc: e.tensor_scalar(out=wr2[:, kc, :], in0=wrt[:, kc, :], scalar1=mod[:, 3, kc:kc + 1], scalar2=None, op0=ALU.mult),
                  r=["wrt", "modb"], w=["wr2"])
        rbt = m.sb([1, 8], F32, "rbt"); m.dma(rbt[:], rb, w=["rbt"])
        bc = m.sb([128, 8, TT], F32, "bc")
        sm = {n: m.sb([128, 8], F32, "sm_" + n) for n in ("lg", "v8", "c1", "c2", "g")}
        dg = Ring([m.sb([128, 128], F32, f"dg{i}") for i in range(2)], "dg")
        hp = Ring([m.sb([128, TT + HALO], F32, f"hp{i}") for i in range(2)], "hp")
        hq = Ring([m.sb([128, TT + HALO], F32, f"hq{i}") for i in range(2)], "hq")
        hs_ = Ring([m.sb([128, TT + HALO], F32, f"hs{i}") for i in range(3)], "hs")
        ic = m.sb([128, 4, TT], F32, "ic")

    def layer_norm(li, gate_next):
        ps1, k1 = pss.get(); ps2, k2 = pss.get()
        for kc in range(KC):
            m.pe(lambda e, kc=kc: e.matmul(ps1[:, :], lhsT=onesD, rhs=X[:, kc, :], start=(kc == 0), stop=(kc == KC - 1)), r=["cst", ("X", kc)], w=[k1])
        for kc in range(KC):
            t, tk = tr.get()
            m.act(lambda e, t=t, kc=kc: e.activation(out=t[:], in_=X[:, kc, :], func=AF.Square), r=[("X", kc)], w=[tk])
            m.pe(lambda e, t=t, kc=kc: e.matmul(ps2[:, :], lhsT=onesD, rhs=t[:], start=(kc == 0), stop=(kc == KC - 1)), r=["cst", tk], w=[k2])
        m.dve(lambda e: e.tensor_copy(out=stat[:, 0, :], in_=ps1[:, :]), r=[k1], w=["stat"])
        m.dve(lambda e: e.tensor_tensor(out=stat[:, 1, :], in0=stat[:, 0, :], in1=stat[:, 0, :], op=ALU.mult), r=["stat"], w=["stat"])
        m.dve(lambda e: e.tensor_tensor(out=stat[:, 1, :], in0=ps2[:, :], in1=stat[:, 1, :], op=ALU.subtract), r=[k2, "stat"], w=["stat"])
        m.dve(lambda e: e.tensor_scalar(out=stat[:, 1, :], in0=stat[:, 1, :], scalar1=LN_EPS, scalar2=None, op0=ALU.add), r=["stat"], w=["stat"])
        m.act(lambda e: e.activation(out=stat[:, 1, :], in_=stat[:, 1, :], func=AF.Ln), r=["stat"], w=["stat"])
        m.act(lambda e: e.activation(out=stat[:, 2, :], in_=stat[:, 1, :], func=AF.Exp, scale=-0.5), r=["stat"], w=["stat"])
        m.dve(lambda e: e.scalar_tensor_tensor(out=stat[:, 3, :], in0=stat[:, 0, :], scalar=-1.0, in1=stat[:, 2, :], op0=ALU.mult, op1=ALU.mult),
              r=["stat"], w=["stat"])
        for kc in range(KC):
            m.dve(lambda e, kc=kc: e.tensor_tensor(out=X[:, kc, :], in0=X[:, kc, :], in1=stat[:, 2, :], op=ALU.mult), r=[("X", kc), "stat"], w=[("X", kc)])
            m.dve(lambda e, kc=kc: e.tensor_tensor(out=X[:, kc, :], in0=X[:, kc, :], in1=stat[:, 3, :], op=ALU.add), r=[("X", kc), "stat"], w=[("X", kc)])
            m.act(lambda e, kc=kc: e.activation(out=X[:, kc, :], in_=X[:, kc, :], func=AF.Identity, scale=lg[:, li, kc:kc + 1], bias=lb[:, li, kc:kc + 1]),
                  r=[("X", kc), "lg", "lb"], w=[("X", kc)])
            if gate_next:
                m.act(lambda e, kc=kc: e.activation(out=hT[:, kc, :], in_=X[:, kc, :], func=AF.Identity, scale=mod[:, 4, kc:kc + 1], bias=mod[:, 3, kc:kc + 1]),
                      r=[("X", kc), "modb"], w=[("hT", kc)])

    for ti in range(2):
        c0 = ti * TT
        if even:
            for kc in range(KC):
                m.dma(hT[:, kc, :], yT[kc * 128:(kc + 1) * 128, c0:c0 + TT], w=[("hT", kc)], eng="pool")
                m.dma(X[:, kc, :], xT[kc * 128:(kc + 1) * 128, c0:c0 + TT], w=[("X", kc)])
            for dc in range(KC):
                wt, wk = wr.get()
                m.dma(wt[:], Wo[dc], w=[wk], eng="pool")
                ps, pk = pss.get()
                for kc in range(KC):
                    m.pe(lambda e, ps=ps, wt=wt, kc=kc: e.matmul(ps[:, :], lhsT=wt[:, kc, :], rhs=hT[:, kc, :], start=(kc == 0), stop=(kc == KC - 1)),
                         r=[wk, ("hT", kc)], w=[pk])
                m.act(lambda e, dc=dc: e.mul(out=X[:, dc, :], in_=X[:, dc, :], mul=ALPHA), r=[("X", dc)], w=[("X", dc)])
                m.dve(lambda e, ps=ps, dc=dc: e.scalar_tensor_tensor(out=X[:, dc, :], in0=ps[:, :], scalar=mod[:, 2, dc:dc + 1], in1=X[:, dc, :], op0=ALU.mult, op1=ALU.add),
                      r=[pk, ("X", dc), "modb"], w=[("X", dc)])
        else:
            m.dma(ic[:], icnt[:, :, c0:c0 + TT], w=["ic"])
            for kc in range(KC):
                g = kc // 8
                xh, xk = hp.get()
                g0 = tok0 + c0
                if io is None:
                    m.dma(xh[:], xT[kc * 128:(kc + 1) * 128, c0:c0 + TT + HALO], w=[xk])
                elif g0 == 0:
                    m.dve(lambda e, xh=xh: e.memset(xh[:, 0:HALO], 0.0), w=[xk])
                    m.dma(xh[:, HALO:], xfull[kc * 128:(kc + 1) * 128, 0:TT], r=[xk], w=[xk])
                else:
                    m.dma(xh[:], xfull[kc * 128:(kc + 1) * 128, g0 - HALO:g0 + TT], w=[xk])
                m.dve(lambda e, xh=xh, kc=kc: e.tensor_copy(out=X[:, kc, :], in_=xh[:, HALO:]), r=[xk], w=[("X", kc)])
                h, hk = hq.get()
                m.act(lambda e, xh=xh, h=h, kc=kc: e.activation(out=h[:], in_=xh[:], func=AF.Identity, scale=mod[:, 1, kc:kc + 1], bias=mod[:, 0, kc:kc + 1]),
                      r=[xk, "modb"], w=[hk])
                if io is None and ti == 0:
                    m.dve(lambda e, h=h: e.tensor_scalar(out=h[:, 0:HALO], in0=h[:, 0:HALO], scalar1=hvt[:, 0:1], scalar2=None, op0=ALU.mult),
                          r=[hk, "hvt"], w=[hk])
                elif io is not None and g0 == 0:
                    m.dve(lambda e, h=h: e.memset(h[:, 0:HALO], 0.0), r=[hk], w=[hk])
                s, sk = h, hk
                for st in range(g + 1):
                    step = 1 << st
                    lo = 2 * step - 1
                    s2, s2k = hs_.get()
                    m.dve(lambda e, s=s, s2=s2, step=step, lo=lo: e.tensor_tensor(out=s2[:, lo:], in0=s[:, lo:], in1=s[:, lo - step:TT + HALO - step], op=ALU.add),
                          r=[sk], w=[s2k])
                    s, sk = s2, s2k
                t, tk = tr.get()
                m.dve(lambda e, s=s, t=t, g=g: e.tensor_tensor(out=t[:], in0=s[:, HALO:], in1=ic[:, g, :], op=ALU.mult), r=[sk, "ic"], w=[tk])
                m.dve(lambda e, t=t, h=h, kc=kc: e.tensor_tensor(out=hT[:, kc, :], in0=t[:], in1=h[:, HALO:], op=ALU.subtract), r=[tk, hk], w=[("hT", kc)])
            for dc in range(KC):
                g, ec = dc // 8, dc % 8
                wt, wk = wr.get()
                m.dma(wt[:, 0:8, :], Wp[g, ec], w=[wk], eng="pool")
                ps, pk = pss.get()
                for cc in range(8):
                    m.pe(lambda e, ps=ps, wt=wt, cc=cc, g=g: e.matmul(ps[:, :], lhsT=wt[:, cc, :], rhs=hT[:, g * 8 + cc, :], start=(cc == 0), stop=(cc == 7)),
                         r=[wk, ("hT", g * 8 + cc)], w=[pk])
                m.act(lambda e, dc=dc: e.mul(out=X[:, dc, :], in_=X[:, dc, :], mul=ALPHA), r=[("X", dc)], w=[("X", dc)])
                m.dve(lambda e, ps=ps, dc=dc: e.scalar_tensor_tensor(out=X[:, dc, :], in0=ps[:, :], scalar=ps_t[:, dc:dc + 1], in1=X[:, dc, :], op0=ALU.mult, op1=ALU.add),
                      r=[pk, ("X", dc), "pst"], w=[("X", dc)])
        layer_norm(0, True)
        if not even:
            for tc_ in range(4):
                tsl = slice(tc_ * 128, (tc_ + 1) * 128)
                ps, pk = pss.get()
                for kc in range(KC):
                    m.pe(lambda e, ps=ps, kc=kc, tsl=tsl: e.matmul(ps[:, 0:8], lhsT=X[:, kc, tsl], rhs=wr1[:, kc, :], start=(kc == 0), stop=False),
                         r=[("X", kc), "wr1"], w=[pk])
                for kc in range(KC):
                    m.pe(lambda e, ps=ps, kc=kc: e.matmul(ps[:, 0:8], lhsT=ones[:, :], rhs=wr2[:, kc, :], start=False, stop=False), r=["ones", "wr2"], w=[pk])
                m.pe(lambda e, ps=ps: e.matmul(ps[:, 0:8], lhsT=ones[0:1, :], rhs=rbt[:, :], start=False, stop=True), r=["ones", "rbt"], w=[pk])
                L_ = sm["lg"]; V8 = sm["v8"]; C1 = sm["c1"]; C2 = sm["c2"]; G = sm["g"]
                m.dve(lambda e, ps=ps: e.tensor_copy(out=L_[:], in_=ps[:, 0:8]), r=[pk], w=["sm_lg"])
                m.dve(lambda e: e.max(out=V8[:], in_=L_[:]), r=["sm_lg"], w=["sm_v8"])
                m.dve(lambda e: e.tensor_tensor(out=G[:, 0:1], in0=V8[:, 0:1], in1=V8[:, 1:2], op=ALU.subtract), r=["sm_v8"], w=["sm_g"])
                m.act(lambda e: e.activation(out=G[:, 1:2], in_=G[:, 0:1], func=AF.Sigmoid), r=["sm_g"], w=["sm_g"])
                m.act(lambda e: e.activation(out=G[:, 2:3], in_=G[:, 0:1], func=AF.Sigmoid, scale=-1.0), r=["sm_g"], w=["sm_g"])
                m.dve(lambda e: e.tensor_scalar(out=C1[:], in0=L_[:], scalar1=V8[:, 0:1], scalar2=G[:, 1:2], op0=ALU.is_equal, op1=ALU.mult),
                      r=["sm_lg", "sm_v8", "sm_g"], w=["sm_c1"])
                m.dve(lambda e: e.tensor_scalar(out=C2[:], in0=L_[:], scalar1=V8[:, 1:2], scalar2=G[:, 2:3], op0=ALU.is_equal, op1=ALU.mult),
                      r=["sm_lg", "sm_v8", "sm_g"], w=["sm_c2"])
                m.dve(lambda e: e.tensor_tensor(out=C1[:], in0=C1[:], in1=C2[:], op=ALU.add), r=["sm_c1", "sm_c2"], w=["sm_c1"])
                for ex in range(8):
                    dt_, dk = dg.get()
                    m.dve(lambda e, dt_=dt_, ex=ex: e.tensor_scalar(out=dt_[:], in0=ident, scalar1=C1[:, ex:ex + 1], scalar2=None, op0=ALU.mult),
                          r=["cst", "sm_c1"], w=[dk])
                    ps2, pk2 = pss.get()
                    m.pe(lambda e, ps2=ps2, dt_=dt_: e.matmul(ps2[:, 0:128], lhsT=ones[:, :], rhs=dt_[:], start=True, stop=True), r=["ones", dk], w=[pk2])
                    m.act(lambda e, ps2=ps2, ex=ex, tsl=tsl: e.copy(out=bc[:, ex, tsl], in_=ps2[:, 0:128]), r=[pk2], w=[("bc", ex)])
        for kc in range(KC):
            m.act(lambda e, kc=kc: e.mul(out=X[:, kc, :], in_=X[:, kc, :], mul=ALPHA), r=[("X", kc)], w=[("X", kc)])
        for (ex, f0, nf) in plist:
            for fi in range(nf):
                fc = f0 + fi
                wg, wgk = wr.get()
                m.dma(wg[:], Wg[ex][fc], w=[wgk], eng="pool")
                wu, wuk = wr.get()
                m.dma(wu[:], Wu[ex][fc], w=[wuk], eng="pool")
                psg, pgk = pss.get(); psu, puk = pss.get()
                for kc in range(KC):
                    m.pe(lambda e, psg=psg, wg=wg, kc=kc: e.matmul(psg[:, :], lhsT=wg[:, kc, :], rhs=hT[:, kc, :], start=(kc == 0), stop=(kc == KC - 1)),
                         r=[wgk, ("hT", kc)], w=[pgk])
                for kc in range(KC):
                    m.pe(lambda e, psu=psu, wu=wu, kc=kc: e.matmul(psu[:, :], lhsT=wu[:, kc, :], rhs=hT[:, kc, :], start=(kc == 0), stop=(kc == KC - 1)),
                         r=[wuk, ("hT", kc)], w=[puk])
                t, tk = tr.get()
                m.act(lambda e, t=t, psg=psg: e.activation(out=t[:], in_=psg[:, :], func=AF.Silu), r=[pgk], w=[tk])
                if even:
                    m.dve(lambda e, t=t, psu=psu, fi=fi: e.tensor_tensor(out=act[:, fi, :], in0=t[:], in1=psu[:, :], op=ALU.mult), r=[tk, puk], w=[("act", fi)])
                else:
                    m.dve(lambda e, t=t, psu=psu: e.tensor_tensor(out=t[:], in0=t[:], in1=psu[:, :], op=ALU.mult), r=[tk, puk], w=[tk])
                    m.dve(lambda e, t=t, fi=fi, ex=ex: e.tensor_tensor(out=act[:, fi, :], in0=t[:], in1=bc[:, ex, :], op=ALU.mult), r=[tk, ("bc", ex)], w=[("act", fi)])
            for dc in range(KC):
                wd, wdk = wdr.get()
                m.dma(wd[:, 0:nf, :], Wd[ex][dc, :, f0:f0 + nf, :], w=[wdk], eng="pool")
                ps, pk = pss.get()
                for fi in range(nf):
                    m.pe(lambda e, ps=ps, wd=wd, fi=fi, nf=nf: e.matmul(ps[:, :], lhsT=wd[:, fi, :], rhs=act[:, fi, :], start=(fi == 0), stop=(fi == nf - 1)),
                         r=[wdk, ("act", fi)], w=[pk])
                m.dve(lambda e, ps=ps, dc=dc: e.scalar_tensor_tensor(out=X[:, dc, :], in0=ps[:, :], scalar=mod[:, 5, dc:dc + 1], in1=X[:, dc, :], op0=ALU.mult, op1=ALU.add),
                      r=[pk, ("X", dc), "modb"], w=[("X", dc)])
        layer_norm(1, False)
        for kc in range(KC):
            m.dma(outT[kc * 128:(kc + 1) * 128, c0:c0 + TT], X[:, kc, :], r=[("X", kc)])
    m.build()
    return m


def build_p0f(nc, io):
    m = MK(nc=nc)
    cT = io["cT"]; W = io["ada_w"]; bT = io["bT"]; oh = io["oh"]; out = io["modbase"]
    Wv = W.rearrange("(kc p) n -> p kc n", p=128)
    ct = m.sb([128, KC, 4], F32, "ct"); m.dma(ct[:], cT, w=["ct"])
    m.act(lambda e: e.activation(out=ct[:], in_=ct[:], func=AF.Silu), r=["ct"], w=["ct"])
    bt = m.sb([128, 192], F32, "bt"); m.dma(bt[:], bT, w=["bt"])
    oht = m.sb([128, 4], F32, "oht"); m.dma(oht[:], oh, w=["oht"])
    wr = Ring([m.sb([128, 8, 512], F32, f"w{i}") for i in range(8)], "w")
    pss = Ring([m.ps([128, 512], F32, f"ps{i}") for i in range(4)], "ps")
    baseT = m.sb([128, 192, 4], F32, "baseT")
    for g in range(48):
        n0 = g * 512
        tiles = []
        for q in range(4):
            wt, wk = wr.get()
            m.dma(wt[:], Wv[:, q * 8:(q + 1) * 8, n0:n0 + 512], w=[wk])
            tiles.append((wt, wk))
        ps, pk = pss.get()
        for j in range(4):
            for kc in range(KC):
                wt, wk = tiles[kc // 8]
                m.pe(lambda e, ps=ps, wt=wt, kc=kc, j=j: e.matmul(ps[:, j * 4:(j + 1) * 4], lhsT=wt[:, kc % 8, j * 128:(j + 1) * 128], rhs=ct[:, kc, :],
                                                                 start=(kc == 0), stop=(kc == KC - 1)), r=["ct", wk], w=[pk])
        m.dve(lambda e, ps=ps, g=g: e.tensor_copy(out=baseT[:, g * 4:(g + 1) * 4, :], in_=ps[:, 0:16].rearrange("p (a b) -> p a b", b=4)), r=[pk], w=["baseT"])
    mb = m.sb([128, 192], F32, "mb")
    m.dve(lambda e: e.tensor_scalar(out=mb[:], in0=baseT[:, :, 0], scalar1=oht[:, 0:1], scalar2=None, op0=ALU.mult), r=["baseT", "oht"], w=["mb"])
    for b in range(1, 4):
        m.dve(lambda e, b=b: e.scalar_tensor_tensor(out=mb[:], in0=baseT[:, :, b], scalar=oht[:, b:b + 1], in1=mb[:], op0=ALU.mult, op1=ALU.add),
              r=["baseT", "oht", "mb"], w=["mb"])
    m.dve(lambda e: e.tensor_tensor(out=mb[:], in0=mb[:], in1=bt[:], op=ALU.add), r=["mb", "bt"], w=["mb"])
    m.dma(out, mb[:], r=["mb"])
    m.build()
    return m


SEQ = 2048


def build_fused():
    nc = bass.Bass("TRN2", target_bir_lowering=False)

    def dr(name, shape, kind="ExternalInput"):
        return nc.dram_tensor(name, list(shape), F32, kind=kind).ap()

    xin = dr("xT", [D, SEQ]); out = dr("outT", [D, SEQ], "ExternalOutput")
    cT = dr("cT", [128, KC, 4]); ada_w = dr("ada_w", [D, 6 * D]); bT = dr("bT", [128, 192]); oh = dr("oh", [128, 4])
    tabs = dr("tabs", [4, 128, 6, KC]); lngs = dr("lngs", [4, 128, 2, KC]); lnbs = dr("lnbs", [4, 128, 2, KC])
    cstT = dr("cstT", [128, 2, 128]); pbc = dr("pbc", [128, 6, 128]); rope = dr("rope", [128, 2, SEQ]); cmask = dr("cmask", [128, 2, 256])
    identb = dr("identb", [128, 128]); icnt = dr("icnt", [128, 4, SEQ])
    ev = []
    for i in range(2):
        ev.append(dict(Wt=dr(f"Wt{i}", [102, 128, KC, 128]), mu_rkv=dr(f"mu_rkv{i}", [2, 128, 24]), mu_l=dr(f"mu_l{i}", [128, 6]),
                       w2=dr(f"w2_{i}", [2, 128, 1024]), a2=dr(f"a2_{i}", [2, 128, 1024]), g2=dr(f"g2_{i}", [2, 128, 4, 1024]), pp=dr(f"pp{i}", [2, 128, 8, 8]),
                       Wo=dr(f"Wo{i}", [32, 128, KC, 128]), Wg=dr(f"Wg{i}", [86, 128, KC, 128]), Wu=dr(f"Wu{i}", [86, 128, KC, 128]), Wd=dr(f"Wd{i}", [32, 128, 86, 128])))
    od = []
    for i in range(2):
        od.append(dict(Wp=dr(f"Wp{i}", [4, 8, 128, 8, 128]), pscale=dr(f"pscale{i}", [128, KC]), Wr=dr(f"Wr{i}", [128, KC, 8]), rb=dr(f"rb{i}", [1, 8]),
                       Wg=dr(f"mWg{i}", [8, 14, 128, KC, 128]), Wu=dr(f"mWu{i}", [8, 14, 128, KC, 128]), Wd=dr(f"mWd{i}", [8, 32, 128, 14, 128])))
    modbase = dr("modbase", [128, 192], "Internal")
    xbuf = [dr("xbufA", [D, SEQ], "Internal"), dr("xbufB", [D, SEQ], "Internal")]
    zT = dr("zT_i", [102 * 128, SEQ], "Internal")
    yfull = dr("yfull", [D, SEQ], "Internal")
    modb = modbase.rearrange("p (i k) -> p i k", k=KC)

    build_p0f(nc, dict(cT=cT, ada_w=ada_w, bT=bT, oh=oh, modbase=modbase))
    src = xin
    for l in range(4):
        i = l // 2
        dst = out if l == 3 else xbuf[l % 2]
        if l % 2 == 0:
            E = ev[i]
            for th in range(2):
                ts_ = slice(th * 1024, (th + 1) * 1024)
                build_pa(nc=nc, io=dict(xT=src[:, ts_], modb=modb, tab=tabs[l], Wt=E["Wt"], zT=zT[:, ts_]))
            z3 = zT[0:6144, :].rearrange("(s c) t -> s c t", s=3)
            for hh in range(2):
                hs = slice(hh * 1024, (hh + 1) * 1024)
                q0 = 6880 + hh * 1024
                pbio = dict(zrkv=z3[:, hs, :], zl=zT[6144:6912, :], mu_rkv=E["mu_rkv"][hh], mu_l=E["mu_l"], w2=E["w2"][hh], a2=E["a2"][hh],
                                        g2=E["g2"][hh], pp=E["pp"][hh], consts=pbc, zq=zT[q0:q0 + 1024, :], zkm=zT[q0 + 2048:q0 + 3072, :],
                                        zvT=zT[q0 + 4096:q0 + 5120, :], rope=rope, cmask=cmask, identb=identb,
                                        yT_r=yfull[hh * 1024:(hh + 1) * 1024, :], yT_m=yfull[2048 + hh * 1024:2048 + (hh + 1) * 1024, :])
                build_pb(do_rwkv=True, do_moba=False, nc=nc, io=pbio)
                build_pb(do_rwkv=False, do_moba=True, nc=nc, io=pbio)
            for th in range(2):
                ts_ = slice(th * 1024, (th + 1) * 1024)
                build_pt("even", nc=nc, io=dict(xT=src[:, ts_], yT=yfull[:, ts_], Wo=E["Wo"], Wg=[E["Wg"]], Wu=[E["Wu"]], Wd=[E["Wd"]], modb=modb, tab=tabs[l],
                                                lng=lngs[l], lnb=lnbs[l], cst=cstT, outT=dst[:, ts_]))
        else:
            O = od[i]
            for th in range(2):
                ts_ = slice(th * 1024, (th + 1) * 1024)
                build_pt("odd", nc=nc, io=dict(xT=None, xfull=src, tok0=th * 1024, icnt=icnt[:, :, ts_], Wp=O["Wp"], pscale=O["pscale"], Wr=O["Wr"], rb=O["rb"],
                                               Wg=[O["Wg"][e] for e in range(8)], Wu=[O["Wu"][e] for e in range(8)], Wd=[O["Wd"][e] for e in range(8)],
                                               modb=modb, tab=tabs[l], lng=lngs[l], lnb=lnbs[l], cst=cstT, outT=dst[:, ts_]))
        src = dst
    return nc


_NC = None


def _c(a):
    return np.ascontiguousarray(a, dtype=np.float32)


def _tile_in(W, nch):
    N = W.shape[1]
    if N < nch * 128:
        W = np.concatenate([W, np.zeros((W.shape[0], nch * 128 - N), np.float32)], 1)
    return _c(W.reshape(32, 128, nch, 128).transpose(2, 1, 0, 3))


def _tile_down(W, nfc):
    return _c(W.reshape(nfc, 128, 32, 128).transpose(2, 1, 0, 3))


def kernel(**inp):
    global _NC
    inp = {k: np.asarray(v) for k, v in inp.items()}
    x = inp["x"].astype(np.float32)
    Bn, Tn, Dn = x.shape
    sh = {}
    sh["cT"] = _c(inp["c"].T.reshape(32, 128, 4).transpose(1, 0, 2))
    sh["ada_w"] = _c(inp["ada_w"])
    sh["bT"] = _c(inp["ada_b"].reshape(192, 128).T)
    sh["tabs"] = _c(inp["ada_table"].reshape(4, 6, 32, 128).transpose(0, 3, 1, 2))
    sh["lngs"] = _c(inp["ln_g"].reshape(4, 2, 32, 128).transpose(0, 3, 1, 2))
    sh["lnbs"] = _c(inp["ln_b"].reshape(4, 2, 32, 128).transpose(0, 3, 1, 2))
    cstT = np.zeros((128, 2, 128), np.float32); cstT[:, 0, :] = 1.0 / Dn; cstT[:, 1, :] = np.eye(128)
    sh["cstT"] = cstT; sh["pbc"] = pb_consts(); sh["rope"] = rope_tables(); sh["cmask"] = causal_masks(); sh["identb"] = np.eye(128, dtype=np.float32)
    tpos = np.arange(Tn, dtype=np.float32) + 1.0
    ic = np.stack([1.0 / np.minimum(tpos, float(w)) for w in (2, 4, 8, 16)]).astype(np.float32)
    sh["icnt"] = _c(np.broadcast_to(ic[None], (128, 4, Tn)))
    DA = 2048
    for i in range(2):
        sh[f"Wt{i}"] = _tile_in(inp["mix_w_in"][i], 102)
        mu = inp["mix_mu"][i]
        sh[f"mu_rkv{i}"] = _c(np.stack([np.concatenate([mu[s * DA + hh * 1024: s * DA + hh * 1024 + 1024].reshape(8, 128).T for s in range(3)], 1) for hh in range(2)]))
        mul = np.zeros(768, np.float32); mul[:736] = mu[3 * DA:3 * DA + 736]
        sh[f"mu_l{i}"] = _c(mul.reshape(6, 128).T)
        sh[f"w2_{i}"] = _c(np.stack([inp["rwkv_w2"][i][:, hh * 1024:(hh + 1) * 1024] for hh in range(2)]))
        sh[f"a2_{i}"] = _c(np.stack([inp["rwkv_a2"][i][:, hh * 1024:(hh + 1) * 1024] for hh in range(2)]))
        g2p = np.zeros((512, DA), np.float32); g2p[:480] = inp["rwkv_g2"][i]
        sh[f"g2_{i}"] = _c(np.stack([g2p[:, hh * 1024:(hh + 1) * 1024].reshape(4, 128, 1024).transpose(1, 0, 2) for hh in range(2)]))
        pv = lambda v, hh: np.asarray(v).reshape(-1)[hh * 1024:(hh + 1) * 1024].reshape(8, 128).T
        sh[f"pp{i}"] = _c(np.stack([np.stack([pv(inp["rwkv_w0"][i], hh), pv(inp["rwkv_a0"][i], hh), pv(inp["rwkv_kk"][i], hh), pv(inp["rwkv_ka"][i], hh),
                                             pv(inp["rwkv_rk"][i], hh), pv(inp["rwkv_lnx_g"][i], hh), pv(inp["rwkv_lnx_b"][i], hh),
                                             np.ones((128, 8), np.float32)], 1) for hh in range(2)]))
        sh[f"Wo{i}"] = _tile_in(inp["mix_w_out"][i], 32)
        sh[f"Wg{i}"] = _tile_in(inp["ffn_w_gate"][i], 86); sh[f"Wu{i}"] = _tile_in(inp["ffn_w_up"][i], 86); sh[f"Wd{i}"] = _tile_down(inp["ffn_w_down"][i], 86)
        sh[f"Wp{i}"] = _c(inp["pool_w"][i].reshape(4, 8, 128, 8, 128).transpose(0, 3, 2, 1, 4))
        sh[f"pscale{i}"] = _c(inp["pool_scale"][i].reshape(32, 128).T)
        sh[f"Wr{i}"] = _c(inp["moe_router_w"][i].reshape(32, 128, 8).transpose(1, 0, 2)); sh[f"rb{i}"] = _c(inp["moe_router_b"][i][None, :])
        sh[f"mWg{i}"] = np.stack([_tile_in(inp["moe_w_gate"][i, e], 14) for e in range(8)])
        sh[f"mWu{i}"] = np.stack([_tile_in(inp["moe_w_up"][i, e], 14) for e in range(8)])
        sh[f"mWd{i}"] = np.stack([_tile_down(inp["moe_w_down"][i, e], 14) for e in range(8)])
    if _NC is None:
        _NC = build_fused()
    maps = []
    for b in range(Bn):
        d = dict(sh)
        d["xT"] = _c(x[b].T)
        ohb = np.zeros((128, 4), np.float32); ohb[:, b] = 1.0
        d["oh"] = ohb
        maps.append(d)
    res = run_bass_kernel_spmd(_NC, maps, core_ids=list(range(Bn))).results
    return np.stack([res[b]["outT"].T for b in range(Bn)]).astype(np.float32)
```

```python
import numpy as np
from contextlib import ExitStack
import concourse.bass as bass
import concourse.mybir as mybir
from concourse.bass_utils import run_bass_kernel_spmd

F32 = mybir.dt.float32
BF16 = mybir.dt.bfloat16
AF = mybir.ActivationFunctionType
ALU = mybir.AluOpType
AX = mybir.AxisListType


class _Op:
    __slots__ = ("eng", "fn", "deps", "inc", "sem", "val", "dma", "idx", "waits", "call")


class _Rec:
    def __init__(self):
        self.call = None

    def __getattr__(self, name):
        def f(*a, **k):
            self.call = (name, a, k)
        return f


class MK:
    BLK = {"pe": "tensor", "act": "scalar", "dve": "vector", "pool": "gpsimd", "sp": "sync"}
    EPOCH = 4000
    NSLOT = 12

    _uid = 0

    def __init__(self, inorder=("pe",), nc=None):
        MK._uid += 1
        self.pfx = "" if nc is None else f"k{MK._uid}_"
        self.nc = nc if nc is not None else bass.Bass("TRN2", target_bir_lowering=False)
        self.st = ExitStack()
        self.ops = []
        self.lastw = {}
        self.readers = {}
        self.nname = 0
        self.inorder = set(inorder)

    def dram(self, name, shape, dt, kind, addr_space=None):
        if addr_space is not None:
            return self.nc.dram_tensor(name, list(shape), dt, kind=kind, addr_space=addr_space).ap()
        return self.nc.dram_tensor(name, list(shape), dt, kind=kind).ap()

    def sb(self, shape, dt, name=None):
        self.nname += 1
        return self.st.enter_context(self.nc.sbuf_tensor(self.pfx + (name or f"sb{self.nname}"), list(shape), dt))

    def ps(self, shape, dt=F32, name=None):
        self.nname += 1
        return self.st.enter_context(self.nc.psum_tensor(self.pfx + (name or f"ps{self.nname}"), list(shape), dt))

    def add(self, eng, fn, r=(), w=(), dma=False):
        op = _Op()
        op.eng, op.fn, op.dma, op.inc = eng, fn, dma, dma
        op.idx = len(self.ops)
        rec = _Rec(); fn(rec); op.call = rec.call
        assert op.call is not None
        op.sem = op.val = None
        isps = lambda k: (isinstance(k, tuple) and str(k[0]).startswith("ps")) or (isinstance(k, str) and k.startswith("ps"))
        w = list(w) + [k for k in r if isps(k) and k not in w]
        deps = set()
        for k in r:
            if k in self.lastw:
                deps.add(self.lastw[k])
        for k in w:
            if k in self.lastw:
                deps.add(self.lastw[k])
            for rd in self.readers.get(k, {}).values():
                deps.add(rd)
        deps.discard(op.idx)
        op.deps = deps
        for k in w:
            self.lastw[k] = op.idx
            self.readers[k] = {}
        for k in r:
            d = self.readers.setdefault(k, {})
            d[(eng, op.idx) if dma else eng] = op.idx
        self.ops.append(op)
        return op

    def pe(self, fn, r=(), w=()): return self.add("pe", fn, r, w)
    def act(self, fn, r=(), w=()): return self.add("act", fn, r, w)
    def dve(self, fn, r=(), w=()): return self.add("dve", fn, r, w)
    def pool(self, fn, r=(), w=()): return self.add("pool", fn, r, w)
    def dma(self, out, in_, r=(), w=(), eng="sp", **kw):
        return self.add(eng, lambda e: e.dma_start(out=out, in_=in_, **kw), r, w, dma=True)

    def coll(self, kind, op, groups, in_ap, out_ap, r=(), w=()):
        return self.add("pool", lambda e: e.collective_compute(kind, op, replica_groups=groups, ins=[in_ap], outs=[out_ap]), r, w, dma=True)

    def build(self):
        nc = self.nc
        ops = self.ops
        for op in ops:
            for d in op.deps:
                dop = ops[d]
                if dop.dma or dop.eng != op.eng or op.eng not in self.inorder or op.dma:
                    dop.inc = True
        cnt = {}
        semobj = {}

        def getsem(key):
            if key not in semobj:
                semobj[key] = nc.alloc_semaphore(name=self.pfx + "s_%s_%s" % key)
            return semobj[key]

        dcnt = {}
        slotval = {}
        for op in ops:
            op.waits = []
            if op.dma:
                n = dcnt.get(op.eng, 0)
                dcnt[op.eng] = n + 1
                key = ("d" + op.eng, n % self.NSLOT)
                prev = slotval.get(key, 0)
                if prev:
                    op.waits.append((getsem(key), prev))
                op.sem, op.val = getsem(key), prev + 16
                slotval[key] = prev + 16
            elif op.inc:
                n = cnt.get(op.eng, 0)
                cnt[op.eng] = n + 1
                op.sem, op.val = getsem((op.eng, n // self.EPOCH)), n % self.EPOCH + 1
        for op in ops:
            for d in sorted(op.deps):
                dop = ops[d]
                if (not dop.dma) and (not op.dma) and dop.eng == op.eng and op.eng in self.inorder:
                    continue
                op.waits.append((dop.sem, dop.val))
        finals = [(getsem(k), v) for k, v in slotval.items()]
        with nc.Block() as block:
            for eng, bname in self.BLK.items():
                eops = [op for op in ops if op.eng == eng]
                if not eops and eng != "sp":
                    continue

                def body(e, eops=eops, eng=eng):
                    waited = {}
                    for op in eops:
                        for sem, val in op.waits:
                            if waited.get(id(sem), 0) >= val:
                                continue
                            e.wait_ge(sem, val)
                            waited[id(sem)] = val
                        ins = getattr(e, op.call[0])(*op.call[1], **op.call[2])
                        if op.inc:
                            ins.then_inc(op.sem, 16 if op.dma else 1)
                    if eng == "sp":
                        for sem, val in finals:
                            if waited.get(id(sem), 0) < val:
                                e.wait_ge(sem, val)

                getattr(block, bname)(body)
        nc.clear_and_free_semaphores(list(semobj.values()))
        nc.all_engine_barrier()
        self.st.close()
        return nc


def run(mk_or_nc, in_maps, trace=False):
    nc = mk_or_nc.nc if isinstance(mk_or_nc, MK) else mk_or_nc
    return run_bass_kernel_spmd(nc, in_maps, core_ids=list(range(len(in_maps))), trace=trace)


T = 2048
L = 64
NCH = T // L
C0 = float(np.exp(-0.5))
NEG = -1e30


class Ring:
    def __init__(self, tiles, name):
        self.t = tiles; self.n = 0; self.name = name

    def get(self):
        i = self.n % len(self.t); self.n += 1
        return self.t[i], (self.name, i)


def build_pb(do_rwkv=True, do_moba=True, npairs=8, nheads=8, nt=None, stage=3, nc=None, io=None, tag=''):
    m = MK(nc=nc)
    PADDED = io is None
    if io is None:
        zrkv = m.dram("zrkv", [3, 1024, T + 1], F32, "ExternalInput")
        zl = m.dram("zl", [768, T + 1], F32, "ExternalInput")
        mu_rkv = m.dram("mu_rkv", [128, 24], F32, "ExternalInput")
        mu_l = m.dram("mu_l", [128, 6], F32, "ExternalInput")
        w2 = m.dram("w2", [128, 1024], F32, "ExternalInput")
        a2 = m.dram("a2", [128, 1024], F32, "ExternalInput")
        g2 = m.dram("g2", [128, 4, 1024], F32, "ExternalInput")
        pp = m.dram("pp", [128, 8, 8], F32, "ExternalInput")
        consts = m.dram("consts", [128, 6, 128], F32, "ExternalInput")
        zq = m.dram("zq", [1024, T], F32, "ExternalInput")
        zk = m.dram("zkm", [1024, T], F32, "ExternalInput")
        zv = m.dram("zvm", [128, 16, 1024], F32, "ExternalInput")
        rope = m.dram("rope", [128, 2, T], F32, "ExternalInput")
        cmask = m.dram("cmask", [128, 2, 256], F32, "ExternalInput")
        identb = m.dram("identb", [128, 128], F32, "ExternalInput")
        yT = m.dram("yT", [2048, T], F32, "ExternalOutput")


    else:
        zrkv = io["zrkv"]; zl = io["zl"]; mu_rkv = io["mu_rkv"]; mu_l = io["mu_l"]; w2 = io["w2"]; a2 = io["a2"]; g2 = io["g2"]; pp = io["pp"]
        consts = io["consts"]; zq = io["zq"]; zk = io["zkm"]; zvT = io["zvT"]; rope = io["rope"]; cmask = io["cmask"]; identb = io["identb"]; yT = None
    yT_r = yT[0:1024, :] if io is None else io["yT_r"]
    yT_m = yT[1024:2048, :] if io is None else io["yT_m"]
    cst = m.sb([128, 6, 128], F32, "cst")
    m.dma(cst[:], consts, w=["cst"])
    ident = cst[:, 0, :]; bones = cst[:, 1, :]; bones64 = cst[:, 2, :]; rsw = cst[:, 3, :]
    ppt = m.sb([128, 8, 8], F32, "ppt"); m.dma(ppt[:], pp, w=["pp"])
    m.dve(lambda e: e.tensor_scalar(out=ppt[:, 7, :], in0=ppt[:, 3, :], scalar1=-1.0, scalar2=1.0, op0=ALU.mult, op1=ALU.add), r=["pp"], w=["pp"])
    murkv = m.sb([128, 24], F32, "murkv"); m.dma(murkv[:], mu_rkv, w=["mu"])
    mul = m.sb([128, 6], F32, "mul"); m.dma(mul[:], mu_l, w=["mu"])
    psr = Ring([m.ps([128, 512], F32, f"psb{i}") for i in range(7)], "ps")
    psb16 = m.ps([128, 1024], BF16, "psb16")

    if do_rwkv:
        w2t = m.sb([128, 1024], F32, "w2t"); m.dma(w2t[:], w2, w=["w2"])
        a2t = m.sb([128, 1024], F32, "a2t"); m.dma(a2t[:], a2, w=["a2"])
        g2t = m.sb([128, 4, 1024], F32, "g2t"); m.dma(g2t[:], g2, w=["g2"])
        TT = 512 if do_moba else 256
        NT = nt or (T // TT)
        CPT = TT // L
        gmask = m.sb([64, 512], F32, "gmask")
        for q in range(4):
            m.dve(lambda e, q=q: e.tensor_copy(out=gmask[:, q * 128:(q + 1) * 128], in_=cst[0:64, 4, :]), r=["cst"], w=["gmask"])
        lmask = m.sb([64, 128], F32, "lmask")
        for q in range(2):
            m.dve(lambda e, q=q: e.tensor_copy(out=lmask[:, q * 64:(q + 1) * 64], in_=cst[0:64, 5, 0:64]), r=["cst"], w=["lmask"])
        id2 = m.sb([64, 128], F32, "id2")
        for q in range(2):
            m.dve(lambda e, q=q: e.tensor_copy(out=id2[:, q * 64:(q + 1) * 64], in_=cst[0:64, 0, 0:64]), r=["cst"], w=["id2"])
        rmask = m.sb([128, TT], F32, "rmask")
        m.dve(lambda e: e.memset(rmask[:], 1.0), w=["rmask"])
        m.dve(lambda e: e.memset(rmask[:].rearrange("p (c l) -> p c l", l=L)[:, :, 0:1], 0.0), r=["rmask"], w=["rmask"])
        P = [m.sb([64, 2, 64], F32, f"P{p}") for p in range(npairs)]
        for p in range(npairs):
            m.dve(lambda e, p=p: e.memset(P[p][:], 0.0), w=[("P", p)])
        lor = [m.sb([128, TT], F32, f"lor{i}") for i in range(6)]
        zring = Ring([m.sb([128, TT + 1], F32, f"zraw{i}") for i in range(3 if do_moba else 6)], "zraw")
        NSLOT = 1 if do_moba else 4
        names = ["r", "k", "v", "a", "sg", "cl", "kkn", "kmod", "e1", "t1", "kt", "bt", "g", "y", "t2"]
        slots = [{n: m.sb([128, TT], F32, f"{n}_{s}") for n in names} for s in range(NSLOT)]
        ARs = [m.sb([128, CPT, 2, L], F32, f"AR{s}") for s in range(NSLOT)]
        los = [dict(AR=m.sb([64, CPT, 2, L], F32, f"ARlo{s}"), bt=m.sb([64, TT], F32, f"btlo{s}"), kt=m.sb([64, TT], F32, f"ktlo{s}"),
                    e1=m.sb([64, TT], F32, f"e1lo{s}"), y=m.sb([64, 2, TT], F32, f"ylo{s}")) for s in range(NSLOT)]
        smalls = [Ring([m.sb([64, 256], F32, f"sm{s}_{i}") for i in range(6)], f"sm{s}") for s in range(NSLOT)]
        gmrs = [Ring([m.sb([64, 512], F32, f"gm{s}_{i}") for i in range(2)], f"gm{s}") for s in range(NSLOT)]
        tokrs = [Ring([m.sb([64, 384], F32, f"tok{s}_{i}") for i in range(2)], f"tok{s}") for s in range(NSLOT)]
        xurs = [Ring([m.sb([64, 128], F32, f"xu{s}_{i}") for i in range(4)], f"xu{s}") for s in range(NSLOT)]

        def shift(dst, dkey, src_dram_rows, t0, mucol, extra_act=None):
            zt, zkey = zring.get()
            if PADDED:
                m.dma(zt[:], src_dram_rows[:, t0:t0 + TT + 1], w=[zkey])
            elif t0 == 0:
                m.dve(lambda e: e.memset(zt[:, 0:1], 0.0), w=[zkey])
                m.dma(zt[:, 1:TT + 1], src_dram_rows[:, 0:TT], r=[zkey], w=[zkey])
            else:
                m.dma(zt[:], src_dram_rows[:, t0 - 1:t0 + TT], w=[zkey])
            m.dve(lambda e: e.tensor_tensor(out=dst[:], in0=zt[:, 0:TT], in1=zt[:, 1:TT + 1], op=ALU.subtract),
                  r=[zkey], w=[dkey])
            m.dve(lambda e: e.scalar_tensor_tensor(out=dst[:], in0=dst[:], scalar=mucol, in1=zt[:, 1:TT + 1],
                                                   op0=ALU.mult, op1=ALU.add), r=[zkey, dkey, "mu"], w=[dkey])

        for ti in range(NT):
            t0 = ti * TT
            for i in range(6):
                shift(lor[i], ("lor", i), zl[i * 128:(i + 1) * 128, :], t0, mul[:, i:i + 1])
                if i == 0:
                    m.act(lambda e: e.activation(out=lor[0][:], in_=lor[0][:], func=AF.Tanh), r=[("lor", 0)], w=[("lor", 0)])
                elif i >= 2:
                    m.act(lambda e, i=i: e.activation(out=lor[i][:], in_=lor[i][:], func=AF.Sigmoid), r=[("lor", i)], w=[("lor", i)])
            def pair_body(p, s):
                small = smalls[s]; gmr = gmrs[s]; tokr = tokrs[s]; xur = xurs[s]
                B = slots[s]
                K = lambda n: (n, s)
                pc = slice(p * 128, (p + 1) * 128)
                par = lambda j: ppt[:, j, p:p + 1]
                AR = ARs[s]
                shift(B["r"], K("r"), zrkv[0, pc, :], t0, murkv[:, p:p + 1])
                shift(B["k"], K("k"), zrkv[1, pc, :], t0, murkv[:, 8 + p:9 + p])
                shift(B["v"], K("v"), zrkv[2, pc, :], t0, murkv[:, 16 + p:17 + p])
                yield
                ps, pk = psr.get()
                m.pe(lambda e, ps=ps: e.matmul(ps[:, 0:TT], lhsT=w2t[:, pc], rhs=lor[0][:], start=True, stop=True),
                     r=["w2", ("lor", 0)], w=[pk])
                m.act(lambda e, ps=ps: e.activation(out=B["sg"][:], in_=ps[:, 0:TT], func=AF.Sigmoid, bias=par(0)),
                      r=[pk, "pp"], w=[K("sg")])
                yield
                ps, pk = psr.get()
                m.pe(lambda e, ps=ps: e.matmul(ps[:, 0:TT], lhsT=a2t[:, pc], rhs=lor[1][:], start=True, stop=True),
                     r=["a2", ("lor", 1)], w=[pk])
                m.act(lambda e, ps=ps: e.activation(out=B["a"][:], in_=ps[:, 0:TT], func=AF.Sigmoid, bias=par(1)),
                      r=[pk, "pp"], w=[K("a")])
                yield
                ps, pk = psr.get()
                for j in range(4):
                    m.pe(lambda e, ps=ps, j=j: e.matmul(ps[:, 0:TT], lhsT=g2t[:, j, pc], rhs=lor[2 + j][:], start=(j == 0), stop=(j == 3)),
                         r=["g2", ("lor", 2 + j)], w=[pk])
                m.act(lambda e, ps=ps: e.copy(out=B["g"][:], in_=ps[:, 0:TT]), r=[pk], w=[K("g")])
                m.dve(lambda e: e.tensor_scalar(out=B["kkn"][:], in0=B["k"][:], scalar1=par(2), scalar2=None, op0=ALU.mult),
                      r=[K("k"), "pp"], w=[K("kkn")])
                m.act(lambda e: e.activation(out=B["t1"][:], in_=B["kkn"][:], func=AF.Square), r=[K("kkn")], w=[K("t1")])
                yield
                ps, pk = psr.get()
                m.pe(lambda e, ps=ps: e.matmul(ps[:, 0:TT], lhsT=bones, rhs=B["t1"][:], start=True, stop=True), r=["cst", K("t1")], w=[pk])
                m.dve(lambda e, ps=ps: e.tensor_scalar(out=B["t1"][:], in0=ps[:, 0:TT], scalar1=1e-24, scalar2=None, op0=ALU.max),
                      r=[pk], w=[K("t1")])
                m.act(lambda e: e.activation(out=B["t1"][:], in_=B["t1"][:], func=AF.Ln), r=[K("t1")], w=[K("t1")])
                m.act(lambda e: e.activation(out=B["t1"][:], in_=B["t1"][:], func=AF.Exp, scale=-0.5), r=[K("t1")], w=[K("t1")])
                m.dve(lambda e: e.tensor_tensor(out=B["kkn"][:], in0=B["kkn"][:], in1=B["t1"][:], op=ALU.mult),
                      r=[K("kkn"), K("t1")], w=[K("kkn")])
                m.dve(lambda e: e.tensor_scalar(out=B["kmod"][:], in0=B["a"][:], scalar1=par(3), scalar2=par(7), op0=ALU.mult, op1=ALU.add),
                      r=[K("a"), "pp"], w=[K("kmod")])
                m.dve(lambda e: e.tensor_tensor(out=B["kmod"][:], in0=B["kmod"][:], in1=B["k"][:], op=ALU.mult),
                      r=[K("kmod"), K("k")], w=[K("kmod")])
                m.dve(lambda e: e.tensor_tensor_scan(out=B["cl"][:], data0=rmask[:], data1=B["sg"][:], initial=0.0, op0=ALU.mult, op1=ALU.add),
                      r=["rmask", K("sg")], w=[K("cl")])
                m.act(lambda e: e.activation(out=B["e1"][:], in_=B["cl"][:], func=AF.Exp, scale=-C0), r=[K("cl")], w=[K("e1")])
                m.dve(lambda e: e.tensor_tensor(out=AR[:, :, 1, :], in0=B["r"][:].rearrange("p (c l) -> p c l", l=L),
                                                in1=B["e1"][:].rearrange("p (c l) -> p c l", l=L), op=ALU.mult),
                      r=[K("r"), K("e1")], w=[("AR", s)])
                m.dve(lambda e: e.tensor_tensor(out=B["t1"][:], in0=B["cl"][:], in1=B["sg"][:], op=ALU.subtract),
                      r=[K("cl"), K("sg")], w=[K("t1")])
                m.act(lambda e: e.activation(out=B["t1"][:], in_=B["t1"][:], func=AF.Exp, scale=-C0), r=[K("t1")], w=[K("t1")])
                m.dve(lambda e: e.scalar_tensor_tensor(out=AR[:, :, 0, :], in0=B["kkn"][:].rearrange("p (c l) -> p c l", l=L), scalar=-1.0,
                                                       in1=B["t1"][:].rearrange("p (c l) -> p c l", l=L), op0=ALU.mult, op1=ALU.mult),
                      r=[K("kkn"), K("t1"), ("AR", s)], w=[("AR", s)])
                m.act(lambda e: e.activation(out=B["t2"][:], in_=B["cl"][:], func=AF.Exp, scale=C0), r=[K("cl")], w=[K("t2")])
                m.dve(lambda e: e.tensor_tensor(out=B["kt"][:], in0=B["kmod"][:], in1=B["t2"][:], op=ALU.mult),
                      r=[K("kmod"), K("t2")], w=[K("kt")])
                m.dve(lambda e: e.tensor_tensor(out=B["bt"][:], in0=B["kkn"][:], in1=B["a"][:], op=ALU.mult),
                      r=[K("kkn"), K("a")], w=[K("bt")])
                m.dve(lambda e: e.tensor_tensor(out=B["bt"][:], in0=B["bt"][:], in1=B["t2"][:], op=ALU.mult),
                      r=[K("bt"), K("t2")], w=[K("bt")])
                if stage == 0:
                    m.dma(yT[p * 128:(p + 1) * 128, t0:t0 + TT], B["kt"][:], r=[K("kt")])
                    return
                LO = los[s]
                m.dma(LO["AR"][:], AR[64:128, :, :, :], r=[("AR", s)], w=[("ARlo", s)])
                m.dma(LO["bt"][:], B["bt"][64:128, :], r=[K("bt")], w=[("btlo", s)])
                m.dma(LO["kt"][:], B["kt"][64:128, :], r=[K("kt")], w=[("ktlo", s)])
                m.dma(LO["e1"][:], B["e1"][64:128, :], r=[K("e1")], w=[("e1lo", s)])
                ARk = [("AR", s), ("ARlo", s)]; btk = [K("bt"), ("btlo", s)]; ktk = [K("kt"), ("ktlo", s)]
                P2 = P[p]; Pk = ("P", p)
                ylo = LO["y"]
                for c in range(CPT):
                    cs = slice(c * L, (c + 1) * L)
                    ARh = [AR[0:64, c, :, :], LO["AR"][:, c, :, :]]
                    ARa = [AR[0:64, c, 0, :], LO["AR"][:, c, 0, :]]
                    ARr = [AR[0:64, c, 1, :], LO["AR"][:, c, 1, :]]
                    bth = [B["bt"][0:64, cs], LO["bt"][:, cs]]
                    kth = [B["kt"][0:64, cs], LO["kt"][:, cs]]
                    yield
                    ps, pk = psr.get()
                    for h in range(2):
                        m.pe(lambda e, ps=ps, h=h: e.matmul(ps[0:64, h * 128:(h + 1) * 128], lhsT=bth[h], rhs=ARh[h], start=True, stop=True),
                             r=[btk[h], ARk[h]], w=[pk])
                        m.pe(lambda e, ps=ps, h=h: e.matmul(ps[0:64, 256 + h * 128:256 + (h + 1) * 128], lhsT=kth[h], rhs=ARh[h], start=True, stop=True),
                             r=[ktk[h], ARk[h]], w=[pk])
                    GM, gk = gmr.get()
                    m.dve(lambda e, ps=ps, GM=GM: e.tensor_tensor(out=GM[:, :], in0=ps[0:64, :], in1=gmask[:, :], op=ALU.mult),
                          r=[pk, "gmask"], w=[gk])
                    yield
                    ps, pk = psr.get()
                    for h in range(2):
                        m.pe(lambda e, ps=ps, h=h: e.matmul(ps[0:64, h * 64:(h + 1) * 64], lhsT=ARa[h], rhs=bth[h], start=True, stop=True),
                             r=[btk[h], ARk[h]], w=[pk])
                    FE, fk = small.get()
                    m.dve(lambda e, ps=ps, FE=FE: e.tensor_tensor(out=FE[:, 0:128], in0=ps[0:64, 0:128], in1=lmask[:, :], op=ALU.mult),
                          r=[pk, "lmask"], w=[fk])
                    for h in range(2):
                        m.act(lambda e, FE=FE, GM=GM, h=h: e.copy(out=FE[:, 128 + h * 64:128 + (h + 1) * 64], in_=GM[:, h * 128:h * 128 + 64]),
                              r=[gk, fk], w=[fk])
                    yield
                    ps, pk = psr.get()
                    for j, nm in enumerate(["v", "bt", "kt"]):
                        m.pe(lambda e, ps=ps, j=j, nm=nm: e.transpose(ps[0:64, j * 128:(j + 1) * 128], B[nm][:, cs], ident),
                             r=[K(nm), "cst"], w=[pk])
                    FE0, fk0 = FE, fk
                    TOK, tk = tokr.get()
                    m.act(lambda e, ps=ps, TOK=TOK: e.copy(out=TOK[:, 0:384], in_=ps[0:64, 0:384]), r=[pk], w=[tk])
                    Tt, ttk = small.get()
                    m.dve(lambda e, Tt=Tt, FE=FE: e.tensor_tensor(out=Tt[:, 0:128], in0=FE[:, 128:256], in1=id2[:, :], op=ALU.add), r=[fk, "id2"], w=[ttk])
                    for lev in range(6):
                        if lev == 0 or lev < 5 or True:
                            yield
                            ps, pk = psr.get()
                        if lev >= 1:
                            for h in range(2):
                                f = slice(h * 64, (h + 1) * 64)
                                m.pe(lambda e, ps=ps, Tt=Tt, FE=FE, f=f: e.matmul(ps[0:64, f], lhsT=FE[:, f], rhs=Tt[:, f], start=True, stop=True),
                                     r=[fk, ttk], w=[pk])
                        if lev < 5:
                            for h in range(2):
                                f = slice(h * 64, (h + 1) * 64)
                                ef = slice(128 + h * 64, 128 + (h + 1) * 64)
                                m.pe(lambda e, ps=ps, FE=FE, f=f, ef=ef, h=h: e.matmul(ps[0:64, 128 + h * 64:128 + (h + 1) * 64], lhsT=FE[:, ef], rhs=FE[:, f], start=True, stop=True),
                                     r=[fk], w=[pk])
                                if lev < 4:
                                    m.pe(lambda e, ps=ps, FE=FE, f=f, ef=ef, h=h: e.matmul(ps[0:64, 256 + h * 64:256 + (h + 1) * 64], lhsT=FE[:, f], rhs=FE[:, ef], start=True, stop=True),
                                         r=[fk], w=[pk])
                        if lev >= 1:
                            Tn, tnk = small.get()
                            m.dve(lambda e, ps=ps, Tn=Tn, Tt=Tt: e.tensor_tensor(out=Tn[:, 0:128], in0=ps[0:64, 0:128], in1=Tt[:, 0:128], op=ALU.add), r=[pk, ttk], w=[tnk])
                            Tt, ttk = Tn, tnk
                        if lev < 5:
                            FEn, fnk = small.get()
                            wdt = 256 if lev < 4 else 128
                            m.act(lambda e, ps=ps, FEn=FEn, wdt=wdt: e.copy(out=FEn[:, 0:wdt], in_=ps[0:64, 128:128 + wdt]), r=[pk], w=[fnk])
                            FE, fk = FEn, fnk
                    if stage == 1:
                        if c == 0 and p == 0 and ti == 0:
                            m.dma(yT[1024:1088, 0:512], GM[:, :], r=[gk])
                            m.dma(yT[1088:1152, 0:384], TOK[:, 0:384], r=[tk])
                            m.dma(yT[1152:1216, 0:128], Tt[:, 0:128], r=[ttk])
                            m.dma(yT[1216:1280, 0:256], FE0[:, 0:256], r=[fk0])
                        continue
                    yield
                    ps, pk = psr.get()
                    for h in range(2):
                        f = slice(h * 64, (h + 1) * 64)
                        m.pe(lambda e, ps=ps, GM=GM, TOK=TOK, h=h, f=f: e.matmul(ps[0:64, f], lhsT=GM[:, 256 + h * 128:256 + h * 128 + 64], rhs=TOK[:, f],
                                                                                 start=True, stop=False), r=[gk, tk], w=[pk])
                        m.pe(lambda e, ps=ps, f=f, h=h: e.matmul(ps[0:64, f], lhsT=ARa[h], rhs=P2[:, h, :], start=False, stop=True),
                             r=[ARk[h], Pk], w=[pk])
                    X0, xk = xur.get()
                    m.dve(lambda e, ps=ps, X0=X0: e.tensor_copy(out=X0[:, 0:128], in_=ps[0:64, 0:128]), r=[pk], w=[xk])
                    yield
                    ps, pk = psr.get()
                    for h in range(2):
                        f = slice(h * 64, (h + 1) * 64)
                        m.pe(lambda e, ps=ps, Tt=Tt, X0=X0, f=f: e.matmul(ps[0:64, f], lhsT=Tt[:, f], rhs=X0[:, f], start=True, stop=True),
                             r=[ttk, xk], w=[pk])
                    U, uk = xur.get()
                    m.act(lambda e, ps=ps, U=U: e.copy(out=U[:, 0:128], in_=ps[0:64, 0:128]), r=[pk], w=[uk])
                    yield
                    ps, pk = psr.get()
                    for h in range(2):
                        f = slice(h * 64, (h + 1) * 64)
                        m.pe(lambda e, ps=ps, TOK=TOK, GM=GM, f=f, h=h: e.matmul(ps[0:64, f], lhsT=TOK[:, f], rhs=GM[:, 256 + h * 128 + 64:256 + (h + 1) * 128],
                                                                                 start=True, stop=False), r=[tk, gk], w=[pk])
                        m.pe(lambda e, ps=ps, U=U, GM=GM, f=f, h=h: e.matmul(ps[0:64, f], lhsT=U[:, f], rhs=GM[:, h * 128 + 64:(h + 1) * 128],
                                                                             start=False, stop=False), r=[uk, gk], w=[pk])
                        m.pe(lambda e, ps=ps, f=f, h=h: e.matmul(ps[0:64, f], lhsT=P2[:, h, :], rhs=ARr[h], start=False, stop=True),
                             r=[Pk, ARk[h]], w=[pk])
                    for h in range(2):
                        f = slice(h * 64, (h + 1) * 64)
                        o = slice(128 + h * 64, 128 + (h + 1) * 64)
                        m.pe(lambda e, ps=ps, TOK=TOK, f=f, o=o, h=h: e.matmul(ps[0:64, o], lhsT=TOK[:, 256 + h * 64:256 + (h + 1) * 64], rhs=TOK[:, f],
                                                                               start=True, stop=False), r=[tk], w=[pk])
                        m.pe(lambda e, ps=ps, TOK=TOK, U=U, f=f, o=o, h=h: e.matmul(ps[0:64, o], lhsT=TOK[:, 128 + h * 64:128 + (h + 1) * 64], rhs=U[:, f],
                                                                                    start=False, stop=True), r=[tk, uk], w=[pk])
                    m.act(lambda e, ps=ps: e.copy(out=ylo[:, :, cs], in_=ps[0:64, 0:128].rearrange("p (h t) -> p h t", h=2)), r=[pk], w=[("ylo", s)])
                    m.dve(lambda e, ps=ps: e.tensor_tensor(out=P2[:, :, :], in0=P2[:, :, :], in1=ps[0:64, 128:256].rearrange("p (h t) -> p h t", h=2), op=ALU.add),
                          r=[pk, Pk], w=[Pk])
                    gcol = c * L + L - 1
                    m.dve(lambda e, gcol=gcol: e.tensor_scalar(out=P2[:, 0, :], in0=P2[:, 0, :], scalar1=B["e1"][0:64, gcol:gcol + 1], scalar2=None, op0=ALU.mult),
                          r=[Pk, K("e1")], w=[Pk])
                    m.dve(lambda e, gcol=gcol: e.tensor_scalar(out=P2[:, 1, :], in0=P2[:, 1, :], scalar1=LO["e1"][:, gcol:gcol + 1], scalar2=None, op0=ALU.mult),
                          r=[Pk, ("e1lo", s)], w=[Pk])
                if stage == 1:
                    m.dma(yT[p * 128:(p + 1) * 128, t0:t0 + TT], B["bt"][:], r=[K("bt")])
                    return
                m.dma(B["y"][0:64, :], ylo[:, 0, :], r=[("ylo", s)], w=[K("y")])
                m.dma(B["y"][64:128, :], ylo[:, 1, :], r=[("ylo", s)], w=[K("y")])
                if stage == 2:
                    m.dma(yT[p * 128:(p + 1) * 128, t0:t0 + TT], B["y"][:], r=[K("y")])
                    return
                yield
                ps, pk = psr.get()
                m.pe(lambda e, ps=ps: e.matmul(ps[:, 0:TT], lhsT=bones64, rhs=B["y"][:], start=True, stop=True), r=["cst", K("y")], w=[pk])
                m.dve(lambda e, ps=ps: e.tensor_tensor(out=B["y"][:], in0=B["y"][:], in1=ps[:, 0:TT], op=ALU.subtract), r=[pk, K("y")], w=[K("y")])
                m.act(lambda e: e.activation(out=B["t1"][:], in_=B["y"][:], func=AF.Square), r=[K("y")], w=[K("t1")])
                yield
                ps, pk = psr.get()
                m.pe(lambda e, ps=ps: e.matmul(ps[:, 0:TT], lhsT=bones64, rhs=B["t1"][:], start=True, stop=True), r=["cst", K("t1")], w=[pk])
                m.dve(lambda e, ps=ps: e.tensor_scalar(out=B["t1"][:], in0=ps[:, 0:TT], scalar1=64e-5, scalar2=None, op0=ALU.add), r=[pk], w=[K("t1")])
                m.act(lambda e: e.activation(out=B["t1"][:], in_=B["t1"][:], func=AF.Ln), r=[K("t1")], w=[K("t1")])
                m.act(lambda e: e.activation(out=B["t1"][:], in_=B["t1"][:], func=AF.Exp, scale=-0.5), r=[K("t1")], w=[K("t1")])
                m.dve(lambda e: e.tensor_tensor(out=B["y"][:], in0=B["y"][:], in1=B["t1"][:], op=ALU.mult), r=[K("y"), K("t1")], w=[K("y")])
                m.act(lambda e: e.activation(out=B["y"][:], in_=B["y"][:], func=AF.Identity, scale=par(5), bias=par(6)), r=[K("y"), "pp"], w=[K("y")])
                m.dve(lambda e: e.scalar_tensor_tensor(out=B["t2"][:], in0=B["r"][:], scalar=par(4), in1=B["kmod"][:], op0=ALU.mult, op1=ALU.mult),
                      r=[K("r"), K("kmod"), "pp"], w=[K("t2")])
                yield
                ps, pk = psr.get()
                m.pe(lambda e, ps=ps: e.matmul(ps[:, 0:TT], lhsT=bones, rhs=B["t2"][:], start=True, stop=True), r=["cst", K("t2")], w=[pk])
                m.dve(lambda e, ps=ps: e.tensor_tensor(out=B["t2"][:], in0=ps[:, 0:TT], in1=B["v"][:], op=ALU.mult), r=[pk, K("v")], w=[K("t2")])
                m.dve(lambda e: e.tensor_tensor(out=B["y"][:], in0=B["y"][:], in1=B["t2"][:], op=ALU.add), r=[K("y"), K("t2")], w=[K("y")])
                m.dve(lambda e: e.tensor_tensor(out=B["t2"][:], in0=B["y"][:], in1=B["g"][:], op=ALU.mult), r=[K("y"), K("g"), K("t2")], w=[K("t2")])
                m.dma(yT_r[p * 128:(p + 1) * 128, t0:t0 + TT], B["t2"][:], r=[K("t2")])

            for p0 in range(0, npairs, NSLOT):
                gens = [pair_body(p0 + j, j) for j in range(min(NSLOT, npairs - p0))]
                while gens:
                    for g_ in list(gens):
                        try:
                            next(g_)
                        except StopIteration:
                            gens.remove(g_)
    if do_moba:
        ropet = m.sb([128, 2, T], F32, "ropet"); m.dma(ropet[:], rope, w=["rope"])
        cm = m.sb([128, 2, 256], F32, "cm"); m.dma(cm[:], cmask, w=["cm"])
        idb = m.sb([128, 128], BF16, "idb"); m.dma(idb[:], identb, w=["idb"], eng="pool")
        qf = m.sb([128, T], F32, "qf"); kf = m.sb([128, T], F32, "kf")
        qb = m.sb([128, T], BF16, "qb"); kb = m.sb([128, T], BF16, "kb")
        vb = m.sb([128, 16, 128], BF16, "vb")
        raw = m.sb([128, T], F32, "mraw")
        kmean = m.sb([128, 8], F32, "kmean")
        oT = m.sb([128, T], F32, "oT")
        NB = 1 if do_rwkv else 4
        sS = [m.sb([128, T], F32, f"sS{i}") for i in range(NB)]
        pB = [m.sb([128, T], BF16, f"pB{i}") for i in range(NB)]
        pT = [m.sb([128, 16, 128], BF16, f"pT{i}") for i in range(NB)]
        gt = [m.sb([128, 8], F32, f"gt{i}") for i in range(4)]
        v8 = [m.sb([128, 8], F32, f"v8{i}") for i in range(4)]
        bias = [m.sb([128, 8], F32, f"bias{i}") for i in range(4)]
        st = [m.sb([128, 4], F32, f"st{i}") for i in range(4)]
        SC = 128 ** -0.5
        for hd in range(nheads):
            hr = slice(hd * 128, (hd + 1) * 128)
            for nm, src, dstf, dstb in (("q", zq, qf, qb), ("k", zk, kf, kb)):
                m.dma(raw[:], src[hr, :], w=["mraw"])
                for tt in range(4):
                    ts_ = slice(tt * 512, (tt + 1) * 512)
                    ps, pk = psr.get()
                    m.pe(lambda e, ps=ps, ts_=ts_: e.matmul(ps[:, :], lhsT=rsw, rhs=raw[:, ts_], start=True, stop=True), r=["cst", "mraw"], w=[pk])
                    m.dve(lambda e, ps=ps, ts_=ts_, dstf=dstf: e.tensor_tensor(out=dstf[:, ts_], in0=ps[:, :], in1=ropet[:, 1, ts_], op=ALU.mult),
                          r=[pk, "rope"], w=[nm + "f"])
                m.dve(lambda e: e.tensor_tensor(out=raw[:], in0=raw[:], in1=ropet[:, 0, :], op=ALU.mult), r=["mraw", "rope"], w=["mraw"])
                m.dve(lambda e, dstf=dstf: e.tensor_tensor(out=dstf[:], in0=dstf[:], in1=raw[:], op=ALU.add), r=["mraw", nm + "f"], w=[nm + "f"])
                m.act(lambda e, dstf=dstf, dstb=dstb: e.copy(out=dstb[:], in_=dstf[:]), r=[nm + "f"], w=[nm + "b"])
            m.dve(lambda e: e.tensor_reduce(out=kmean[:], in_=kf[:].rearrange("p (n k) -> p n k", k=256), axis=AX.X, op=ALU.add),
                  r=["kf"], w=["kmean"])
            m.dve(lambda e: e.tensor_scalar(out=kmean[:], in0=kmean[:], scalar1=1.0 / 256, scalar2=None, op0=ALU.mult), r=["kmean"], w=["kmean"])
            if PADDED:
                m.dma(vb[:], zv[:, :, hr], w=["vb"], eng="pool")
            else:
                m.dma(raw[:], zvT[hr, :], w=["mraw"])
                for g4 in range(4):
                    ps, pk = psr.get()
                    for j in range(4):
                        kc = g4 * 4 + j
                        m.pe(lambda e, ps=ps, j=j, kc=kc: e.transpose(ps[:, j * 128:(j + 1) * 128], raw[:, kc * 128:(kc + 1) * 128], ident), r=["mraw", "cst"], w=[pk])
                    m.act(lambda e, ps=ps, g4=g4: e.copy(out=vb[:, g4 * 4:(g4 + 1) * 4, :], in_=ps[:, :].rearrange("p (a b) -> p a b", b=128)), r=[pk], w=["vb"])
            def qtile_body(qi):
                s = qi % NB
                blk = qi // 2
                nk = (blk + 1) * 256
                qs = slice(qi * 128, (qi + 1) * 128)
                if blk > 0:
                    yield
                    ps, pk = psr.get()
                    m.pe(lambda e, ps=ps, qs=qs: e.matmul(ps[:, 0:8], lhsT=qf[:, qs], rhs=kmean[:, :], start=True, stop=True), r=["qf", "kmean"], w=[pk])
                    m.dve(lambda e, s=s: e.memset(gt[s][:], NEG), w=[("gt", s)])
                    m.dve(lambda e, ps=ps, s=s, blk=blk: e.tensor_copy(out=gt[s][:, 0:blk], in_=ps[:, 0:blk]), r=[pk, ("gt", s)], w=[("gt", s)])
                    m.dve(lambda e, s=s: e.max(out=v8[s][:], in_=gt[s][:]), r=[("gt", s)], w=[("v8", s)])
                    m.dve(lambda e, s=s: e.tensor_scalar(out=bias[s][:], in0=gt[s][:], scalar1=v8[s][:, 2:3], scalar2=NEG, op0=ALU.is_lt, op1=ALU.mult),
                          r=[("gt", s), ("v8", s)], w=[("bias", s)])
                for kg in range((nk + 511) // 512):
                    w_ = min(512, nk - kg * 512)
                    yield
                    ps, pk = psr.get()
                    m.pe(lambda e, ps=ps, qs=qs, kg=kg, w_=w_: e.matmul(ps[:, 0:w_], lhsT=qb[:, qs], rhs=kb[:, kg * 512:kg * 512 + w_], start=True, stop=True),
                         r=["qb", "kb"], w=[pk])
                    for j in range(w_ // 256):
                        n = kg * 2 + j
                        cols = slice(n * 256, (n + 1) * 256)
                        if n < blk:
                            m.act(lambda e, ps=ps, s=s, j=j, n=n, cols=cols: e.activation(out=sS[s][:, cols], in_=ps[:, j * 256:(j + 1) * 256], func=AF.Identity,
                                                                                         scale=SC, bias=bias[s][:, n:n + 1]),
                                  r=[pk, ("bias", s)], w=[("sS", s)])
                        else:
                            m.dve(lambda e, ps=ps, s=s, j=j, cols=cols, qi=qi: e.scalar_tensor_tensor(out=sS[s][:, cols], in0=ps[:, j * 256:(j + 1) * 256], scalar=SC,
                                                                                                      in1=cm[:, qi % 2, :], op0=ALU.mult, op1=ALU.add),
                                  r=[pk, "cm"], w=[("sS", s)])
                yield
                m.dve(lambda e, s=s, nk=nk: e.tensor_reduce(out=st[s][:, 0:1], in_=sS[s][:, 0:nk], axis=AX.X, op=ALU.max), r=[("sS", s)], w=[("st", s)])
                m.dve(lambda e, s=s: e.tensor_scalar(out=st[s][:, 1:2], in0=st[s][:, 0:1], scalar1=-1.0, scalar2=None, op0=ALU.mult), r=[("st", s)], w=[("st", s)])
                m.act(lambda e, s=s, nk=nk: e.activation(out=sS[s][:, 0:nk], in_=sS[s][:, 0:nk], func=AF.Exp, bias=st[s][:, 1:2], accum_out=st[s][:, 2:3]),
                      r=[("sS", s), ("st", s)], w=[("sS", s), ("st", s)])
                yield
                m.dve(lambda e, s=s: e.reciprocal(out=st[s][:, 3:4], in_=st[s][:, 2:3]), r=[("st", s)], w=[("st", s)])
                m.dve(lambda e, s=s, nk=nk: e.tensor_scalar(out=pB[s][:, 0:nk], in0=sS[s][:, 0:nk], scalar1=st[s][:, 3:4], scalar2=None, op0=ALU.mult),
                      r=[("sS", s), ("st", s)], w=[("pB", s)])
                nkc = nk // 128
                yield
                for g0 in range(0, nkc, 8):
                    gn = min(8, nkc - g0)
                    for j in range(gn):
                        m.pe(lambda e, s=s, g0=g0, j=j: e.transpose(psb16[:, j * 128:(j + 1) * 128], pB[s][:, (g0 + j) * 128:(g0 + j + 1) * 128], idb[:]),
                             r=[("pB", s), "idb"], w=["psb16"])
                    eng = m.act if (g0 // 8) % 2 == 0 else m.dve
                    if eng is m.act:
                        m.act(lambda e, s=s, g0=g0, gn=gn: e.copy(out=pT[s][:, g0:g0 + gn, :], in_=psb16[:, 0:gn * 128].rearrange("p (a b) -> p a b", b=128)),
                              r=["psb16"], w=[("pT", s)])
                    else:
                        m.dve(lambda e, s=s, g0=g0, gn=gn: e.tensor_copy(out=pT[s][:, g0:g0 + gn, :], in_=psb16[:, 0:gn * 128].rearrange("p (a b) -> p a b", b=128)),
                              r=["psb16"], w=[("pT", s)])
                yield
                ps, pk = psr.get()
                for kc in range(nkc):
                    m.pe(lambda e, ps=ps, s=s, kc=kc, nkc=nkc: e.matmul(ps[:, 0:128], lhsT=vb[:, kc, :], rhs=pT[s][:, kc, :], start=(kc == 0), stop=(kc == nkc - 1)),
                         r=["vb", ("pT", s)], w=[pk])
                m.act(lambda e, ps=ps, qs=qs: e.copy(out=oT[:, qs], in_=ps[:, 0:128]), r=[pk], w=["oT"])
                yield
            for q0 in range(0, 16, NB):
                gens = [qtile_body(q0 + j) for j in range(NB)]
                while gens:
                    for g_ in list(gens):
                        try:
                            next(g_)
                        except StopIteration:
                            gens.remove(g_)
            m.dma(yT_m[hd * 128:(hd + 1) * 128, :], oT[:], r=["oT"])
    m.build()
    return m


def pb_consts():
    c = np.zeros((128, 6, 128), np.float32)
    c[:, 0, :] = np.eye(128)
    bo = np.zeros((128, 128), np.float32); bo[:64, :64] = 1; bo[64:, 64:] = 1
    c[:, 1, :] = bo; c[:, 2, :] = bo / 64
    R = np.zeros((128, 128), np.float32)
    for mm in range(64):
        R[mm + 64, mm] = 1; R[mm, mm + 64] = 1
    c[:, 3, :] = R
    s_ = np.arange(64)[:, None]; t_ = np.arange(64)[None, :]
    c[:64, 4, 0:64] = (s_ < t_); c[:64, 4, 64:128] = (s_ <= t_)
    c[:64, 5, 0:64] = (s_ > t_)
    return c


def rope_tables():
    inv = np.power(10000.0, -np.arange(0, 128, 2, dtype=np.float32) / 128).astype(np.float32)
    ang = np.arange(T, dtype=np.float32)[:, None] * inv[None, :]
    cos = np.cos(ang).T.astype(np.float32); sin = np.sin(ang).T.astype(np.float32)
    r = np.zeros((128, 2, T), np.float32)
    r[:64, 0] = cos; r[64:, 0] = cos
    r[:64, 1] = -sin; r[64:, 1] = sin
    return r


def causal_masks():
    cmk = np.zeros((128, 2, 256), np.float32)
    q = np.arange(128)[:, None]; k = np.arange(256)[None, :]
    cmk[:, 0, :] = np.where(k <= q, 0, NEG)
    cmk[:, 1, :] = np.where(k <= q + 128, 0, NEG)
    return cmk


D = 4096; KC = 32; TT = 512
ALPHA = float(8 ** 0.25)
LN_EPS = 1e-5


def load_mods(m, modb, tab, names=("mod",)):
    mb = m.sb([128, 6, KC], F32, "modb_sb"); tb = m.sb([128, 6, KC], F32, "modt_sb")
    m.dma(mb[:], modb, w=["modb"]); m.dma(tb[:], tab, w=["modt"])
    m.dve(lambda e: e.tensor_tensor(out=mb[:], in0=mb[:], in1=tb[:], op=ALU.add), r=["modb", "modt"], w=["modb"])
    for i in (1, 4):
        m.dve(lambda e, i=i: e.tensor_scalar_add(out=mb[:, i, :], in0=mb[:, i, :], scalar1=1.0), r=["modb"], w=["modb"])
    return mb


def build_p0():
    m = MK()
    cT = m.dram("cT", [128, KC, 4], F32, "ExternalInput")
    W = m.dram("W", [D, 3072], F32, "ExternalInput")
    bvec = m.dram("b", [1, 3072], F32, "ExternalInput")
    out = m.dram("out", [4, 3072], F32, "ExternalOutput")
    Wv = W.rearrange("(kc p) n -> p kc n", p=128)
    ct = m.sb([128, KC, 4], F32, "ct"); m.dma(ct[:], cT, w=["ct"])
    m.act(lambda e: e.activation(out=ct[:], in_=ct[:], func=AF.Silu), r=["ct"], w=["ct"])
    bt = m.sb([1, 3072], F32, "bt"); m.dma(bt[:], bvec, w=["bt"])
    ones = m.sb([1, 4], F32, "ones"); m.dve(lambda e: e.memset(ones[:], 1.0), w=["ones"])
    wr = Ring([m.sb([128, 8, 512], F32, f"w{i}") for i in range(4)], "w")
    pss = Ring([m.ps([128, 512], F32, f"ps{i}") for i in range(2)], "ps")
    ob = m.sb([4, 3072], F32, "ob")
    for g in range(6):
        n0 = g * 512
        ps, pk = pss.get()
        for q in range(4):
            wt, wk = wr.get()
            m.dma(wt[:], Wv[:, q * 8:(q + 1) * 8, n0:n0 + 512], w=[wk])
            for j in range(8):
                kc = q * 8 + j
                m.pe(lambda e, ps=ps, wt=wt, kc=kc, j=j: e.matmul(ps[0:4, :], lhsT=ct[:, kc, :], rhs=wt[:, j, :], start=(kc == 0), stop=False),
                     r=["ct", wk], w=[pk])
        m.pe(lambda e, ps=ps: e.matmul(ps[0:4, :], lhsT=ones[:, :], rhs=bt[:, n0:n0 + 512], start=False, stop=True), r=["ones", "bt"], w=[pk])
        m.dve(lambda e, ps=ps: e.tensor_copy(out=ob[:, n0:n0 + 512], in_=ps[0:4, :]), r=[pk], w=["ob"])
    m.dma(out, ob[:], r=["ob"])
    m.build()
    return m


N_IN = 13024


def build_pa(nc=None, io=None):
    m = MK(nc=nc)
    NT = 1024
    NCH = (N_IN + 127) // 128
    if io is None:
        xT = m.dram("xT", [D, NT], F32, "ExternalInput")
        modb = m.dram("modb", [128, 6, KC], F32, "ExternalInput")
        tab = m.dram("tab", [128, 6, KC], F32, "ExternalInput")
        Wt = m.dram("Wt", [NCH, 128, KC, 128], F32, "ExternalInput")
        zT = m.dram("zT", [NCH * 128, NT], F32, "ExternalOutput")
    else:
        xT = io["xT"]; modb = io["modb"]; tab = io["tab"]; Wt = io["Wt"]; zT = io["zT"]
    mod = load_mods(m, modb, tab)
    hT = m.sb([128, KC, NT], BF16, "hT")
    xr = Ring([m.sb([128, NT], F32, f"xs{i}") for i in range(3)], "xs")
    for kc in range(KC):
        xs, xk = xr.get()
        m.dma(xs[:], xT[kc * 128:(kc + 1) * 128, :], w=[xk])
        m.act(lambda e, xs=xs, kc=kc: e.activation(out=hT[:, kc, :], in_=xs[:], func=AF.Identity, scale=mod[:, 1, kc:kc + 1], bias=mod[:, 0, kc:kc + 1]),
              r=[xk, "modb"], w=[("hT", kc)])
    wr = Ring([m.sb([128, KC, 128], BF16, f"w{i}") for i in range(4)], "w")
    pss = Ring([m.ps([128, 512], F32, f"ps{i}") for i in range(6)], "ps")
    obr = Ring([m.sb([128, NT], F32, f"ob{i}") for i in range(3)], "ob")
    for nch in range(NCH):
        wt, wk = wr.get()
        m.dma(wt[:], Wt[nch], w=[wk], eng="pool")
        ob, ok = obr.get()
        for tt in range(2):
            ps, pk = pss.get()
            for kc in range(KC):
                m.pe(lambda e, ps=ps, wt=wt, kc=kc, tt=tt: e.matmul(ps[:, :], lhsT=wt[:, kc, :], rhs=hT[:, kc, tt * 512:(tt + 1) * 512], start=(kc == 0), stop=(kc == KC - 1)),
                     r=[wk, ("hT", kc)], w=[pk])
            if tt == 0:
                m.dve(lambda e, ps=ps, ob=ob: e.tensor_copy(out=ob[:, 0:512], in_=ps[:, :]), r=[pk], w=[ok])
            else:
                m.act(lambda e, ps=ps, ob=ob: e.copy(out=ob[:, 512:1024], in_=ps[:, :]), r=[pk, ok], w=[ok])
        m.dma(zT[nch * 128:(nch + 1) * 128, :], ob[:], r=[ok])
    m.build()
    return m


def build_pt(kind, nc=None, io=None):
    m = MK(nc=nc)
    even = kind == "even"
    NT = 1024
    HALO = 16
    tok0 = 0 if io is None else io.get("tok0", 0)
    if even:
        NFC = 86
        parts = [(0, 22), (22, 22), (44, 21), (65, 21)]
        plist = [(0, f0, n) for (f0, n) in parts]
    else:
        NFC = 14
        plist = [(e, 0, NFC) for e in range(8)]
    if io is None:
        if even:
            xT = m.dram("xT", [D, NT], F32, "ExternalInput")
            yT = m.dram("yT", [D, NT], F32, "ExternalInput")
            Wo = m.dram("Wo", [32, 128, KC, 128], F32, "ExternalInput")
            Wg = [m.dram("Wg", [NFC, 128, KC, 128], F32, "ExternalInput")]
            Wu = [m.dram("Wu", [NFC, 128, KC, 128], F32, "ExternalInput")]
            Wd = [m.dram("Wd", [32, 128, NFC, 128], F32, "ExternalInput")]
        else:
            xT = m.dram("xT", [D, NT + HALO], F32, "ExternalInput")
            hv = m.dram("hv", [128, 1], F32, "ExternalInput")
            icnt = m.dram("icnt", [128, 4, NT], F32, "ExternalInput")
            Wp = m.dram("Wp", [4, 8, 128, 8, 128], F32, "ExternalInput")
            pscale = m.dram("pscale", [128, KC], F32, "ExternalInput")
            Wr = m.dram("Wr", [128, KC, 8], F32, "ExternalInput")
            rb = m.dram("rb", [1, 8], F32, "ExternalInput")
            Wg = [m.dram(f"Wg{e}", [NFC, 128, KC, 128], F32, "ExternalInput") for e in range(8)]
            Wu = [m.dram(f"Wu{e}", [NFC, 128, KC, 128], F32, "ExternalInput") for e in range(8)]
            Wd = [m.dram(f"Wd{e}", [32, 128, NFC, 128], F32, "ExternalInput") for e in range(8)]
        modb = m.dram("modb", [128, 6, KC], F32, "ExternalInput")
        tab = m.dram("tab", [128, 6, KC], F32, "ExternalInput")
        lng = m.dram("lng", [128, 2, KC], F32, "ExternalInput")
        lnb = m.dram("lnb", [128, 2, KC], F32, "ExternalInput")
        cdram = m.dram("cst", [128, 2, 128], F32, "ExternalInput")
        outT = m.dram("outT", [D, NT], F32, "ExternalOutput")
    else:
        xT = io["xT"]; modb = io["modb"]; tab = io["tab"]; lng = io["lng"]; lnb = io["lnb"]; cdram = io["cst"]; outT = io["outT"]
        Wg = io["Wg"]; Wu = io["Wu"]; Wd = io["Wd"]
        if even:
            yT = io["yT"]; Wo = io["Wo"]
        else:
            icnt = io["icnt"]; Wp = io["Wp"]; pscale = io["pscale"]; Wr = io["Wr"]; rb = io["rb"]; xfull = io["xfull"]; hv = None

    mod = load_mods(m, modb, tab)
    lg = m.sb([128, 2, KC], F32, "lg"); m.dma(lg[:], lng, w=["lg"])
    lb = m.sb([128, 2, KC], F32, "lb"); m.dma(lb[:], lnb, w=["lb"])
    cst = m.sb([128, 2, 128], F32, "cst_sb"); m.dma(cst[:], cdram, w=["cst"])
    onesD = cst[:, 0, :]; ident = cst[:, 1, :]
    ones = m.sb([128, 128], F32, "ones"); m.dve(lambda e: e.memset(ones[:], 1.0), w=["ones"])

    X = m.sb([128, KC, TT], F32, "X")
    hT = m.sb([128, KC, TT], BF16, "hT")
    wr = Ring([m.sb([128, KC, 128], BF16, f"w{i}") for i in range(4 if even else 3)], "w")
    wdr = Ring([m.sb([128, 22 if even else 14, 128], BF16, f"wd{i}") for i in range(3)], "wd")
    pss = Ring([m.ps([128, 512], F32, f"ps{i}") for i in range(8)], "ps")
    tr = Ring([m.sb([128, TT], F32, f"tmp{i}") for i in range(4)], "tmp")
    act = m.sb([128, 22 if even else 14, TT], BF16, "act")
    stat = m.sb([128, 4, TT], F32, "stat")

    if not even:
        hvt = m.sb([128, 1], F32, "hvt")
        if hv is not None:
            m.dma(hvt[:], hv, w=["hvt"])
        ps_t = m.sb([128, KC], F32, "pst"); m.dma(ps_t[:], pscale, w=["pst"])
        m.dve(lambda e: e.tensor_tensor(out=ps_t[:], in0=ps_t[:], in1=mod[:, 2, :], op=ALU.mult), r=["pst", "modb"], w=["pst"])
        wrt = m.sb([128, KC, 8], F32, "wrt"); m.dma(wrt[:], Wr, w=["wrt"])
        wr1 = m.sb([128, KC, 8], F32, "wr1"); wr2 = m.sb([128, KC, 8], F32, "wr2")
        for kc in range(KC):
            m.dve(lambda e, kc=kc: e.tensor_scalar(out=wr1[:, kc, :], in0=wrt[:, kc, :], scalar1=mod[:, 4, kc:kc + 1], scalar2=None, op0=ALU.mult),
                  r=["wrt", "modb"], w=["wr1"])
            m.dve(lambda e, kc=kc: e.tensor_scalar(out=wr2[:, kc, :], in0=wrt[:, kc, :], scalar1=mod[:, 3, kc:kc + 1], scalar2=None, op0=ALU.mult),
                  r=["wrt", "modb"], w=["wr2"])
        rbt = m.sb([1, 8], F32, "rbt"); m.dma(rbt[:], rb, w=["rbt"])
        bc = m.sb([128, 8, TT], F32, "bc")
        sm = {n: m.sb([128, 8], F32, "sm_" + n) for n in ("lg", "v8", "c1", "c2", "g")}
        dg = Ring([m.sb([128, 128], F32, f"dg{i}") for i in range(2)], "dg")
        hp = Ring([m.sb([128, TT + HALO], F32, f"hp{i}") for i in range(2)], "hp")
        hq = Ring([m.sb([128, TT + HALO], F32, f"hq{i}") for i in range(2)], "hq")
        hs_ = Ring([m.sb([128, TT + HALO], F32, f"hs{i}") for i in range(3)], "hs")
        ic = m.sb([128, 4, TT], F32, "ic")

    def layer_norm(li, gate_next):
        ps1, k1 = pss.get(); ps2, k2 = pss.get()
        for kc in range(KC):
            m.pe(lambda e, kc=kc: e.matmul(ps1[:, :], lhsT=onesD, rhs=X[:, kc, :], start=(kc == 0), stop=(kc == KC - 1)), r=["cst", ("X", kc)], w=[k1])
        for kc in range(KC):
            t, tk = tr.get()
            m.act(lambda e, t=t, kc=kc: e.activation(out=t[:], in_=X[:, kc, :], func=AF.Square), r=[("X", kc)], w=[tk])
            m.pe(lambda e, t=t, kc=kc: e.matmul(ps2[:, :], lhsT=onesD, rhs=t[:], start=(kc == 0), stop=(kc == KC - 1)), r=["cst", tk], w=[k2])
        m.dve(lambda e: e.tensor_copy(out=stat[:, 0, :], in_=ps1[:, :]), r=[k1], w=["stat"])
        m.dve(lambda e: e.tensor_tensor(out=stat[:, 1, :], in0=stat[:, 0, :], in1=stat[:, 0, :], op=ALU.mult), r=["stat"], w=["stat"])
        m.dve(lambda e: e.tensor_tensor(out=stat[:, 1, :], in0=ps2[:, :], in1=stat[:, 1, :], op=ALU.subtract), r=[k2, "stat"], w=["stat"])
        m.dve(lambda e: e.tensor_scalar(out=stat[:, 1, :], in0=stat[:, 1, :], scalar1=LN_EPS, scalar2=None, op0=ALU.add), r=["stat"], w=["stat"])
        m.act(lambda e: e.activation(out=stat[:, 1, :], in_=stat[:, 1, :], func=AF.Ln), r=["stat"], w=["stat"])
        m.act(lambda e: e.activation(out=stat[:, 2, :], in_=stat[:, 1, :], func=AF.Exp, scale=-0.5), r=["stat"], w=["stat"])
        m.dve(lambda e: e.scalar_tensor_tensor(out=stat[:, 3, :], in0=stat[:, 0, :], scalar=-1.0, in1=stat[:, 2, :], op0=ALU.mult, op1=ALU.mult),
              r=["stat"], w=["stat"])
        for kc in range(KC):
            m.dve(lambda e, kc=kc: e.tensor_tensor(out=X[:, kc, :], in0=X[:, kc, :], in1=stat[:, 2, :], op=ALU.mult), r=[("X", kc), "stat"], w=[("X", kc)])
            m.dve(lambda e, kc=kc: e.tensor_tensor(out=X[:, kc, :], in0=X[:, kc, :], in1=stat[:, 3, :], op=ALU.add), r=[("X", kc), "stat"], w=[("X", kc)])
            m.act(lambda e, kc=kc: e.activation(out=X[:, kc, :], in_=X[:, kc, :], func=AF.Identity, scale=lg[:, li, kc:kc + 1], bias=lb[:, li, kc:kc + 1]),
                  r=[("X", kc), "lg", "lb"], w=[("X", kc)])
            if gate_next:
                m.act(lambda e, kc=kc: e.activation(out=hT[:, kc, :], in_=X[:, kc, :], func=AF.Identity, scale=mod[:, 4, kc:kc + 1], bias=mod[:, 3, kc:kc + 1]),
                      r=[("X", kc), "modb"], w=[("hT", kc)])

    for ti in range(2):
        c0 = ti * TT
        if even:
            for kc in range(KC):
                m.dma(hT[:, kc, :], yT[kc * 128:(kc + 1) * 128, c0:c0 + TT], w=[("hT", kc)], eng="pool")
                m.dma(X[:, kc, :], xT[kc * 128:(kc + 1) * 128, c0:c0 + TT], w=[("X", kc)])
            for dc in range(KC):
                wt, wk = wr.get()
                m.dma(wt[:], Wo[dc], w=[wk], eng="pool")
                ps, pk = pss.get()
                for kc in range(KC):
                    m.pe(lambda e, ps=ps, wt=wt, kc=kc: e.matmul(ps[:, :], lhsT=wt[:, kc, :], rhs=hT[:, kc, :], start=(kc == 0), stop=(kc == KC - 1)),
                         r=[wk, ("hT", kc)], w=[pk])
                m.act(lambda e, dc=dc: e.mul(out=X[:, dc, :], in_=X[:, dc, :], mul=ALPHA), r=[("X", dc)], w=[("X", dc)])
                m.dve(lambda e, ps=ps, dc=dc: e.scalar_tensor_tensor(out=X[:, dc, :], in0=ps[:, :], scalar=mod[:, 2, dc:dc + 1], in1=X[:, dc, :], op0=ALU.mult, op1=ALU.add),
                      r=[pk, ("X", dc), "modb"], w=[("X", dc)])
        else:
            m.dma(ic[:], icnt[:, :, c0:c0 + TT], w=["ic"])
            for kc in range(KC):
                g = kc // 8
                xh, xk = hp.get()
                g0 = tok0 + c0
                if io is None:
                    m.dma(xh[:], xT[kc * 128:(kc + 1) * 128, c0:c0 + TT + HALO], w=[xk])
                elif g0 == 0:
                    m.dve(lambda e, xh=xh: e.memset(xh[:, 0:HALO], 0.0), w=[xk])
                    m.dma(xh[:, HALO:], xfull[kc * 128:(kc + 1) * 128, 0:TT], r=[xk], w=[xk])
                else:
                    m.dma(xh[:], xfull[kc * 128:(kc + 1) * 128, g0 - HALO:g0 + TT], w=[xk])
                m.dve(lambda e, xh=xh, kc=kc: e.tensor_copy(out=X[:, kc, :], in_=xh[:, HALO:]), r=[xk], w=[("X", kc)])
                h, hk = hq.get()
                m.act(lambda e, xh=xh, h=h, kc=kc: e.activation(out=h[:], in_=xh[:], func=AF.Identity, scale=mod[:, 1, kc:kc + 1], bias=mod[:, 0, kc:kc + 1]),
                      r=[xk, "modb"], w=[hk])
                if io is None and ti == 0:
                    m.dve(lambda e, h=h: e.tensor_scalar(out=h[:, 0:HALO], in0=h[:, 0:HALO], scalar1=hvt[:, 0:1], scalar2=None, op0=ALU.mult),
                          r=[hk, "hvt"], w=[hk])
                elif io is not None and g0 == 0:
                    m.dve(lambda e, h=h: e.memset(h[:, 0:HALO], 0.0), r=[hk], w=[hk])
                s, sk = h, hk
                for st in range(g + 1):
                    step = 1 << st
                    lo = 2 * step - 1
                    s2, s2k = hs_.get()
                    m.dve(lambda e, s=s, s2=s2, step=step, lo=lo: e.tensor_tensor(out=s2[:, lo:], in0=s[:, lo:], in1=s[:, lo - step:TT + HALO - step], op=ALU.add),
                          r=[sk], w=[s2k])
                    s, sk = s2, s2k
                t, tk = tr.get()
                m.dve(lambda e, s=s, t=t, g=g: e.tensor_tensor(out=t[:], in0=s[:, HALO:], in1=ic[:, g, :], op=ALU.mult), r=[sk, "ic"], w=[tk])
                m.dve(lambda e, t=t, h=h, kc=kc: e.tensor_tensor(out=hT[:, kc, :], in0=t[:], in1=h[:, HALO:], op=ALU.subtract), r=[tk, hk], w=[("hT", kc)])
            for dc in range(KC):
                g, ec = dc // 8, dc % 8
                wt, wk = wr.get()
                m.dma(wt[:, 0:8, :], Wp[g, ec], w=[wk], eng="pool")
                ps, pk = pss.get()
                for cc in range(8):
                    m.pe(lambda e, ps=ps, wt=wt, cc=cc, g=g: e.matmul(ps[:, :], lhsT=wt[:, cc, :], rhs=hT[:, g * 8 + cc, :], start=(cc == 0), stop=(cc == 7)),
                         r=[wk, ("hT", g * 8 + cc)], w=[pk])
                m.act(lambda e, dc=dc: e.mul(out=X[:, dc, :], in_=X[:, dc, :], mul=ALPHA), r=[("X", dc)], w=[("X", dc)])
                m.dve(lambda e, ps=ps, dc=dc: e.scalar_tensor_tensor(out=X[:, dc, :], in0=ps[:, :], scalar=ps_t[:, dc:dc + 1], in1=X[:, dc, :], op0=ALU.mult, op1=ALU.add),
                      r=[pk, ("X", dc), "pst"], w=[("X", dc)])
        layer_norm(0, True)
        if not even:
            for tc_ in range(4):
                tsl = slice(tc_ * 128, (tc_ + 1) * 128)
                ps, pk = pss.get()
                for kc in range(KC):
                    m.pe(lambda e, ps=ps, kc=kc, tsl=tsl: e.matmul(ps[:, 0:8], lhsT=X[:, kc, tsl], rhs=wr1[:, kc, :], start=(kc == 0), stop=False),
                         r=[("X", kc), "wr1"], w=[pk])
                for kc in range(KC):
                    m.pe(lambda e, ps=ps, kc=kc: e.matmul(ps[:, 0:8], lhsT=ones[:, :], rhs=wr2[:, kc, :], start=False, stop=False), r=["ones", "wr2"], w=[pk])
                m.pe(lambda e, ps=ps: e.matmul(ps[:, 0:8], lhsT=ones[0:1, :], rhs=rbt[:, :], start=False, stop=True), r=["ones", "rbt"], w=[pk])
                L_ = sm["lg"]; V8 = sm["v8"]; C1 = sm["c1"]; C2 = sm["c2"]; G = sm["g"]
                m.dve(lambda e, ps=ps: e.tensor_copy(out=L_[:], in_=ps[:, 0:8]), r=[pk], w=["sm_lg"])
                m.dve(lambda e: e.max(out=V8[:], in_=L_[:]), r=["sm_lg"], w=["sm_v8"])
                m.dve(lambda e: e.tensor_tensor(out=G[:, 0:1], in0=V8[:, 0:1], in1=V8[:, 1:2], op=ALU.subtract), r=["sm_v8"], w=["sm_g"])
                m.act(lambda e: e.activation(out=G[:, 1:2], in_=G[:, 0:1], func=AF.Sigmoid), r=["sm_g"], w=["sm_g"])
                m.act(lambda e: e.activation(out=G[:, 2:3], in_=G[:, 0:1], func=AF.Sigmoid, scale=-1.0), r=["sm_g"], w=["sm_g"])
                m.dve(lambda e: e.tensor_scalar(out=C1[:], in0=L_[:], scalar1=V8[:, 0:1], scalar2=G[:, 1:2], op0=ALU.is_equal, op1=ALU.mult),
                      r=["sm_lg", "sm_v8", "sm_g"], w=["sm_c1"])
                m.dve(lambda e: e.tensor_scalar(out=C2[:], in0=L_[:], scalar1=V8[:, 1:2], scalar2=G[:, 2:3], op0=ALU.is_equal, op1=ALU.mult),
                      r=["sm_lg", "sm_v8", "sm_g"], w=["sm_c2"])
                m.dve(lambda e: e.tensor_tensor(out=C1[:], in0=C1[:], in1=C2[:], op=ALU.add), r=["sm_c1", "sm_c2"], w=["sm_c1"])
                for ex in range(8):
                    dt_, dk = dg.get()
                    m.dve(lambda e, dt_=dt_, ex=ex: e.tensor_scalar(out=dt_[:], in0=ident, scalar1=C1[:, ex:ex + 1], scalar2=None, op0=ALU.mult),
                          r=["cst", "sm_c1"], w=[dk])
                    ps2, pk2 = pss.get()
                    m.pe(lambda e, ps2=ps2, dt_=dt_: e.matmul(ps2[:, 0:128], lhsT=ones[:, :], rhs=dt_[:], start=True, stop=True), r=["ones", dk], w=[pk2])
                    m.act(lambda e, ps2=ps2, ex=ex, tsl=tsl: e.copy(out=bc[:, ex, tsl], in_=ps2[:, 0:128]), r=[pk2], w=[("bc", ex)])
        for kc in range(KC):
            m.act(lambda e, kc=kc: e.mul(out=X[:, kc, :], in_=X[:, kc, :], mul=ALPHA), r=[("X", kc)], w=[("X", kc)])
        for (ex, f0, nf) in plist:
            for fi in range(nf):
                fc = f0 + fi
                wg, wgk = wr.get()
                m.dma(wg[:], Wg[ex][fc], w=[wgk], eng="pool")
                wu, wuk = wr.get()
                m.dma(wu[:], Wu[ex][fc], w=[wuk], eng="pool")
                psg, pgk = pss.get(); psu, puk = pss.get()
                for kc in range(KC):
                    m.pe(lambda e, psg=psg, wg=wg, kc=kc: e.matmul(psg[:, :], lhsT=wg[:, kc, :], rhs=hT[:, kc, :], start=(kc == 0), stop=(kc == KC - 1)),
                         r=[wgk, ("hT", kc)], w=[pgk])
                for kc in range(KC):
                    m.pe(lambda e, psu=psu, wu=wu, kc=kc: e.matmul(psu[:, :], lhsT=wu[:, kc, :], rhs=hT[:, kc, :], start=(kc == 0), stop=(kc == KC - 1)),
                         r=[wuk, ("hT", kc)], w=[puk])
                t, tk = tr.get()
                m.act(lambda e, t=t, psg=psg: e.activation(out=t[:], in_=psg[:, :], func=AF.Silu), r=[pgk], w=[tk])
                if even:
                    m.dve(lambda e, t=t, psu=psu, fi=fi: e.tensor_tensor(out=act[:, fi, :], in0=t[:], in1=psu[:, :], op=ALU.mult), r=[tk, puk], w=[("act", fi)])
                else:
                    m.dve(lambda e, t=t, psu=psu: e.tensor_tensor(out=t[:], in0=t[:], in1=psu[:, :], op=ALU.mult), r=[tk, puk], w=[tk])
                    m.dve(lambda e, t=t, fi=fi, ex=ex: e.tensor_tensor(out=act[:, fi, :], in0=t[:], in1=bc[:, ex, :], op=ALU.mult), r=[tk, ("bc", ex)], w=[("act", fi)])
            for dc in range(KC):
                wd, wdk = wdr.get()
                m.dma(wd[:, 0:nf, :], Wd[ex][dc, :, f0:f0 + nf, :], w=[wdk], eng="pool")
                ps, pk = pss.get()
                for fi in range(nf):
                    m.pe(lambda e, ps=ps, wd=wd, fi=fi, nf=nf: e.matmul(ps[:, :], lhsT=wd[:, fi, :], rhs=act[:, fi, :], start=(fi == 0), stop=(fi == nf - 1)),
                         r=[wdk, ("act", fi)], w=[pk])
                m.dve(lambda e, ps=ps, dc=dc: e.scalar_tensor_tensor(out=X[:, dc, :], in0=ps[:, :], scalar=mod[:, 5, dc:dc + 1], in1=X[:, dc, :], op0=ALU.mult, op1=ALU.add),
                      r=[pk, ("X", dc), "modb"], w=[("X", dc)])
        layer_norm(1, False)
        for kc in range(KC):
            m.dma(outT[kc * 128:(kc + 1) * 128, c0:c0 + TT], X[:, kc, :], r=[("X", kc)])
    m.build()
    return m


def build_p0f(nc, io):
    m = MK(nc=nc)
    cT = io["cT"]; W = io["ada_w"]; bT = io["bT"]; oh = io["oh"]; out = io["modbase"]
    Wv = W.rearrange("(kc p) n -> p kc n", p=128)
    ct = m.sb([128, KC, 4], F32, "ct"); m.dma(ct[:], cT, w=["ct"])
    m.act(lambda e: e.activation(out=ct[:], in_=ct[:], func=AF.Silu), r=["ct"], w=["ct"])
    bt = m.sb([128, 192], F32, "bt"); m.dma(bt[:], bT, w=["bt"])
    oht = m.sb([128, 4], F32, "oht"); m.dma(oht[:], oh, w=["oht"])
    wr = Ring([m.sb([128, 8, 512], F32, f"w{i}") for i in range(8)], "w")
    pss = Ring([m.ps([128, 512], F32, f"ps{i}") for i in range(4)], "ps")
    baseT = m.sb([128, 192, 4], F32, "baseT")
    for g in range(48):
        n0 = g * 512
        tiles = []
        for q in range(4):
            wt, wk = wr.get()
            m.dma(wt[:], Wv[:, q * 8:(q + 1) * 8, n0:n0 + 512], w=[wk])
            tiles.append((wt, wk))
        ps, pk = pss.get()
        for j in range(4):
            for kc in range(KC):
                wt, wk = tiles[kc // 8]
                m.pe(lambda e, ps=ps, wt=wt, kc=kc, j=j: e.matmul(ps[:, j * 4:(j + 1) * 4], lhsT=wt[:, kc % 8, j * 128:(j + 1) * 128], rhs=ct[:, kc, :],
                                                                 start=(kc == 0), stop=(kc == KC - 1)), r=["ct", wk], w=[pk])
        m.dve(lambda e, ps=ps, g=g: e.tensor_copy(out=baseT[:, g * 4:(g + 1) * 4, :], in_=ps[:, 0:16].rearrange("p (a b) -> p a b", b=4)), r=[pk], w=["baseT"])
    mb = m.sb([128, 192], F32, "mb")
    m.dve(lambda e: e.tensor_scalar(out=mb[:], in0=baseT[:, :, 0], scalar1=oht[:, 0:1], scalar2=None, op0=ALU.mult), r=["baseT", "oht"], w=["mb"])
    for b in range(1, 4):
        m.dve(lambda e, b=b: e.scalar_tensor_tensor(out=mb[:], in0=baseT[:, :, b], scalar=oht[:, b:b + 1], in1=mb[:], op0=ALU.mult, op1=ALU.add),
              r=["baseT", "oht", "mb"], w=["mb"])
    m.dve(lambda e: e.tensor_tensor(out=mb[:], in0=mb[:], in1=bt[:], op=ALU.add), r=["mb", "bt"], w=["mb"])
    m.dma(out, mb[:], r=["mb"])
    m.build()
    return m


SEQ = 2048


def build_fused():
    nc = bass.Bass("TRN2", target_bir_lowering=False)

    def dr(name, shape, kind="ExternalInput"):
        return nc.dram_tensor(name, list(shape), F32, kind=kind).ap()

    xin = dr("xT", [D, SEQ]); out = dr("outT", [D, SEQ], "ExternalOutput")
    cT = dr("cT", [128, KC, 4]); ada_w = dr("ada_w", [D, 6 * D]); bT = dr("bT", [128, 192]); oh = dr("oh", [128, 4])
    tabs = dr("tabs", [4, 128, 6, KC]); lngs = dr("lngs", [4, 128, 2, KC]); lnbs = dr("lnbs", [4, 128, 2, KC])
    cstT = dr("cstT", [128, 2, 128]); pbc = dr("pbc", [128, 6, 128]); rope = dr("rope", [128, 2, SEQ]); cmask = dr("cmask", [128, 2, 256])
    identb = dr("identb", [128, 128]); icnt = dr("icnt", [128, 4, SEQ])
    ev = []
    for i in range(2):
        ev.append(dict(Wt=dr(f"Wt{i}", [102, 128, KC, 128]), mu_rkv=dr(f"mu_rkv{i}", [2, 128, 24]), mu_l=dr(f"mu_l{i}", [128, 6]),
                       w2=dr(f"w2_{i}", [2, 128, 1024]), a2=dr(f"a2_{i}", [2, 128, 1024]), g2=dr(f"g2_{i}", [2, 128, 4, 1024]), pp=dr(f"pp{i}", [2, 128, 8, 8]),
                       Wo=dr(f"Wo{i}", [32, 128, KC, 128]), Wg=dr(f"Wg{i}", [86, 128, KC, 128]), Wu=dr(f"Wu{i}", [86, 128, KC, 128]), Wd=dr(f"Wd{i}", [32, 128, 86, 128])))
    od = []
    for i in range(2):
        od.append(dict(Wp=dr(f"Wp{i}", [4, 8, 128, 8, 128]), pscale=dr(f"pscale{i}", [128, KC]), Wr=dr(f"Wr{i}", [128, KC, 8]), rb=dr(f"rb{i}", [1, 8]),
                       Wg=dr(f"mWg{i}", [8, 14, 128, KC, 128]), Wu=dr(f"mWu{i}", [8, 14, 128, KC, 128]), Wd=dr(f"mWd{i}", [8, 32, 128, 14, 128])))
    modbase = dr("modbase", [128, 192], "Internal")
    xbuf = [dr("xbufA", [D, SEQ], "Internal"), dr("xbufB", [D, SEQ], "Internal")]
    zT = dr("zT_i", [102 * 128, SEQ], "Internal")
    yfull = dr("yfull", [D, SEQ], "Internal")
    modb = modbase.rearrange("p (i k) -> p i k", k=KC)

    build_p0f(nc, dict(cT=cT, ada_w=ada_w, bT=bT, oh=oh, modbase=modbase))
    src = xin
    for l in range(4):
        i = l // 2
        dst = out if l == 3 else xbuf[l % 2]
        if l % 2 == 0:
            E = ev[i]
            for th in range(2):
                ts_ = slice(th * 1024, (th + 1) * 1024)
                build_pa(nc=nc, io=dict(xT=src[:, ts_], modb=modb, tab=tabs[l], Wt=E["Wt"], zT=zT[:, ts_]))
            z3 = zT[0:6144, :].rearrange("(s c) t -> s c t", s=3)
            for hh in range(2):
                hs = slice(hh * 1024, (hh + 1) * 1024)
                q0 = 6880 + hh * 1024
                pbio = dict(zrkv=z3[:, hs, :], zl=zT[6144:6912, :], mu_rkv=E["mu_rkv"][hh], mu_l=E["mu_l"], w2=E["w2"][hh], a2=E["a2"][hh],
                                        g2=E["g2"][hh], pp=E["pp"][hh], consts=pbc, zq=zT[q0:q0 + 1024, :], zkm=zT[q0 + 2048:q0 + 3072, :],
                                        zvT=zT[q0 + 4096:q0 + 5120, :], rope=rope, cmask=cmask, identb=identb,
                                        yT_r=yfull[hh * 1024:(hh + 1) * 1024, :], yT_m=yfull[2048 + hh * 1024:2048 + (hh + 1) * 1024, :])
                build_pb(do_rwkv=True, do_moba=False, nc=nc, io=pbio)
                build_pb(do_rwkv=False, do_moba=True, nc=nc, io=pbio)
            for th in range(2):
                ts_ = slice(th * 1024, (th + 1) * 1024)
                build_pt("even", nc=nc, io=dict(xT=src[:, ts_], yT=yfull[:, ts_], Wo=E["Wo"], Wg=[E["Wg"]], Wu=[E["Wu"]], Wd=[E["Wd"]], modb=modb, tab=tabs[l],
                                                lng=lngs[l], lnb=lnbs[l], cst=cstT, outT=dst[:, ts_]))
        else:
            O = od[i]
            for th in range(2):
                ts_ = slice(th * 1024, (th + 1) * 1024)
                build_pt("odd", nc=nc, io=dict(xT=None, xfull=src, tok0=th * 1024, icnt=icnt[:, :, ts_], Wp=O["Wp"], pscale=O["pscale"], Wr=O["Wr"], rb=O["rb"],
                                               Wg=[O["Wg"][e] for e in range(8)], Wu=[O["Wu"][e] for e in range(8)], Wd=[O["Wd"][e] for e in range(8)],
                                               modb=modb, tab=tabs[l], lng=lngs[l], lnb=lnbs[l], cst=cstT, outT=dst[:, ts_]))
        src = dst
    return nc


_NC = None


def _c(a):
    return np.ascontiguousarray(a, dtype=np.float32)


def _tile_in(W, nch):
    N = W.shape[1]
    if N < nch * 128:
        W = np.concatenate([W, np.zeros((W.shape[0], nch * 128 - N), np.float32)], 1)
    return _c(W.reshape(32, 128, nch, 128).transpose(2, 1, 0, 3))


def _tile_down(W, nfc):
    return _c(W.reshape(nfc, 128, 32, 128).transpose(2, 1, 0, 3))


def kernel(**inp):
    global _NC
    inp = {k: np.asarray(v) for k, v in inp.items()}
    x = inp["x"].astype(np.float32)
    Bn, Tn, Dn = x.shape
    sh = {}
    sh["cT"] = _c(inp["c"].T.reshape(32, 128, 4).transpose(1, 0, 2))
    sh["ada_w"] = _c(inp["ada_w"])
    sh["bT"] = _c(inp["ada_b"].reshape(192, 128).T)
    sh["tabs"] = _c(inp["ada_table"].reshape(4, 6, 32, 128).transpose(0, 3, 1, 2))
    sh["lngs"] = _c(inp["ln_g"].reshape(4, 2, 32, 128).transpose(0, 3, 1, 2))
    sh["lnbs"] = _c(inp["ln_b"].reshape(4, 2, 32, 128).transpose(0, 3, 1, 2))
    cstT = np.zeros((128, 2, 128), np.float32); cstT[:, 0, :] = 1.0 / Dn; cstT[:, 1, :] = np.eye(128)
    sh["cstT"] = cstT; sh["pbc"] = pb_consts(); sh["rope"] = rope_tables(); sh["cmask"] = causal_masks(); sh["identb"] = np.eye(128, dtype=np.float32)
    tpos = np.arange(Tn, dtype=np.float32) + 1.0
    ic = np.stack([1.0 / np.minimum(tpos, float(w)) for w in (2, 4, 8, 16)]).astype(np.float32)
    sh["icnt"] = _c(np.broadcast_to(ic[None], (128, 4, Tn)))
    DA = 2048
    for i in range(2):
        sh[f"Wt{i}"] = _tile_in(inp["mix_w_in"][i], 102)
        mu = inp["mix_mu"][i]
        sh[f"mu_rkv{i}"] = _c(np.stack([np.concatenate([mu[s * DA + hh * 1024: s * DA + hh * 1024 + 1024].reshape(8, 128).T for s in range(3)], 1) for hh in range(2)]))
        mul = np.zeros(768, np.float32); mul[:736] = mu[3 * DA:3 * DA + 736]
        sh[f"mu_l{i}"] = _c(mul.reshape(6, 128).T)
        sh[f"w2_{i}"] = _c(np.stack([inp["rwkv_w2"][i][:, hh * 1024:(hh + 1) * 1024] for hh in range(2)]))
        sh[f"a2_{i}"] = _c(np.stack([inp["rwkv_a2"][i][:, hh * 1024:(hh + 1) * 1024] for hh in range(2)]))
        g2p = np.zeros((512, DA), np.float32); g2p[:480] = inp["rwkv_g2"][i]
        sh[f"g2_{i}"] = _c(np.stack([g2p[:, hh * 1024:(hh + 1) * 1024].reshape(4, 128, 1024).transpose(1, 0, 2) for hh in range(2)]))
        pv = lambda v, hh: np.asarray(v).reshape(-1)[hh * 1024:(hh + 1) * 1024].reshape(8, 128).T
        sh[f"pp{i}"] = _c(np.stack([np.stack([pv(inp["rwkv_w0"][i], hh), pv(inp["rwkv_a0"][i], hh), pv(inp["rwkv_kk"][i], hh), pv(inp["rwkv_ka"][i], hh),
                                             pv(inp["rwkv_rk"][i], hh), pv(inp["rwkv_lnx_g"][i], hh), pv(inp["rwkv_lnx_b"][i], hh),
                                             np.ones((128, 8), np.float32)], 1) for hh in range(2)]))
        sh[f"Wo{i}"] = _tile_in(inp["mix_w_out"][i], 32)
        sh[f"Wg{i}"] = _tile_in(inp["ffn_w_gate"][i], 86); sh[f"Wu{i}"] = _tile_in(inp["ffn_w_up"][i], 86); sh[f"Wd{i}"] = _tile_down(inp["ffn_w_down"][i], 86)
        sh[f"Wp{i}"] = _c(inp["pool_w"][i].reshape(4, 8, 128, 8, 128).transpose(0, 3, 2, 1, 4))
        sh[f"pscale{i}"] = _c(inp["pool_scale"][i].reshape(32, 128).T)
        sh[f"Wr{i}"] = _c(inp["moe_router_w"][i].reshape(32, 128, 8).transpose(1, 0, 2)); sh[f"rb{i}"] = _c(inp["moe_router_b"][i][None, :])
        sh[f"mWg{i}"] = np.stack([_tile_in(inp["moe_w_gate"][i, e], 14) for e in range(8)])
        sh[f"mWu{i}"] = np.stack([_tile_in(inp["moe_w_up"][i, e], 14) for e in range(8)])
        sh[f"mWd{i}"] = np.stack([_tile_down(inp["moe_w_down"][i, e], 14) for e in range(8)])
    if _NC is None:
        _NC = build_fused()
    maps = []
    for b in range(Bn):
        d = dict(sh)
        d["xT"] = _c(x[b].T)
        ohb = np.zeros((128, 4), np.float32); ohb[:, b] = 1.0
        d["oh"] = ohb
        maps.append(d)
    res = run_bass_kernel_spmd(_NC, maps, core_ids=list(range(Bn))).results
    return np.stack([res[b]["outT"].T for b in range(Bn)]).astype(np.float32)
```
